# Optimizing a Trainium2 kernel written in Bass

```python
import math
import jax
import jax.numpy as jnp
from jax import lax
import numpy as np

D_MODEL = 1024
BATCH = 16
SEQ = 2048
DEPTH = 2

CTX_LEN = 256
GRID_W = 64
N_MIXERS = 4
GROUP_W = D_MODEL // N_MIXERS
MIX_W = N_MIXERS * GROUP_W
EPS = 1e-6
F32 = jnp.float32

HG_HEADS = 4
HG_DK = GROUP_W // HG_HEADS
HG_DV = GROUP_W // HG_HEADS
HG_CHUNK = 16
S5_CH = 16
S5_GROUPS = GROUP_W // S5_CH
S5_STATE = 64
DT_MIN = 1e-3
DT_MAX = 1e-1
DA_HEADS = 4
DA_DQK = GROUP_W // (2 * DA_HEADS)
DA_DV = GROUP_W // DA_HEADS
Q_BLOCK = 128
ROPE_BASE = 10000.0
ML_HEADS = 4
ML_DK = GROUP_W // ML_HEADS
ML_DV = GROUP_W // ML_HEADS
ML_CHUNK = 64
N_EXPERTS = 16
EC_CAPACITY = 2
D_EXPERT = 2 * D_MODEL

HG_OFF = 0
S5_OFF = HG_OFF + 5 * GROUP_W
DA_OFF = S5_OFF + GROUP_W
ML_OFF = DA_OFF + 3 * GROUP_W
ML_GATE_OFF = ML_OFF + 4 * GROUP_W
IN_COLS = ML_GATE_OFF + 4 * ML_HEADS

kernel_name = 'hybrid_diffusion_parallel_groups_ecmoe'


def rms_norm(x, w):
    xf = x.astype(F32)
    y = xf * lax.rsqrt(jnp.mean(xf * xf, axis=-1, keepdims=True) + EPS)
    return (y * w.astype(F32)).astype(x.dtype)


def head_rms_norm(x, w, n_heads):
    shp = x.shape
    xh = x.reshape(shp[:-1] + (n_heads, -1))
    return rms_norm(xh, w.reshape(n_heads, -1)).reshape(shp)


def modulated(h, norm_w, shift, scale):
    return rms_norm(h, norm_w) * (1.0 + scale) + shift


def flip_seq(a, reverse):
    return a[:, ::-1] if reverse else a


def rope_2d(n, dim):
    axis_dim = dim // 2
    inv = ROPE_BASE ** (-jnp.arange(0, axis_dim, 2, dtype=F32) / axis_dim)
    t = jnp.arange(n, dtype=jnp.int32)
    row = (t // GRID_W).astype(F32)
    col = (t % GRID_W).astype(F32)
    ang = jnp.concatenate([row[:, None] * inv, col[:, None] * inv], axis=-1)
    return jnp.cos(ang), jnp.sin(ang)


def apply_rope(x, cos, sin):
    xr = x.astype(F32).reshape(x.shape[:-1] + (-1, 2))
    x1, x2 = xr[..., 0], xr[..., 1]
    shp = (1, cos.shape[0]) + (1,) * (x.ndim - 3) + (cos.shape[1],)
    c, s = cos.reshape(shp), sin.reshape(shp)
    y = jnp.stack([x1 * c - x2 * s, x1 * s + x2 * c], axis=-1)
    return y.reshape(x.shape).astype(x.dtype)


def gla_chunked(q, k, v, log_f, s0, with_out=True):
    bsz, t_len, nh, _ = q.shape
    nc = t_len // HG_CHUNK
    def chunks(a):
        return a.astype(F32).reshape(bsz, nc, HG_CHUNK, nh, a.shape[-1])
    q, k, v, log_f = chunks(q), chunks(k), chunks(v), chunks(log_f)
    b = jnp.cumsum(log_f, axis=2)
    b_end = b[:, :, -1]
    ds = jnp.einsum('bcshd,bcshv->bchdv', k * jnp.exp(b_end[:, :, None] - b), v)
    def step(s, inp):
        g_c, ds_c = inp
        return jnp.exp(g_c)[..., None] * s + ds_c, s
    s_fin, s_start = lax.scan(step, s0, (jnp.moveaxis(b_end, 1, 0), jnp.moveaxis(ds, 1, 0)))
    if not with_out:
        return None, s_fin
    s_start = jnp.moveaxis(s_start, 0, 1)
    tri = jnp.tril(jnp.ones((HG_CHUNK, HG_CHUNK), bool))[None, None, :, :, None, None]
    decay = jnp.exp(jnp.where(tri, b[:, :, :, None] - b[:, :, None], -jnp.inf))
    scores = jnp.sum(q[:, :, :, None] * decay * k[:, :, None], axis=-1)
    o = (jnp.einsum('bctsh,bcshv->bcthv', scores, v)
         + jnp.einsum('bcthd,bchdv->bcthv', q * jnp.exp(b), s_start))
    return o.reshape(bsz, t_len, nh, -1), s_fin


def hgrn2_mixer(zc, zl, lb, norm_w, with_ctx_out):
    def heads(a):
        return a.reshape(a.shape[:2] + (HG_HEADS, -1))
    def split(z):
        q, i, ff, fb, g = jnp.split(z, 5, axis=-1)
        return heads(q) * HG_DK ** -0.5, heads(i), (heads(ff), heads(fb)), g
    qc, ic, fc, gc = split(zc)
    ql, il, fl, gl = split(zl)
    oc, ol = 0.0, 0.0
    for d in range(2):
        lbd = lb[d].reshape(HG_HEADS, HG_DK)
        f_c = lbd + (1.0 - lbd) * jax.nn.sigmoid(fc[d].astype(F32))
        f_l = lbd + (1.0 - lbd) * jax.nn.sigmoid(fl[d].astype(F32))
        s0 = jnp.zeros((zc.shape[0], HG_HEADS, HG_DK, HG_DV), F32)
        o_c, s_c = gla_chunked(flip_seq(qc, d), flip_seq(1.0 - f_c, d), flip_seq(ic, d),
                               flip_seq(jnp.log(f_c), d), s0, with_ctx_out)
        o_l, _ = gla_chunked(flip_seq(ql, d), flip_seq(1.0 - f_l, d), flip_seq(il, d),
                             flip_seq(jnp.log(f_l), d), s_c)
        ol = ol + flip_seq(o_l, d)
        if with_ctx_out:
            oc = oc + flip_seq(o_c, d)
    def readout(o, g):
        o = o.reshape(o.shape[:2] + (-1,)).astype(g.dtype)
        return head_rms_norm(o, norm_w, HG_HEADS) * jax.nn.silu(g)
    return (readout(oc, gc) if with_ctx_out else None), readout(ol, gl)


def cmul(ar, ai, br, bi):
    return ar * br - ai * bi, ar * bi + ai * br


def s5_combine(e1, e2):
    a1r, a1i, x1r, x1i = e1
    a2r, a2i, x2r, x2i = e2
    ar, ai = cmul(a2r, a2i, a1r, a1i)
    yr, yi = cmul(a2r, a2i, x1r, x1i)
    return ar, ai, yr + x2r, yi + x2i


def s5_mixer(uc, ul, lam_re, lam_im, log_dt, b_re, b_im, c_re, c_im, d_skip, glu_w, glu_b, with_ctx_out):
    def groups(u):
        return u.astype(F32).reshape(u.shape[:2] + (S5_GROUPS, S5_CH))
    gc, gl = groups(uc), groups(ul)
    b_re, b_im, c_re, c_im = (a.astype(F32) for a in (b_re, b_im, c_re, c_im))
    def b_proj(u):
        return jnp.einsum('bngc,gpc->bngp', u, b_re), jnp.einsum('bngc,gpc->bngp', u, b_im)
    def c_read(xr, xi):
        return jnp.einsum('bngp,gcp->bngc', xr, c_re) - jnp.einsum('bngp,gcp->bngc', xi, c_im)
    buc, bul = b_proj(gc), b_proj(gl)
    yc, yl = 0.0, 0.0
    for d in range(2):
        lr, li = lam_re[d].astype(F32), lam_im[d].astype(F32)
        dt = jnp.exp(log_dt[d].astype(F32))[:, None]
        mag = jnp.exp(lr * dt)
        ab_re, ab_im = mag * jnp.cos(li * dt), mag * jnp.sin(li * dt)
        den = lr * lr + li * li
        co_re = ((ab_re - 1.0) * lr + ab_im * li) / den
        co_im = (ab_im * lr - (ab_re - 1.0) * li) / den
        def run(bu, x0):
            br, bi = cmul(co_re, co_im, flip_seq(bu[0], d), flip_seq(bu[1], d))
            a_shape = (1, br.shape[1]) + ab_re.shape
            ar, ai, xr, xi = lax.associative_scan(
                s5_combine, (jnp.broadcast_to(ab_re, a_shape), jnp.broadcast_to(ab_im, a_shape), br, bi), axis=1)
            if x0 is not None:
                pr, pm = cmul(ar, ai, x0[0][:, None], x0[1][:, None])
                xr, xi = xr + pr, xi + pm
            return xr, xi
        xcr, xci = run(buc, None)
        xlr, xli = run(bul, (xcr[:, -1], xci[:, -1]))
        yl = yl + flip_seq(c_read(xlr, xli), d)
        if with_ctx_out:
            yc = yc + flip_seq(c_read(xcr, xci), d)
    dsk = d_skip.astype(F32).reshape(S5_GROUPS, S5_CH)
    def glu(y, g, like):
        y = jax.nn.gelu((y + dsk * g).reshape(g.shape[:2] + (GROUP_W,)))
        return (y * jax.nn.sigmoid(y @ glu_w.astype(F32) + glu_b.astype(F32))).astype(like.dtype)
    return (glu(yc, gc, uc) if with_ctx_out else None), glu(yl, gl, ul)


def diff_softmax_blocks(q, k, v, lam):
    bsz, nq = q.shape[:2]
    nb = nq // Q_BLOCK
    scale = DA_DQK ** -0.5
    def block(qb):
        s = jnp.einsum('bqhmd,bkhmd->bhmqk', qb, k).astype(F32) * scale
        p = jax.nn.softmax(s, axis=-1)
        a = p[:, :, 0] - lam * p[:, :, 1]
        return jnp.einsum('bhqk,bkhv->bqhv', a.astype(v.dtype), v)
    qb = jnp.moveaxis(q.reshape((bsz, nb, Q_BLOCK) + q.shape[2:]), 1, 0)
    o = lax.map(block, qb)
    return jnp.moveaxis(o, 0, 1).reshape((bsz, nq) + o.shape[3:])


def diff_attention(zc, zl, lam_q1, lam_k1, lam_q2, lam_k2, norm_w, layer_idx, with_ctx_out):
    def split(z):
        q, k, v = jnp.split(z, 3, axis=-1)
        shp = z.shape[:2]
        return (q.reshape(shp + (DA_HEADS, 2, DA_DQK)), k.reshape(shp + (DA_HEADS, 2, DA_DQK)),
                v.reshape(shp + (DA_HEADS, DA_DV)))
    qc, kc, vc = split(zc)
    ql, kl, vl = split(zl)
    cos, sin = rope_2d(zl.shape[1], DA_DQK)
    ql, kl = apply_rope(ql, cos, sin), apply_rope(kl, cos, sin)
    lam_init = 0.8 - 0.6 * math.exp(-0.3 * layer_idx)
    lam = (jnp.exp(jnp.sum(lam_q1.astype(F32) * lam_k1.astype(F32)))
           - jnp.exp(jnp.sum(lam_q2.astype(F32) * lam_k2.astype(F32))) + lam_init)
    def readout(o):
        return head_rms_norm(o.reshape(o.shape[:2] + (-1,)), norm_w, DA_HEADS) * (1.0 - lam_init)
    out_l = readout(diff_softmax_blocks(ql, jnp.concatenate([kc, kl], axis=1),
                                        jnp.concatenate([vc, vl], axis=1), lam))
    out_c = readout(diff_softmax_blocks(qc, kc, vc, lam)) if with_ctx_out else None
    return out_c, out_l


def mlstm_chunked(q, k, v, log_i, log_f, state, with_out=True):
    bsz, t_len, nh, _ = q.shape
    nc = t_len // ML_CHUNK
    def chunks(a):
        return a.astype(F32).reshape((bsz, nc, ML_CHUNK) + a.shape[2:])
    q, k, v, log_i, log_f = chunks(q), chunks(k), chunks(v), chunks(log_i), chunks(log_f)
    b = jnp.cumsum(log_f, axis=2)
    b_end = b[:, :, -1]
    w_end = b_end[:, :, None] - b + log_i
    m_loc = jnp.max(w_end, axis=2)
    e_end = jnp.exp(w_end - m_loc[:, :, None])
    c_loc = jnp.einsum('bcshv,bcshd->bchvd', e_end[..., None] * v, k)
    n_loc = jnp.einsum('bcsh,bcshd->bchd', e_end, k)
    def step(carry, inp):
        c_st, n_st, m_st = carry
        be, ml, cl, nl = inp
        m_new = jnp.maximum(be + m_st, ml)
        a_old, a_loc = jnp.exp(be + m_st - m_new), jnp.exp(ml - m_new)
        c_new = a_old[..., None, None] * c_st + a_loc[..., None, None] * cl
        n_new = a_old[..., None] * n_st + a_loc[..., None] * nl
        return (c_new, n_new, m_new), carry
    mv = lambda a: jnp.moveaxis(a, 1, 0)
    final, starts = lax.scan(step, state, (mv(b_end), mv(m_loc), mv(c_loc), mv(n_loc)))
    if not with_out:
        return None, final
    c_s, n_s, m_s = (jnp.moveaxis(a, 0, 1) for a in starts)
    tri = jnp.tril(jnp.ones((ML_CHUNK, ML_CHUNK), bool))[None, None, :, :, None]
    d_log = jnp.where(tri, b[:, :, :, None] - b[:, :, None] + log_i[:, :, None], -jnp.inf)
    w_inter = b + m_s[:, :, None]
    m_t = jnp.maximum(w_inter, jnp.max(d_log, axis=3))
    e_inter = jnp.exp(w_inter - m_t)
    wts = jnp.exp(d_log - m_t[:, :, :, None]) * jnp.einsum('bcthd,bcshd->bctsh', q, k)
    num = (jnp.einsum('bctsh,bcshv->bcthv', wts, v)
           + e_inter[..., None] * jnp.einsum('bcthd,bchvd->bcthv', q, c_s))
    den = jnp.sum(wts, axis=3) + e_inter * jnp.einsum('bcthd,bchd->bcth', q, n_s)
    h = num / jnp.maximum(jnp.abs(den), jnp.exp(-m_t))[..., None]
    return h.reshape(bsz, t_len, nh, -1), final


def mlstm_mixer(zc, zl, norm_w, with_ctx_out):
    def split(z):
        shp = z.shape[:2]
        q, k, v, o = (z[..., j * GROUP_W:(j + 1) * GROUP_W] for j in range(4))
        gates = z[..., 4 * GROUP_W:].astype(F32).reshape(shp + (4, ML_HEADS))
        hd = lambda a: a.reshape(shp + (ML_HEADS, -1))
        return hd(q), hd(k) * ML_DK ** -0.5, hd(v), o, gates
    qc, kc, vc, oc, gtc = split(zc)
    ql, kl, vl, ol, gtl = split(zl)
    bsz = zc.shape[0]
    hc, hl = 0.0, 0.0
    for d in range(2):
        state0 = (jnp.zeros((bsz, ML_HEADS, ML_DV, ML_DK), F32), jnp.zeros((bsz, ML_HEADS, ML_DK), F32),
                  jnp.zeros((bsz, ML_HEADS), F32))
        h_c, st_c = mlstm_chunked(flip_seq(qc, d), flip_seq(kc, d), flip_seq(vc, d), flip_seq(gtc[:, :, d], d),
                                  flip_seq(jax.nn.log_sigmoid(gtc[:, :, 2 + d]), d), state0, with_ctx_out)
        h_l, _ = mlstm_chunked(flip_seq(ql, d), flip_seq(kl, d), flip_seq(vl, d), flip_seq(gtl[:, :, d], d),
                               flip_seq(jax.nn.log_sigmoid(gtl[:, :, 2 + d]), d), st_c)
        hl = hl + flip_seq(h_l, d)
        if with_ctx_out:
            hc = hc + flip_seq(h_c, d)
    def readout(h, o):
        h = h.reshape(h.shape[:2] + (-1,)).astype(o.dtype)
        return head_rms_norm(h, norm_w, ML_HEADS) * jax.nn.sigmoid(o)
    return (readout(hc, oc) if with_ctx_out else None), readout(hl, ol)


def expert_choice_moe(x, router_w, w1, w3, w2):
    bsz, n, _ = x.shape
    cap = EC_CAPACITY * n // N_EXPERTS
    aff = jax.nn.softmax((x @ router_w).astype(F32), axis=-1)
    gate, idx = lax.top_k(jnp.swapaxes(aff, 1, 2), cap)
    bidx = jnp.arange(bsz)[:, None, None]
    xs = x[bidx, idx]
    def expert(args):
        xe, w1e, w3e, w2e = args
        return (jax.nn.silu(xe @ w1e) * (xe @ w3e)) @ w2e
    ye = lax.map(expert, (jnp.swapaxes(xs, 0, 1), w1, w3, w2))
    ye = jnp.swapaxes(ye, 0, 1) * gate[..., None].astype(x.dtype)
    return jnp.zeros_like(x).at[bidx, idx].add(ye)


def setup_inputs(seed: int = 0) -> dict:
    key = jax.random.key(seed)
    it = iter(jax.random.split(key, 48))
    def nrm(shape, scale):
        return scale * jax.random.normal(next(it), shape, F32)
    L, D = DEPTH, D_MODEL
    x = nrm((BATCH, SEQ, D), 1.0)
    c = nrm((BATCH, D), 1.0)
    ctx = nrm((BATCH, CTX_LEN, D), 1.0)
    c_ctx = nrm((D,), 1.0)
    mod_w = nrm((L, D, 6 * D), 0.5 * D ** -0.5)
    mod_b = nrm((L, 6 * D), 0.02)
    norm1_w = 1.0 + nrm((L, D), 0.02)
    norm2_w = 1.0 + nrm((L, D), 0.02)
    w_in = nrm((L, D, IN_COLS), D ** -0.5)
    fg_bias = jnp.linspace(3.0, 6.0, ML_HEADS, dtype=F32)
    b_in = nrm((L, IN_COLS), 0.02).at[:, ML_GATE_OFF + 2 * ML_HEADS:].add(jnp.concatenate([fg_bias, fg_bias]))
    hg_lb_logits = nrm((L, 2, GROUP_W), 1.0)
    hg_norm_w = 1.0 + nrm((L, GROUP_W), 0.02)
    s5_lam_re = -0.5 + nrm((L, 2, S5_GROUPS, S5_STATE), 0.01)
    s5_lam_im = math.pi * jnp.arange(S5_STATE, dtype=F32) + nrm((L, 2, S5_GROUPS, S5_STATE), 0.01)
    s5_log_dt = math.log(DT_MIN) + (math.log(DT_MAX) - math.log(DT_MIN)) * jax.random.uniform(
        next(it), (L, 2, S5_GROUPS), F32)
    s5_b_re = nrm((L, S5_GROUPS, S5_STATE, S5_CH), (2 * S5_CH) ** -0.5)
    s5_b_im = nrm((L, S5_GROUPS, S5_STATE, S5_CH), (2 * S5_CH) ** -0.5)
    s5_c_re = nrm((L, S5_GROUPS, S5_CH, S5_STATE), (2 * S5_STATE) ** -0.5)
    s5_c_im = nrm((L, S5_GROUPS, S5_CH, S5_STATE), (2 * S5_STATE) ** -0.5)
    s5_d = nrm((L, GROUP_W), 1.0)
    s5_glu_w = nrm((L, GROUP_W, GROUP_W), GROUP_W ** -0.5)
    s5_glu_b = nrm((L, GROUP_W), 0.02)
    da_lq1 = nrm((L, DA_DQK), 0.1)
    da_lk1 = nrm((L, DA_DQK), 0.1)
    da_lq2 = nrm((L, DA_DQK), 0.1)
    da_lk2 = nrm((L, DA_DQK), 0.1)
    da_norm_w = 1.0 + nrm((L, GROUP_W), 0.02)
    ml_norm_w = 1.0 + nrm((L, GROUP_W), 0.02)
    w_out = nrm((L, MIX_W, D), MIX_W ** -0.5)
    router_w = nrm((L, D, N_EXPERTS), D ** -0.5)
    exp_w1 = nrm((L, N_EXPERTS, D, D_EXPERT), D ** -0.5)
    exp_w3 = nrm((L, N_EXPERTS, D, D_EXPERT), D ** -0.5)
    exp_w2 = nrm((L, N_EXPERTS, D_EXPERT, D), D_EXPERT ** -0.5)
    final_norm_w = 1.0 + nrm((D,), 0.02)
    return {'x': x, 'c': c, 'ctx': ctx, 'c_ctx': c_ctx, 'mod_w': mod_w, 'mod_b': mod_b,
            'norm1_w': norm1_w, 'norm2_w': norm2_w, 'w_in': w_in, 'b_in': b_in,
            'hg_lb_logits': hg_lb_logits, 'hg_norm_w': hg_norm_w,
            's5_lam_re': s5_lam_re, 's5_lam_im': s5_lam_im, 's5_log_dt': s5_log_dt,
            's5_b_re': s5_b_re, 's5_b_im': s5_b_im, 's5_c_re': s5_c_re, 's5_c_im': s5_c_im,
            's5_d': s5_d, 's5_glu_w': s5_glu_w, 's5_glu_b': s5_glu_b,
            'da_lq1': da_lq1, 'da_lk1': da_lk1, 'da_lq2': da_lq2, 'da_lk2': da_lk2, 'da_norm_w': da_norm_w,
            'ml_norm_w': ml_norm_w, 'w_out': w_out, 'router_w': router_w,
            'exp_w1': exp_w1, 'exp_w3': exp_w3, 'exp_w2': exp_w2, 'final_norm_w': final_norm_w}


def reference(x, c, ctx, c_ctx, mod_w, mod_b, norm1_w, norm2_w, w_in, b_in, hg_lb_logits, hg_norm_w,
              s5_lam_re, s5_lam_im, s5_log_dt, s5_b_re, s5_b_im, s5_c_re, s5_c_im, s5_d, s5_glu_w, s5_glu_b,
              da_lq1, da_lk1, da_lq2, da_lk2, da_norm_w, ml_norm_w, w_out, router_w,
              exp_w1, exp_w3, exp_w2, final_norm_w):
    lb_all = jnp.cumsum(jax.nn.softmax(hg_lb_logits.astype(F32), axis=0), axis=0)
    lb_all = lb_all - lb_all[0]
    hl, hc = x, ctx
    sc, scc = jax.nn.silu(c), jax.nn.silu(c_ctx)
    for li in range(DEPTH):
        ctx_out = li < DEPTH - 1
        mod_l = jnp.split((sc @ mod_w[li] + mod_b[li])[:, None, :], 6, axis=-1)
        mod_c = jnp.split(scc @ mod_w[li] + mod_b[li], 6, axis=-1)
        zl = modulated(hl, norm1_w[li], mod_l[0], mod_l[1]) @ w_in[li] + b_in[li]
        zc = modulated(hc, norm1_w[li], mod_c[0], mod_c[1]) @ w_in[li] + b_in[li]
        a_c, a_l = hgrn2_mixer(zc[..., HG_OFF:S5_OFF], zl[..., HG_OFF:S5_OFF], lb_all[li], hg_norm_w[li], ctx_out)
        b_c, b_l = s5_mixer(zc[..., S5_OFF:DA_OFF], zl[..., S5_OFF:DA_OFF], s5_lam_re[li], s5_lam_im[li],
                            s5_log_dt[li], s5_b_re[li], s5_b_im[li], s5_c_re[li], s5_c_im[li], s5_d[li],
                            s5_glu_w[li], s5_glu_b[li], ctx_out)
        c_c, c_l = diff_attention(zc[..., DA_OFF:ML_OFF], zl[..., DA_OFF:ML_OFF], da_lq1[li], da_lk1[li],
                                  da_lq2[li], da_lk2[li], da_norm_w[li], li, ctx_out)
        d_c, d_l = mlstm_mixer(zc[..., ML_OFF:IN_COLS], zl[..., ML_OFF:IN_COLS], ml_norm_w[li], ctx_out)
        hl = hl + mod_l[2] * (jnp.concatenate([a_l, b_l, c_l, d_l], axis=-1) @ w_out[li])
        hl = hl + mod_l[5] * expert_choice_moe(modulated(hl, norm2_w[li], mod_l[3], mod_l[4]),
                                               router_w[li], exp_w1[li], exp_w3[li], exp_w2[li])
        if ctx_out:
            hc = hc + mod_c[2] * (jnp.concatenate([a_c, b_c, c_c, d_c], axis=-1) @ w_out[li])
            hc = hc + mod_c[5] * expert_choice_moe(modulated(hc, norm2_w[li], mod_c[3], mod_c[4]),
                                                   router_w[li], exp_w1[li], exp_w3[li], exp_w2[li])
    return rms_norm(hl, final_norm_w)
```

```python
import math
from contextlib import ExitStack
import numpy as np
import concourse.bass as bass
import concourse.mybir as mybir
from concourse.ap import AP
from concourse.bass_utils import run_bass_kernel_spmd

F32 = mybir.dt.float32
I32 = mybir.dt.int32
F32R = mybir.dt.float32r
ALU = mybir.AluOpType
AF = mybir.ActivationFunctionType
AX = mybir.AxisListType

D = 1024
T = 2304
NCTX = 256
NLAT = 2048
NT = T // 128
DEPTH = 2
EPS = 1e-6
IN_COLS = 3344
NZT = 3072
NZF = 3584
NDSEM = 6

ZT_A_I, ZT_A_FF, ZT_A_FB, ZT_A_G, ZT_C_V, ZT_D_K, ZT_D_V, ZT_D_O, ZT_D_IF, ZT_D_IB, ZT_D_FF, ZT_D_FB = [256 * i for i in range(12)]
ZF_A_Q, ZF_A_FF, ZF_A_FB, ZF_B_U, ZF_C_Q, ZF_C_K, ZF_C_QS, ZF_C_KS, ZF_D_Q, ZF_D_K, ZF_D_IF, ZF_D_IB, ZF_D_FF, ZF_D_FB = [256 * i for i in range(14)]


def colmaps():
    r = np.arange(256)
    HG, S5, DA, ML, MG = 0, 1280, 1536, 2304, 3328
    def gate(j):
        return MG + j * 4 + r // 64
    zt = np.concatenate([HG + 256 + r, HG + 512 + r, HG + 768 + r, HG + 1024 + r, DA + 512 + r,
                         ML + 256 + r, ML + 512 + r, ML + 768 + r, gate(0), gate(1), gate(2), gate(3)])
    zf = np.concatenate([HG + r, HG + 512 + r, HG + 768 + r, S5 + r, DA + r, DA + 256 + r, DA + (r ^ 1), DA + 256 + (r ^ 1),
                         ML + r, ML + 256 + r, gate(0), gate(1), gate(2), gate(3)])
    return zt, zf


class Sched:
    def __init__(self, nc, es):
        self.nc = nc
        self.eng = {}
        for name in ['pe', 'act', 'dve', 'pool', 'sp']:
            sem = es.enter_context(nc.semaphore("s_" + name))
            self.eng[name] = dict(sem=sem, cnt=0, seen={}, ops=[])
        self.dsem = {}
        for q in ['sp', 'act', 'pool']:
            self.dsem[q] = [[es.enter_context(nc.semaphore("d_%s%d" % (q, i))), 0] for i in range(NDSEM)]
        self.drr = {'sp': 0, 'act': 0, 'pool': 0}
        self.bufs = {}
        self.nops = 0

    def _deps(self, reads, writes):
        deps = []
        for k in reads:
            b = self.bufs.get(k)
            if b and b['w'] is not None:
                deps.append(b['w'])
        for k in writes:
            b = self.bufs.get(k)
            if b:
                if b['w'] is not None:
                    deps.append(b['w'])
                deps.extend(b['r'])
        return deps

    def _commit(self, tok, reads, writes):
        for k in reads:
            b = self.bufs.setdefault(k, dict(w=None, r=[]))
            b['r'].append(tok)
            if len(b['r']) > 48:
                best = {}
                for (s, v, e) in b['r']:
                    if id(s) not in best or best[id(s)][1] < v:
                        best[id(s)] = (s, v, e)
                b['r'] = list(best.values())
        for k in writes:
            self.bufs[k] = dict(w=tok, r=[])

    def _waits(self, ename, deps, skip_same=False):
        e = self.eng[ename]
        waits = []
        for (sem, val, src) in deps:
            if skip_same and src == ename:
                continue
            if e['seen'].get(id(sem), 0) < val:
                e['seen'][id(sem)] = val
                waits.append((sem, val))
        return waits

    def op(self, ename, fn, reads=(), writes=()):
        e = self.eng[ename]
        deps = self._deps(reads, writes)
        waits = self._waits(ename, deps, skip_same=(ename == 'pe'))
        e['cnt'] += 1
        tok = (e['sem'], e['cnt'], ename)
        e['ops'].append((waits, fn, (e['sem'], 1)))
        self._commit(tok, reads, writes)
        self.nops += 1
        return tok

    def dma(self, q, out, in_, reads=(), writes=(), indirect=None, **kw):
        e = self.eng[q]
        slot = self.dsem[q][self.drr[q] % NDSEM]
        self.drr[q] += 1
        deps = self._deps(reads, writes)
        if slot[1] > 0:
            deps.append((slot[0], 16 * slot[1], 'dma'))
        waits = self._waits(q, deps)
        slot[1] += 1
        tok = (slot[0], 16 * slot[1], 'dma')
        if indirect is None:
            fn = (lambda h: h.dma_start(out=out, in_=in_, **kw))
        else:
            fn = indirect
        e['ops'].append((waits, fn, (slot[0], 16)))
        self._commit(tok, reads, writes)
        self.nops += 1
        return tok

    def barrier(self):
        toks = []
        for name, e in self.eng.items():
            if e['cnt'] > 0:
                toks.append((e['sem'], e['cnt'], name))
        for q in self.dsem:
            for slot in self.dsem[q]:
                if slot[1] > 0:
                    toks.append((slot[0], 16 * slot[1], 'dma'))
        for name in self.eng:
            w = self._waits(name, [t for t in toks if t[2] != name or t[2] == 'dma'])
            if w:
                self.eng[name]['ops'].append((w, None, None))
        self.bufs = {}

    def finish(self):
        self.barrier()
        nc = self.nc
        handles = {'pe': 'tensor', 'act': 'scalar', 'dve': 'vector', 'pool': 'gpsimd', 'sp': 'sync'}
        with nc.Block() as block:
            for name in ['pe', 'act', 'dve', 'pool', 'sp']:
                ops = self.eng[name]['ops']

                def body(h, ops=ops):
                    for (waits, fn, inc) in ops:
                        for (sem, val) in waits:
                            h.wait_ge(sem, val)
                        if fn is not None:
                            ins = fn(h)
                            ins.then_inc(inc[0], inc[1])
                getattr(block, handles[name])(body)


def rev(ap):
    a = [list(x) for x in ap.ap]
    st, n = a[-1]
    off = ap.offset + st * (n - 1)
    a[-1] = [-st, n]
    return AP(ap.tensor, off, a)


class KB:
    def __init__(self, nc, es, debug=()):
        self.nc = nc
        self.es = es
        self.S = Sched(nc, es)
        self.debug = set(debug)
        self.dr = {}
        self.uid = 0
        self.qrr = 0

    def dram_in(self, name, shape, dt=F32):
        self.dr[name] = self.nc.dram_tensor(name, list(shape), dt, kind="ExternalInput").ap()
        return self.dr[name]

    def dram(self, name, shape, dt=F32, out=False):
        kind = "ExternalOutput" if (out or name in self.debug) else "Internal"
        self.dr[name] = self.nc.dram_tensor(name, list(shape), dt, kind=kind).ap()
        return self.dr[name]

    def phase(self):
        return Phase(self)

    def q(self):
        self.qrr += 1
        return ['sp', 'pool'][self.qrr % 2]


class Phase:
    def __init__(self, kb):
        self.kb = kb
        self.S = kb.S
        self.nc = kb.nc
        self.st = ExitStack()

    def __enter__(self):
        self.st.__enter__()
        return self

    def __exit__(self, *a):
        self.S.barrier()
        return self.st.__exit__(*a)

    def sb(self, name, shape, dt=F32):
        self.kb.uid += 1
        return self.st.enter_context(self.nc.sbuf_tensor("%s_%d" % (name, self.kb.uid), list(shape), dt))

    def ps(self, name, shape, dt=F32):
        self.kb.uid += 1
        return self.st.enter_context(self.nc.psum_tensor("%s_%d" % (name, self.kb.uid), list(shape), dt))

    def mm(self, out, lhsT, rhs, start, stop, r, w, skip=False, r32=False):
        if r32:
            if lhsT.dtype != F32R:
                lhsT = lhsT.bitcast(F32R)
            if rhs.dtype != F32R:
                rhs = rhs.bitcast(F32R)
        if skip:
            self.S.op('pe', lambda h: h.matmul(out, lhsT, rhs, start=start, stop=stop, skip_group_check=True), reads=r, writes=w)
        else:
            self.S.op('pe', lambda h: h.matmul(out, lhsT, rhs, start=start, stop=stop), reads=r, writes=w)

    def tr(self, out, in_, idn, r, w):
        self.S.op('pe', lambda h: h.transpose(out, in_, idn), reads=list(r) + ['const'], writes=w)

    def act(self, out, in_, func, r, w, bias=None, scale=None, accum=None, eng='act'):
        kw = {}
        if bias is not None:
            kw['bias'] = bias
        if scale is not None:
            kw['scale'] = scale
        if accum is not None:
            kw['accum_out'] = accum
        self.S.op('act', lambda h: h.activation(out=out, in_=in_, func=func, **kw), reads=r, writes=w)

    def tt(self, out, in0, in1, op, r, w, eng='dve'):
        self.S.op(eng, lambda h: h.tensor_tensor(out=out, in0=in0, in1=in1, op=op), reads=r, writes=w)

    def ts(self, out, in0, s1, op0, r, w, s2=None, op1=None, eng='dve', accum=None):
        kw = {}
        if op1 is not None:
            kw['op1'] = op1
        if accum is not None:
            kw['accum_out'] = accum
        self.S.op(eng, lambda h: h.tensor_scalar(out=out, in0=in0, scalar1=s1, scalar2=s2, op0=op0, **kw), reads=r, writes=w)

    def stt(self, out, in0, scalar, in1, op0, op1, r, w, eng='dve'):
        self.S.op(eng, lambda h: h.scalar_tensor_tensor(out=out, in0=in0, scalar=scalar, in1=in1, op0=op0, op1=op1), reads=r, writes=w)

    def cp(self, out, in_, r, w, eng='dve'):
        if eng == 'act':
            self.S.op('act', lambda h: h.copy(out=out, in_=in_), reads=r, writes=w)
        else:
            self.S.op(eng, lambda h: h.tensor_copy(out=out, in_=in_), reads=r, writes=w)

    def round32r(self, ap, key, eng='pool'):
        self.cp(ap.bitcast(F32R), ap, [key], [key], eng=eng)

    def memset(self, ap, val, w, eng='dve'):
        self.S.op(eng, lambda h: h.memset(ap, val), writes=w)

    def scan(self, out, d0, d1, init, r, w, op0=ALU.mult, op1=ALU.add):
        self.S.op('dve', lambda h: h.tensor_tensor_scan(out=out, data0=d0, data1=d1, initial=init, op0=op0, op1=op1), reads=r, writes=w)

    def recip(self, out, in_, r, w):
        self.S.op('dve', lambda h: h.reciprocal(out=out, in_=in_), reads=r, writes=w)

    def dma(self, out, in_, r, w, q=None, **kw):
        self.S.dma(q or self.kb.q(), out, in_, reads=r, writes=w, **kw)


def load_consts(kb, P):
    c = {}
    idn = P.sb("idn", [128, 128])
    P.dma(idn[:], kb.dr['c_idn'], [], ['const'])
    ones = P.sb("ones", [128, 128])
    P.memset(ones[:], 1.0, ['const'])
    onecol = P.sb("onecol", [128, 1])
    P.memset(onecol[:], 1.0, ['const'])
    c['onecol'] = onecol
    c['idn'] = idn
    c['ones'] = ones
    return c


def phase_mod(kb, C, li):
    dr = kb.dr
    with kb.phase() as P:
        cT = P.sb("cT", [128, 3, 8])
        for s_ in range(2):
            P.dma(cT[:, s_, :], dr['c'][s_].rearrange("(k p) -> p k", p=128), [], ['cT'], q='sp', allow_slow_non_contiguous=True)
        P.dma(cT[:, 2, :], dr['c_ctx'].rearrange("(k p) -> p k", p=128), [], ['cT'], q='sp', allow_slow_non_contiguous=True)
        scT = P.sb("scT", [128, 3, 8])
        P.act(scT[:], cT[:], AF.Silu, ['cT'], ['scT'])
        modsb = P.sb("modsb", [3, 6 * D])
        nw = P.sb("nw", [3, 2, D])
        P.dma(nw[:, 0, :], dr['norm1_w'][li].partition_broadcast(3), [], ['nw'], q='sp')
        P.dma(nw[:, 1, :], dr['norm2_w'][li].partition_broadcast(3), [], ['nw'], q='sp')
        wb = [P.sb("mw%d" % i, [128, 8, 512]) for i in range(2)]
        bb = [P.sb("mb%d" % i, [1, 512]) for i in range(2)]
        pp = [P.ps("mps%d" % i, [3, 512]) for i in range(2)]
        for cb in range(12):
            i = cb % 2
            P.dma(wb[i][:], dr['mod_w'][li][:, cb * 512:(cb + 1) * 512].rearrange("(k p) c -> p k c", p=128), [], ['mw%d' % i])
            P.dma(bb[i][:], dr['mod_b'][li:li + 1, cb * 512:(cb + 1) * 512], [], ['mb%d' % i], q='sp')
            for kc in range(8):
                P.mm(pp[i][:], scT[:, :, kc], wb[i][:, kc, :], kc == 0, False, ['scT', 'mw%d' % i], ['mps%d' % i])
            P.mm(pp[i][:], C['ones'][0:1, 0:3], bb[i][:], False, True, ['const', 'mb%d' % i], ['mps%d' % i])
            P.cp(modsb[:, cb * 512:(cb + 1) * 512], pp[i][:], ['mps%d' % i], ['modsb'], eng='act')
        for j, ch in enumerate([1, 4]):
            P.stt(modsb[:, ch * D:(ch + 1) * D], modsb[:, ch * D:(ch + 1) * D], 1.0, nw[:, j, :], ALU.add, ALU.mult, ['modsb', 'nw'], ['modsb'])
        P.dma(dr['MODV'], modsb[:], ['modsb'], ['MODV'], q='sp')


def norm_mod_tile(P, C, ht, xm, weff, shift, tag):
    junk = P._junk
    ss = P._ss
    P.memset(ss[:], 0.0, ['ss'])
    P.act(junk[:], ht, AF.Square, [tag + 'h', 'ss'], ['junk', 'ss'], accum=ss[:])
    P.act(ss[:], ss[:], AF.Sqrt, ['ss'], ['ss'], scale=1.0 / D, bias=P._eps[:])
    P.recip(ss[:], ss[:], ['ss'], ['ss'])
    P.stt(xm, ht, ss[:], weff, ALU.mult, ALU.mult, [tag + 'h', 'ss', 'modbc'], [tag + 'xm'])
    if shift is not None:
        P.tt(xm, xm, shift, ALU.add, [tag + 'xm', 'modbc'], [tag + 'xm'])


def phase_inproj(kb, C, li, s):
    dr = kb.dr
    with kb.phase() as P:
        xmT = P.sb("xmT", [128, 8, T])
        with Sub(P) as Q:
            Q._junk = Q.sb("junk", [128, D])
            Q._ss = Q.sb("ss", [128, 1])
            Q._eps = Q.sb("eps", [128, 1])
            Q.memset(Q._eps[:], EPS, ['eps'])
            modbc = Q.sb("modbc", [128, 2, 2, D])
            for si, st in enumerate([2, s]):
                Q.dma(modbc[:, si, 0, :], dr['MODV'][st, 0:D].partition_broadcast(128), ['MODV'], ['modbc'], q='sp')
                Q.dma(modbc[:, si, 1, :], dr['MODV'][st, D:2 * D].partition_broadcast(128), ['MODV'], ['modbc'], q='sp')
            hts = [Q.sb("ht%d" % i, [128, D]) for i in range(2)]
            xms = [Q.sb("xm%d" % i, [128, D]) for i in range(2)]
            tps = [Q.ps("tp%d" % i, [128, 1024]) for i in range(2)]
            for tt in range(NT):
                i = tt % 2
                si = 0 if tt < 2 else 1
                Q.dma(hts[i][:], dr['H'][s, tt * 128:(tt + 1) * 128, :], ['H%d' % s], ['%dh' % i])
                norm_mod_tile(Q, C, hts[i][:], xms[i][:], modbc[:, si, 1, :], modbc[:, si, 0, :], '%d' % i)
                for kc in range(8):
                    Q.tr(tps[i][:, kc * 128:(kc + 1) * 128], xms[i][:, kc * 128:(kc + 1) * 128], C['idn'][:], ['%dxm' % i], ['tp%d' % i])
                Q.cp(xmT[:, :, tt * 128:(tt + 1) * 128].bitcast(F32R), tps[i][:].rearrange("p (k t) -> p k t", k=8), ['tp%d' % i], ['xmT'], eng=('act' if tt % 2 else 'dve'))
        wraw = P.sb("wraw", [128, 8, 512])
        wts = [P.sb("wt%d" % i, [128, 8, 512]) for i in range(2)]
        bts = [P.sb("bt%d" % i, [1, 512]) for i in range(2)]
        ops_ = [P.ps("ops%d" % i, [128, 512]) for i in range(4)]
        stg = [P.sb("stg%d" % i, [128, 512]) for i in range(2)]
        n = 0
        for cb in range(NZT // 512):
            i = cb % 2
            P.dma(wraw[:], dr['WT'][li][:, cb * 512:(cb + 1) * 512].rearrange("(k p) c -> p k c", p=128), [], ['wraw'])
            P.dma(bts[i][:], dr['BT'][li:li + 1, cb * 512:(cb + 1) * 512], [], ['bt%d' % i], q='sp')
            P.cp(wts[i][:, 0:4, :].bitcast(F32R), wraw[:, 0:4, :], ['wraw'], ['wt%d' % i], eng='act')
            P.cp(wts[i][:, 4:8, :].bitcast(F32R), wraw[:, 4:8, :], ['wraw'], ['wt%d' % i], eng='dve')
            for tt in range(NT):
                j = n % 4
                n += 1
                for kc in range(8):
                    P.mm(ops_[j][:], xmT[:, kc, tt * 128:(tt + 1) * 128], wts[i][:, kc, :], kc == 0, False, ['xmT', 'wt%d' % i], ['ops%d' % j], r32=True)
                P.mm(ops_[j][:], C['ones'][0:1, 0:128], bts[i][:], False, True, ['const', 'bt%d' % i], ['ops%d' % j])
                P.cp(stg[j % 2][:], ops_[j][:], ['ops%d' % j], ['stg%d' % (j % 2)], eng=('act' if j % 2 else 'dve'))
                P.dma(dr['ZT'][s, tt * 128:(tt + 1) * 128, cb * 512:(cb + 1) * 512], stg[j % 2][:], ['stg%d' % (j % 2)], ['ZT%d' % s])
        wfraw = P.sb("wfraw", [128, 8, 128])
        wfs = [P.sb("wf%d" % i, [128, 8, 128]) for i in range(2)]
        bF = P.sb("bF", [128, NZF // 128])
        P.dma(bF[:], dr['BF'][li].rearrange("(m p) -> p m", p=128), [], ['bF'], q='sp', allow_slow_non_contiguous=True)
        stf = [P.sb("stf%d" % i, [128, T]) for i in range(2)]
        for m in range(NZF // 128):
            i = m % 2
            P.dma(wfraw[:], dr['WF'][li][:, m * 128:(m + 1) * 128].rearrange("(k p) c -> p k c", p=128), [], ['wfraw'])
            P.cp(wfs[i][:].bitcast(F32R), wfraw[:], ['wfraw'], ['wf%d' % i], eng='pool' if False else 'dve')
            for tg in range(5):
                t0 = tg * 512
                tn = min(512, T - t0)
                j = n % 4
                n += 1
                for kc in range(8):
                    P.mm(ops_[j][:, 0:tn], wfs[i][:, kc, :], xmT[:, kc, t0:t0 + tn], kc == 0, kc == 7, ['xmT', 'wf%d' % i], ['ops%d' % j], r32=True)
                P.act(stf[i][:, t0:t0 + tn], ops_[j][:, 0:tn], AF.Identity, ['ops%d' % j, 'bF'], ['stf%d' % i], bias=bF[:, m:m + 1])
            P.dma(dr['ZF'][s, m * 128:(m + 1) * 128, :], stf[i][:], ['stf%d' % i], ['ZF%d' % s])


class Sub:
    def __init__(self, P):
        self.P = P
    def __enter__(self):
        self.saved = self.P.st
        self.P.st = ExitStack()
        self.P.st.__enter__()
        return self.P
    def __exit__(self, *a):
        self.P.S.barrier()
        r = self.P.st.__exit__(*a)
        self.P.st = self.saved
        return r


def pp_of_chunk(c, d):
    if d == 0:
        return c
    return (7 - c) if c < 8 else 8 + (71 - c)


def phase_lb(kb, C):
    dr = kb.dr
    with kb.phase() as P:
        a = P.sb("lba", [1, 2, 512])
        P.dma(a[:, 0, :], dr['hg_lb_logits'][0:1].rearrange("o d c -> o (d c)"), [], ['lba'], q='sp')
        P.dma(a[:, 1, :], dr['hg_lb_logits'][1:2].rearrange("o d c -> o (d c)"), [], ['lba'], q='sp')
        o = P.sb("lbo", [1, 2, 512])
        P.memset(o[:], 0.0, ['lbo'])
        P.tt(a[:, 0, :], a[:, 1, :], a[:, 0, :], ALU.subtract, ['lba'], ['lba'])
        P.act(o[:, 1, :], a[:, 0, :], AF.Sigmoid, ['lba', 'lbo'], ['lbo'])
        P.dma(dr['LB'].rearrange("(o l) d c -> o l (d c)", o=1), o[:], ['lbo'], ['LB'], q='sp')


def phase_gla(kb, C, li, s, mixer):
    dr = kb.dr
    ML = (mixer == 'D')
    dv = 65 if ML else 64
    ZT, ZF = dr['ZT'][s], dr['ZF'][s]
    def ztile(colbase):
        return ZT[:, colbase:colbase + 256].rearrange("(a p) c -> p a c", p=128)
    with kb.phase() as P:
        vaug = P.sb("vaug", [128, NT, 4, dv])
        oacc = P.sb("oacc", [128, NT, 4, 64])
        khat = P.sb("khat", [128, NT, 256])
        msk = P.sb("msk", [128, 2, 128])
        onesbd = P.sb("onesbd", [128, 128])
        chm = P.sb("chm", [128, 4])
        rm = P.sb("rm", [64, T])
        P.dma(msk[:, 0, :], dr['c_maskf'], [], ['msk'], q='sp')
        P.dma(msk[:, 1, :], dr['c_maskb'], [], ['msk'], q='sp')
        P.dma(onesbd[:], dr['c_onesbd'], [], ['onesbd'], q='sp')
        P.dma(chm[:], dr['c_chm'], [], ['chm'], q='sp')
        if ML:
            P.memset(vaug[:], 1.0, ['vaug'])
        vsrc = ztile(ZT_D_V if ML else ZT_A_I)
        for tt_ in range(NT):
            P.dma(vaug[:, tt_, :, 0:64], vsrc[:, tt_, :].rearrange("p (h v) -> p h v", h=4), ['ZT%d' % s], ['vaug'])
        for d in range(2):
            P.dma(rm[:], dr['c_rm'][d], [], ['rm'], q='sp')
            with Sub(P) as Q:
                zf = Q.sb("zf", [128, NT, 256])
                kt = Q.sb("kt", [128, NT, 256])
                bt = Q.sb("bt", [128, NT, 256])
                be = Q.sb("be", [128, NT, 256])
                pb = [Q.ps("pb%d" % i, [128, 512]) for i in range(2)]
                if ML:
                    Q.dma(zf[:], ztile(ZT_D_FF + 256 * d), ['ZT%d' % s], ['zf'])
                    Q.dma(kt[:], ztile(ZT_D_K), ['ZT%d' % s], ['kt'])
                    Q.dma(bt[:], ztile(ZT_D_IF + 256 * d), ['ZT%d' % s], ['bt'])
                    Q.act(bt[:], bt[:], AF.Exp, ['bt'], ['bt'])
                    Q.stt(kt[:], kt[:], 0.125, bt[:], ALU.mult, ALU.mult, ['kt', 'bt'], ['kt'])
                    Q.act(zf[:], zf[:], AF.Exp, ['zf'], ['zf'], scale=-1.0)
                    Q.act(zf[:], zf[:], AF.Ln, ['zf'], ['zf'], bias=C['onecol'][:])
                    Q.ts(zf[:], zf[:], -1.0, ALU.mult, ['zf'], ['zf'])
                else:
                    lbb = Q.sb("lbb", [128, 2, 256])
                    Q.dma(zf[:], ztile(ZT_A_FF + 256 * d), ['ZT%d' % s], ['zf'])
                    Q.dma(lbb[:, 0, :], dr['LB'][li, d].partition_broadcast(128), ['LB'], ['lbb'], q='sp')
                    Q.ts(lbb[:, 1, :], lbb[:, 0, :], -1.0, ALU.mult, ['lbb'], ['lbb'], s2=1.0, op1=ALU.add)
                    Q.act(zf[:], zf[:], AF.Sigmoid, ['zf'], ['zf'])
                    Q.tt(zf[:], zf[:], lbb[:, None, 1, :].to_broadcast([128, NT, 256]), ALU.mult, ['zf', 'lbb'], ['zf'])
                    Q.tt(zf[:], zf[:], lbb[:, None, 0, :].to_broadcast([128, NT, 256]), ALU.add, ['zf', 'lbb'], ['zf'])
                    Q.ts(kt[:], zf[:], -1.0, ALU.mult, ['zf'], ['kt'], s2=1.0, op1=ALU.add)
                    Q.act(zf[:], zf[:], AF.Ln, ['zf'], ['zf'])
                zff = zf[:].rearrange("p a c -> p (a c)")
                btf = bt[:].rearrange("p a c -> p (a c)")
                bef = be[:].rearrange("p a c -> p (a c)")
                for j in range(NT * 256 // 512):
                    i = j % 2
                    Q.mm(pb[i][:], msk[:, d, :], zff[:, j * 512:(j + 1) * 512], True, True, ['msk', 'zf'], ['pb%d' % i])
                    Q.cp(btf[:, j * 512:(j + 1) * 512], pb[i][:], ['pb%d' % i], ['bt'], eng='act')
                    Q.mm(pb[i][:], onesbd[:], zff[:, j * 512:(j + 1) * 512], True, True, ['onesbd', 'zf'], ['pb%d' % i])
                    Q.tt(bef[:, j * 512:(j + 1) * 512], pb[i][:], btf[:, j * 512:(j + 1) * 512], ALU.subtract, ['pb%d' % i, 'bt'], ['be'])
                Q.act(be[:], be[:], AF.Exp, ['be'], ['be'])
                Q.tt(khat[:], kt[:], be[:], ALU.mult, ['kt', 'be'], ['khat'])
            for h in range(4):
                with Sub(P) as Q:
                    qT = Q.sb("qT", [64, T]); kT = Q.sb("kT", [64, T]); fT = Q.sb("fT", [64, T]); eb = Q.sb("eb", [64, T])
                    r0 = h * 64
                    if ML:
                        Q.dma(qT[:], ZF[ZF_D_Q + r0:ZF_D_Q + r0 + 64, :], ['ZF%d' % s], ['qT'])
                        Q.dma(kT[:], ZF[ZF_D_K + r0:ZF_D_K + r0 + 64, :], ['ZF%d' % s], ['kT'])
                        Q.dma(fT[:], ZF[ZF_D_FF + 256 * d + r0:ZF_D_FF + 256 * d + r0 + 64, :], ['ZF%d' % s], ['fT'])
                        Q.dma(eb[:], ZF[ZF_D_IF + 256 * d + r0:ZF_D_IF + 256 * d + r0 + 64, :], ['ZF%d' % s], ['eb'])
                        Q.act(eb[:], eb[:], AF.Exp, ['eb'], ['eb'])
                        Q.stt(kT[:], kT[:], 0.125, eb[:], ALU.mult, ALU.mult, ['kT', 'eb'], ['kT'])
                        Q.act(fT[:], fT[:], AF.Exp, ['fT'], ['fT'], scale=-1.0)
                        Q.act(fT[:], fT[:], AF.Ln, ['fT'], ['fT'], bias=C['onecol'][0:64, :])
                        Q.ts(fT[:], fT[:], -1.0, ALU.mult, ['fT'], ['fT'])
                    else:
                        lbc = Q.sb("lbc", [64, 2])
                        Q.dma(qT[:], ZF[ZF_A_Q + r0:ZF_A_Q + r0 + 64, :], ['ZF%d' % s], ['qT'])
                        Q.dma(fT[:], ZF[ZF_A_FF + 256 * d + r0:ZF_A_FF + 256 * d + r0 + 64, :], ['ZF%d' % s], ['fT'])
                        Q.dma(lbc[:, 0:1], dr['LB'][li, d, r0:r0 + 64].rearrange("(p o) -> p o", o=1), ['LB'], ['lbc'], q='sp', allow_slow_non_contiguous=True)
                        Q.ts(lbc[:, 1:2], lbc[:, 0:1], -1.0, ALU.mult, ['lbc'], ['lbc'], s2=1.0, op1=ALU.add)
                        Q.act(fT[:], fT[:], AF.Sigmoid, ['fT'], ['fT'])
                        Q.ts(fT[:], fT[:], lbc[:, 1:2], ALU.mult, ['fT', 'lbc'], ['fT'], s2=lbc[:, 0:1], op1=ALU.add)
                        Q.ts(kT[:], fT[:], -1.0, ALU.mult, ['fT'], ['kT'], s2=1.0, op1=ALU.add)
                        Q.act(fT[:], fT[:], AF.Ln, ['fT'], ['fT'])
                    if d == 0:
                        Q.scan(eb[:], rm[:], fT[:], 0.0, ['rm', 'fT'], ['eb'])
                    else:
                        Q.scan(rev(eb[:]), rev(rm[:]), rev(fT[:]), 0.0, ['rm', 'fT'], ['eb'])
                    Q.act(fT[:], eb[:], AF.Exp, ['eb'], ['fT'], scale=-1.0)
                    Q.tt(fT[:], fT[:], kT[:], ALU.mult, ['fT', 'kT'], ['fT'])
                    Q.act(eb[:], eb[:], AF.Exp, ['eb'], ['eb'])
                    Q.stt(qT[:], qT[:], (1.0 if ML else 0.125), eb[:], ALU.mult, ALU.mult, ['qT', 'eb'], ['qT'])
                    ktil = fT
                    dS = Q.sb("dS", [64, dv, 72]); dec = Q.sb("dec", [64, dv, 72]); So = Q.sb("So", [64, dv, 72])
                    ebt = eb[:]
                    pst = ebt.ap[0][0]
                    if d == 0:
                        src = AP(ebt.tensor, ebt.offset + 31, [[pst, 64], [0, dv], [32, 72]])
                        Q.cp(dec[:], src, ['eb'], ['dec'])
                    else:
                        src = AP(ebt.tensor, ebt.offset + 32 * 7, [[pst, 64], [0, dv], [-32, 8]])
                        Q.cp(dec[:, :, 0:8], src, ['eb'], ['dec'])
                        src = AP(ebt.tensor, ebt.offset + 32 * 71, [[pst, 64], [0, dv], [-32, 64]])
                        Q.cp(dec[:, :, 8:72], src, ['eb'], ['dec'])
                    Q.memset(dec[:, :, 0:1], 0.0, ['dec'])
                    vm = [Q.sb("vm%d" % i, [128, 4, dv]) for i in range(2)]
                    pd = [Q.ps("pd%d" % i, [64, 4 * dv]) for i in range(2)]
                    dSt = dS[:]
                    dpst = dSt.ap[0][0]
                    for tt in range(NT):
                        i = tt % 2
                        Q.tt(vm[i][:], vaug[:, tt, h, None, :].to_broadcast([128, 4, dv]), chm[:, :, None].to_broadcast([128, 4, dv]), ALU.mult, ['vaug', 'chm'], ['vm%d' % i], eng=('pool' if i else 'dve'))
                        Q.mm(pd[i][:], khat[:, tt, h * 64:(h + 1) * 64], vm[i][:].rearrange("p c v -> p (c v)"), True, True, ['khat', 'vm%d' % i], ['pd%d' % i])
                        pp0 = pp_of_chunk(tt * 4, d)
                        pdt = pd[i][:]
                        src = AP(pdt.tensor, pdt.offset, [[pdt.ap[0][0], 64], [1, dv], [dv, 4]])
                        dst_ = AP(dSt.tensor, dSt.offset + pp0, [[dpst, 64], [72, dv], [1 if d == 0 else -1, 4]])
                        Q.cp(dst_, src, ['pd%d' % i], ['dS'], eng='act')
                    Q.scan(So[:].rearrange("p v c -> p (v c)"), dec[:].rearrange("p v c -> p (v c)"), dS[:].rearrange("p v c -> p (v c)"), 0.0, ['dec', 'dS'], ['So'])
                    pa = [Q.ps("pa%d" % i, [128, 128]) for i in range(2)]
                    po = [Q.ps("po%d" % i, [128, dv]) for i in range(2)]
                    pi = [Q.ps("pi%d" % i, [128, 4 * dv]) for i in range(2)]
                    asb = [Q.sb("asb%d" % i, [128, 128]) for i in range(2)]
                    acc = [Q.sb("acc%d" % i, [128, dv]) for i in range(2)]
                    dtmp = Q.sb("dtmp", [128, 1])
                    mcnt = [0]

                    def stage_a(tt):
                        i = tt % 2
                        tsl = slice(tt * 128, (tt + 1) * 128)
                        Q.mm(pa[i][:], ktil[:, tsl], qT[:, tsl], True, True, ['fT', 'qT'], ['pa%d' % i])
                        Q.tt(asb[i][:], pa[i][:], msk[:, d, :], ALU.mult, ['pa%d' % i, 'msk'], ['asb%d' % i])

                    def stage_b(tt):
                        i = tt % 2
                        tsl = slice(tt * 128, (tt + 1) * 128)
                        Q.mm(po[i][:], asb[i][:], vaug[:, tt, h, :], True, True, ['asb%d' % i, 'vaug'], ['po%d' % i])
                        Q.cp(acc[i][:], po[i][:], ['po%d' % i], ['acc%d' % i], eng='act')
                        pps = [pp_of_chunk(tt * 4 + cc, d) for cc in range(4)]
                        if 0 in pps:
                            for cc in range(4):
                                ppx = pps[cc]
                                if ppx == 0:
                                    continue
                                j = mcnt[0] % 2; mcnt[0] += 1
                                Q.mm(pi[j][:, 0:dv], qT[:, tsl], So[:, :, ppx - 1], True, True, ['qT', 'So'], ['pi%d' % j])
                                Q.stt(acc[i][:], pi[j][:, 0:dv], chm[:, cc:cc + 1], acc[i][:], ALU.mult, ALU.add, ['pi%d' % j, 'chm', 'acc%d' % i], ['acc%d' % i])
                        else:
                            j = mcnt[0] % 2; mcnt[0] += 1
                            Sot = So[:]
                            rhs4 = AP(Sot.tensor, Sot.offset + pps[0] - 1, [[Sot.ap[0][0], 64], [1 if d == 0 else -1, 4], [72, dv]])
                            Q.mm(pi[j][:], qT[:, tsl], rhs4, True, True, ['qT', 'So'], ['pi%d' % j])
                            for cc in range(4):
                                Q.stt(acc[i][:], pi[j][:, cc * dv:(cc + 1) * dv], chm[:, cc:cc + 1], acc[i][:], ALU.mult, ALU.add, ['pi%d' % j, 'chm', 'acc%d' % i], ['acc%d' % i])
                        dst = oacc[:, tt, h, :]
                        if ML:
                            den = acc[i][:, 64:65]
                            Q.ts(dtmp[:], den, -1.0, ALU.mult, ['acc%d' % i], ['dtmp'])
                            Q.tt(den, den, dtmp[:], ALU.max, ['acc%d' % i, 'dtmp'], ['acc%d' % i])
                            Q.ts(den, den, 1.0, ALU.max, ['acc%d' % i], ['acc%d' % i])
                            Q.recip(den, den, ['acc%d' % i], ['acc%d' % i])
                            if d == 0:
                                Q.ts(dst, acc[i][:, 0:64], den, ALU.mult, ['acc%d' % i], ['oacc'])
                            else:
                                Q.stt(dst, acc[i][:, 0:64], den, dst, ALU.mult, ALU.add, ['acc%d' % i, 'oacc'], ['oacc'])
                        else:
                            if d == 0:
                                Q.cp(dst, acc[i][:, 0:64], ['acc%d' % i], ['oacc'], eng='pool')
                            else:
                                Q.tt(dst, dst, acc[i][:, 0:64], ALU.add, ['acc%d' % i, 'oacc'], ['oacc'], eng='pool')

                    stage_a(0)
                    for tt in range(NT):
                        if tt + 1 < NT:
                            stage_a(tt + 1)
                        stage_b(tt)
        with Sub(P) as Q:
            g = Q.sb("g", [128, NT, 256]); sq = Q.sb("sq", [128, NT * 4, 64]); ssum = Q.sb("ssum", [128, NT * 4]); nwb = Q.sb("nwb", [128, 256])
            epsc = Q.sb("epsc", [128, 1])
            Q.memset(epsc[:], EPS, ['epsc'])
            Q.dma(g[:], ztile(ZT_D_O if ML else ZT_A_G), ['ZT%d' % s], ['g'])
            Q.dma(nwb[:], dr['ml_norm_w' if ML else 'hg_norm_w'][li].partition_broadcast(128), [], ['nwb'], q='sp')
            Q.act(g[:], g[:], AF.Sigmoid if ML else AF.Silu, ['g'], ['g'])
            of = oacc[:].rearrange("p a h v -> p (a h) v")
            Q.tt(sq[:], of, of, ALU.mult, ['oacc'], ['sq'])
            Q.S.op('dve', lambda hh: hh.tensor_reduce(out=ssum[:], in_=sq[:], axis=AX.X, op=ALU.add), reads=['sq'], writes=['ssum'])
            Q.act(ssum[:], ssum[:], AF.Sqrt, ['ssum', 'epsc'], ['ssum'], scale=1.0 / 64, bias=epsc[:])
            Q.recip(ssum[:], ssum[:], ['ssum'], ['ssum'])
            Q.tt(of, of, ssum[:, :, None].to_broadcast([128, NT * 4, 64]), ALU.mult, ['oacc', 'ssum'], ['oacc'])
            o3 = oacc[:].rearrange("p a h v -> p a (h v)")
            Q.tt(o3, o3, nwb[:, None, :].to_broadcast([128, NT, 256]), ALU.mult, ['oacc', 'nwb'], ['oacc'])
            Q.tt(o3, o3, g[:], ALU.mult, ['oacc', 'g'], ['oacc'])
            base = 512 if ML else 0
            Q.dma(dr['MIX'][s][:, base:base + 256].rearrange("(a p) c -> p a c", p=128), o3, ['oacc'], ['MIX%d' % s], q='sp')


def phase_attn(kb, C, li, s):
    dr = kb.dr
    ZT, ZF = dr['ZT'][s], dr['ZF'][s]
    lam_init = 0.8 - 0.6 * math.exp(-0.3 * li)
    scl = 32 ** -0.5
    with kb.phase() as P:
        KR = P.sb("KR", [128, 2, T]); QR = P.sb("QR", [128, 2, T]); V = P.sb("V", [128, NT, 4, 65])
        chm = P.sb("chm", [128, 4]); epsc = P.sb("epsc", [128, 1]); nwb = P.sb("nwb", [128, 256]); lamcol = P.sb("lamcol", [128, 1])
        negcb = P.sb("negcb", [128, 8]); oall = P.sb("oall", [128, NT, 256])
        P.memset(epsc[:], EPS, ['epsc'])
        P.dma(chm[:], dr['c_chm'], [], ['chm'], q='sp')
        P.dma(nwb[:], dr['da_norm_w'][li].partition_broadcast(128), [], ['nwb'], q='sp')
        P.ts(nwb[:], nwb[:], 1.0 - lam_init, ALU.mult, ['nwb'], ['nwb'])
        P.memset(V[:], 1.0, ['V'])
        vsrc = ZT[:, ZT_C_V:ZT_C_V + 256].rearrange("(a p) c -> p a c", p=128)
        for tt_ in range(NT):
            P.dma(V[:, tt_, :, 0:64], vsrc[:, tt_, :].rearrange("p (h v) -> p h v", h=4), ['ZT%d' % s], ['V'])
        with Sub(P) as Q:
            rc = Q.sb("rc", [128, NLAT]); rs = Q.sb("rs", [128, NLAT]); tmp = Q.sb("tmp", [128, NLAT])
            Q.dma(rc[:], dr['c_ropec'], [], ['rc'])
            Q.dma(rs[:], dr['c_ropes'], [], ['rs'])
            kraw = Q.sb("kraw", [128, T])
            for j in range(2):
                Q.dma(kraw[:], ZF[ZF_C_K + 128 * j:ZF_C_K + 128 * j + 128, :], ['ZF%d' % s], ['kraw'])
                Q.dma(tmp[:], ZF[ZF_C_KS + 128 * j:ZF_C_KS + 128 * j + 128, NCTX:T], ['ZF%d' % s], ['tmp'])
                Q.tt(tmp[:], tmp[:], rs[:], ALU.mult, ['tmp', 'rs'], ['tmp'], eng='pool')
                Q.cp(KR[:, j, 0:NCTX].bitcast(F32R), kraw[:, 0:NCTX], ['kraw'], ['KR'], eng='act')
                Q.tt(kraw[:, NCTX:T], kraw[:, NCTX:T], rc[:], ALU.mult, ['kraw', 'rc'], ['kraw'])
                Q.tt(KR[:, j, NCTX:T].bitcast(F32R), kraw[:, NCTX:T], tmp[:], ALU.add, ['kraw', 'tmp'], ['KR'])
                Q.dma(QR[:, j, :], ZF[ZF_C_Q + 128 * j:ZF_C_Q + 128 * j + 128, :], ['ZF%d' % s], ['QR'])
                Q.dma(tmp[:], ZF[ZF_C_QS + 128 * j:ZF_C_QS + 128 * j + 128, NCTX:T], ['ZF%d' % s], ['tmp'])
                Q.tt(tmp[:], tmp[:], rs[:], ALU.mult, ['tmp', 'rs'], ['tmp'], eng='pool')
                Q.tt(QR[:, j, NCTX:T], QR[:, j, NCTX:T], rc[:], ALU.mult, ['QR', 'rc'], ['QR'])
                Q.tt(QR[:, j, NCTX:T], QR[:, j, NCTX:T], tmp[:], ALU.add, ['QR', 'tmp'], ['QR'])
            l4 = Q.sb("l4", [1, 4, 32]); pr = Q.sb("pr", [1, 2, 32]); sm = Q.sb("sm", [1, 2]); lam1 = Q.sb("lam1", [1, 1])
            pl = Q.ps("pl", [128, 8])
            for i_, nm in enumerate(['da_lq1', 'da_lk1', 'da_lq2', 'da_lk2']):
                Q.dma(l4[:, i_, :], dr[nm][li:li + 1, :], [], ['l4'], q='sp')
            Q.tt(pr[:, 0, :], l4[:, 0, :], l4[:, 1, :], ALU.mult, ['l4'], ['pr'])
            Q.tt(pr[:, 1, :], l4[:, 2, :], l4[:, 3, :], ALU.mult, ['l4', 'pr'], ['pr'])
            Q.S.op('dve', lambda hh: hh.tensor_reduce(out=sm[:], in_=pr[:], axis=AX.X, op=ALU.add), reads=['pr'], writes=['sm'])
            Q.act(sm[:], sm[:], AF.Exp, ['sm'], ['sm'])
            Q.ts(lam1[:], sm[:, 0:1], sm[:, 1:2], ALU.subtract, ['sm'], ['lam1'], s2=lam_init, op1=ALU.add)
            Q.mm(pl[:, 0:1], C['ones'][0:1, 0:128], lam1[:], True, True, ['const', 'lam1'], ['pl'])
            Q.cp(lamcol[:], pl[:, 0:1], ['pl'], ['lamcol'])
            pn = [Q.ps("pn%d" % i, [4, 512]) for i in range(2)]
            nrm = Q.sb("nrm", [4, 2, 2, 5]); nmax = Q.sb("nmax", [4, 2, 2]); dg = Q.sb("dg", [4, 2, 4])
            n_ = 0
            for a_, (src, key) in enumerate([(QR, 'QR'), (KR, 'KR')]):
                for j in range(2):
                    Q.tt(tmp[:, 0:NLAT], src[:, j, 0:NLAT], src[:, j, 0:NLAT], ALU.mult, [key], ['tmp'], eng=('pool' if j else 'dve'))
                    Q.tt(rc[:, 0:NCTX], src[:, j, NLAT:T], src[:, j, NLAT:T], ALU.mult, [key], ['rc'], eng=('pool' if j else 'dve'))
                    for ch in range(5):
                        i = n_ % 2; n_ += 1
                        rhs_ = tmp[:, ch * 512:(ch + 1) * 512] if ch < 4 else rc[:, 0:NCTX]
                        wn = 512 if ch < 4 else NCTX
                        Q.mm(pn[i][:, 0:wn], chm[:, 0:4], rhs_, True, True, ['chm', 'tmp', 'rc'], ['pn%d' % i])
                        Q.S.op('dve', lambda hh, i=i, wn=wn, a_=a_, j=j, ch=ch: hh.reduce_max(out=nrm[:, a_, j, ch:ch + 1], in_=pn[i][:, 0:wn], axis=AX.X), reads=['pn%d' % i], writes=['nrm'])
            Q.S.op('dve', lambda hh: hh.reduce_max(out=nmax[:], in_=nrm[:], axis=AX.X), reads=['nrm'], writes=['nmax'])
            Q.tt(nmax[:, 0, :], nmax[:, 0, :], nmax[:, 1, :], ALU.mult, ['nmax'], ['nmax'])
            Q.act(nmax[:, 0, :], nmax[:, 0, :], AF.Sqrt, ['nmax'], ['nmax'])
            Q.ts(nmax[:, 0, :], nmax[:, 0, :], -scl, ALU.mult, ['nmax'], ['nmax'])
            for j in range(2):
                Q.ts(dg[:, j, :], C['idn'][0:4, 0:4], nmax[:, 0, j:j + 1], ALU.mult, ['const', 'nmax'], ['dg'])
            Q.mm(pl[:, 0:8], C['ones'][0:4, 0:128], dg[:].rearrange("p j c -> p (j c)"), True, True, ['const', 'dg'], ['pl'])
            Q.cp(negcb[:], pl[:, 0:8], ['pl'], ['negcb'])
        with Sub(P) as Q:
            ps = [Q.ps("aps%d" % i, [128, 512]) for i in range(4)]
            pavT = [Q.ps("pavT%d" % i, [65, 512]) for i in range(2)]
            ptn = Q.ps("ptn", [128, 4, 128])
            Vr = Q.sb("Vr", [128, NT, 4, 65])
            Q.cp(Vr[:, 0:9].bitcast(F32R), V[:, 0:9], ['V'], ['Vr'], eng='act')
            Q.cp(Vr[:, 9:NT].bitcast(F32R), V[:, 9:NT], ['V'], ['Vr'], eng='dve')
            PT = [Q.sb("PT%d" % i, [128, NT, 512]) for i in range(2)]
            qp = [Q.sb("qp%d" % i, [128, 512]) for i in range(2)]
            numT = [Q.sb("numT%d" % i, [65, 512]) for i in range(2)]
            num = [Q.sb("num%d" % i, [128, 4, 65]) for i in range(2)]
            rec = Q.sb("rec", [128, 4, 2]); t64 = Q.sb("t64", [128, 64])
            qgroups = [(NCTX + 512 * g, 512, NT) for g in range(4)] + ([(0, NCTX, 2)] if li < DEPTH - 1 else [])
            units = [(h, grp, m) for h in range(4) for grp in qgroups for m in range(2)]
            n1 = [0]

            def s_prep(u):
                h, (q0, N, nkt), m = units[u]
                j = h // 2
                cc = 2 * (h % 2) + m
                Q.ts(qp[u % 2][:, 0:N].bitcast(F32R), QR[:, j, q0:q0 + N], chm[:, cc:cc + 1], ALU.mult, ['QR', 'chm'], ['qp%d' % (u % 2)], eng='dve')

            def s_step(u, kt):
                h, (q0, N, nkt), m = units[u]
                j = h // 2
                col = j * 4 + 2 * (h % 2) + m
                i = n1[0] % 4; n1[0] += 1
                Q.mm(ps[i][:, 0:N], KR[:, j, kt * 128:(kt + 1) * 128], qp[u % 2][:, 0:N], True, True, ['KR', 'qp%d' % (u % 2)], ['aps%d' % i], r32=True)
                Q.act(PT[u % 2][:, kt, 0:N].bitcast(F32R), ps[i][:, 0:N], AF.Exp, ['aps%d' % i, 'negcb'], ['PT%d' % (u % 2)], bias=negcb[:, col:col + 1], scale=scl)

            def av_step(u, kt):
                h, (q0, N, nkt), m = units[u]
                Q.mm(pavT[u % 2][:, 0:N], Vr[:, kt, h, :], PT[u % 2][:, kt, 0:N], kt == 0, kt == nkt - 1, ['PT%d' % (u % 2), 'Vr'], ['pavT%d' % (u % 2)], r32=True)

            def av_finish(u):
                h, (q0, N, nkt), m = units[u]
                nqt = N // 128
                Q.cp(numT[u % 2][:, 0:N], pavT[u % 2][:, 0:N], ['pavT%d' % (u % 2)], ['numT%d' % (u % 2)], eng='act')
                for qt in range(nqt):
                    Q.tr(ptn[:, qt, 0:65], numT[u % 2][:, qt * 128:(qt + 1) * 128], C['idn'][0:65, 0:65], ['numT%d' % (u % 2)], ['ptn'])
                Q.cp(num[m][:, 0:nqt, :], ptn[:, 0:nqt, 0:65], ['ptn'], ['num%d' % m])
                if m == 1:
                    Q.cp(rec[:, 0:nqt, 0], num[0][:, 0:nqt, 64], ['num0'], ['rec'])
                    Q.cp(rec[:, 0:nqt, 1], num[1][:, 0:nqt, 64], ['num1', 'rec'], ['rec'])
                    Q.recip(rec[:, 0:nqt, :], rec[:, 0:nqt, :], ['rec'], ['rec'])
                    Q.ts(rec[:, 0:nqt, 1], rec[:, 0:nqt, 1], lamcol[:], ALU.mult, ['rec', 'lamcol'], ['rec'])
                    for qt in range(nqt):
                        tg = q0 // 128 + qt
                        Q.ts(t64[:], num[1][:, qt, 0:64], rec[:, qt, 1:2], ALU.mult, ['num1', 'rec'], ['t64'], eng='pool')
                        Q.stt(oall[:, tg, h * 64:(h + 1) * 64], num[0][:, qt, 0:64], rec[:, qt, 0:1], t64[:], ALU.mult, ALU.subtract, ['num0', 'rec', 't64'], ['oall'])

            s_prep(0)
            for kt in range(units[0][1][2]):
                s_step(0, kt)
            for u in range(len(units)):
                nkt_u = units[u][1][2]
                nxt = u + 1 if u + 1 < len(units) else None
                nkt_n = units[nxt][1][2] if nxt is not None else 0
                if nxt is not None:
                    s_prep(nxt)
                for kt in range(max(nkt_u, nkt_n)):
                    if kt < nkt_n:
                        s_step(nxt, kt)
                    if kt < nkt_u:
                        av_step(u, kt)
                av_finish(u)
        with Sub(P) as Q:
            t0_ = 0 if li < DEPTH - 1 else 2
            na = NT - t0_
            sq = Q.sb("sq", [128, NT * 4, 64]); ssum = Q.sb("ssum", [128, NT * 4])
            ov = oall[:, t0_:NT, :]
            of = ov.rearrange("p a (h v) -> p (a h) v", h=4)
            Q.tt(sq[:, 0:na * 4, :], of, of, ALU.mult, ['oall'], ['sq'])
            Q.S.op('dve', lambda hh: hh.tensor_reduce(out=ssum[:, 0:na * 4], in_=sq[:, 0:na * 4, :], axis=AX.X, op=ALU.add), reads=['sq'], writes=['ssum'])
            Q.act(ssum[:, 0:na * 4], ssum[:, 0:na * 4], AF.Sqrt, ['ssum', 'epsc'], ['ssum'], scale=1.0 / 64, bias=epsc[:])
            Q.recip(ssum[:, 0:na * 4], ssum[:, 0:na * 4], ['ssum'], ['ssum'])
            Q.tt(of, of, ssum[:, 0:na * 4, None].to_broadcast([128, na * 4, 64]), ALU.mult, ['oall', 'ssum'], ['oall'])
            Q.tt(ov, ov, nwb[:, None, :].to_broadcast([128, na, 256]), ALU.mult, ['oall', 'nwb'], ['oall'])
            Q.dma(dr['MIX'][s][t0_ * 128:T, 256:512].rearrange("(a p) c -> p a c", p=128), ov, ['oall'], ['MIX%d' % s], q='sp')


TWO_PI = 2.0 * math.pi
CW1 = 6.28125
CW2 = TWO_PI - CW1
PI_LO = 3.1415925


def sincos(Q, ang, F, sin_out, cos_out, rkeys, wkeys):
    ni = Q.sb("sc_ni", [128, F], I32); nf = Q.sb("sc_nf", [128, F]); r = Q.sb("sc_r", [128, F]); a2 = Q.sb("sc_a2", [128, F])
    for (shift, dst, wk) in [(0.0, sin_out, wkeys[0]), (math.pi / 2, cos_out, wkeys[1])]:
        Q.ts(a2[:], ang, shift, ALU.add, list(rkeys), ['sc_a2'])
        Q.ts(ni[:], a2[:], 1.0 / TWO_PI, ALU.mult, ['sc_a2'], ['sc_ni'])
        Q.cp(nf[:], ni[:], ['sc_ni'], ['sc_nf'])
        Q.stt(r[:], nf[:], -CW1, a2[:], ALU.mult, ALU.add, ['sc_nf', 'sc_a2'], ['sc_r'])
        Q.stt(r[:], nf[:], -CW2, r[:], ALU.mult, ALU.add, ['sc_nf', 'sc_r'], ['sc_r'])
        Q.ts(r[:], r[:], PI_LO, ALU.min, ['sc_r'], ['sc_r'], s2=-PI_LO, op1=ALU.max)
        Q.act(dst, r[:], AF.Sin, ['sc_r'], [wk])


def phase_s5(kb, C, li, s):
    dr = kb.dr
    ZF = dr['ZF'][s]
    LC = 256
    with kb.phase() as P:
        uT = P.sb("uT", [128, 2, T]); yT = P.sb("yT", [128, 2, T])
        WB = [P.sb("WB%d" % i, [128, 2, 1024]) for i in range(2)]
        WC = [P.sb("WC%d" % i, [128, 8, 256]) for i in range(2)]
        iotaL = P.sb("iotaL", [128, LC])
        cosL = P.sb("cosL", [128, 8, LC]); sinL = P.sb("sinL", [128, 8, LC]); Tr = P.sb("Tr", [128, 8, LC]); Ti = P.sb("Ti", [128, 8, LC])
        magbc = P.sb("magbc", [128, 8, LC])
        cL = P.sb("cL", [128, 8]); sL = P.sb("sL", [128, 8]); nsL = P.sb("nsL", [128, 8])
        for k in range(2):
            P.dma(uT[:, k, :], ZF[ZF_B_U + 128 * k:ZF_B_U + 128 * k + 128, :], ['ZF%d' % s], ['uT'])
        for i, nm in enumerate(['S5_WBre', 'S5_WBim']):
            P.dma(WB[i][:], dr[nm][li].rearrange("(k p) n -> p k n", p=128), [], ['WB'])
        for i, nm in enumerate(['S5_WCre', 'S5_WCim']):
            P.dma(WC[i][:], dr[nm][li].rearrange("(j p) c -> p j c", p=128), [], ['WC'])
        P.dma(iotaL[:], dr['c_iota256'], [], ['iotaL'], q='sp')
        for d in range(2):
            with Sub(P) as Q:
                lr = Q.sb("lr", [128, 8]); lim = Q.sb("lim", [128, 8]); dt = Q.sb("dt", [128, 8])
                for (tile_, nm, key) in [(lr, 's5_lam_re', 'lr'), (lim, 's5_lam_im', 'lim'), (dt, 'S5_LOGDT', 'dt')]:
                    Q.dma(tile_[:], dr[nm][li, d].rearrange("(j g) p -> (g p) j", g=2), [], [key], q='sp', allow_slow_non_contiguous=True)
                mag = Q.sb("mag", [128, 8]); th = Q.sb("th", [128, 8]); sn = Q.sb("sn", [128, 8]); cs = Q.sb("cs", [128, 8])
                Q.act(dt[:], dt[:], AF.Exp, ['dt'], ['dt'])
                Q.tt(mag[:], lr[:], dt[:], ALU.mult, ['lr', 'dt'], ['mag'])
                Q.act(mag[:], mag[:], AF.Exp, ['mag'], ['mag'])
                Q.tt(th[:], lim[:], dt[:], ALU.mult, ['lim', 'dt'], ['th'])
                with Sub(Q) as R:
                    sincos(R, th[:], 8, sn[:], cs[:], ['th'], ['sn', 'cs'])
                abr1 = Q.sb("abr1", [128, 8]); abi = Q.sb("abi", [128, 8]); den = Q.sb("den", [128, 8]); t1 = Q.sb("t1", [128, 8]); t2 = Q.sb("t2", [128, 8])
                cor = Q.sb("cor", [128, 8]); coi = Q.sb("coi", [128, 8]); ncor = Q.sb("ncor", [128, 8]); thL = Q.sb("thL", [128, 8])
                Q.tt(abr1[:], mag[:], cs[:], ALU.mult, ['mag', 'cs'], ['abr1'])
                Q.ts(abr1[:], abr1[:], -1.0, ALU.add, ['abr1'], ['abr1'])
                Q.tt(abi[:], mag[:], sn[:], ALU.mult, ['mag', 'sn'], ['abi'])
                Q.tt(den[:], lr[:], lr[:], ALU.mult, ['lr'], ['den'])
                Q.tt(t1[:], lim[:], lim[:], ALU.mult, ['lim'], ['t1'])
                Q.tt(den[:], den[:], t1[:], ALU.add, ['den', 't1'], ['den'])
                Q.recip(den[:], den[:], ['den'], ['den'])
                Q.tt(t1[:], abr1[:], lr[:], ALU.mult, ['abr1', 'lr'], ['t1'])
                Q.tt(t2[:], abi[:], lim[:], ALU.mult, ['abi', 'lim'], ['t2'])
                Q.tt(t1[:], t1[:], t2[:], ALU.add, ['t1', 't2'], ['t1'])
                Q.tt(cor[:], t1[:], den[:], ALU.mult, ['t1', 'den'], ['cor'])
                Q.tt(t1[:], abi[:], lr[:], ALU.mult, ['abi', 'lr'], ['t1'])
                Q.tt(t2[:], abr1[:], lim[:], ALU.mult, ['abr1', 'lim'], ['t2'])
                Q.tt(t1[:], t1[:], t2[:], ALU.subtract, ['t1', 't2'], ['t1'])
                Q.tt(coi[:], t1[:], den[:], ALU.mult, ['t1', 'den'], ['coi'])
                Q.ts(ncor[:], cor[:], -1.0, ALU.mult, ['cor'], ['ncor'])
                Q.ts(thL[:], th[:], float(LC), ALU.mult, ['th'], ['thL'])
                with Sub(Q) as R:
                    sincos(R, thL[:], 8, sL[:], cL[:], ['thL'], ['sL', 'cL'])
                Q.ts(nsL[:], sL[:], -1.0, ALU.mult, ['sL'], ['nsL'])
                with Sub(Q) as R:
                    angL = R.sb("angL", [128, 8, LC])
                    for j in range(8):
                        R.ts(angL[:, j, :], iotaL[:], th[:, j:j + 1], ALU.mult, ['iotaL', 'th'], ['angL'])
                    sincos(R, angL[:].rearrange("p j l -> p (j l)"), 8 * LC, sinL[:].rearrange("p j l -> p (j l)"), cosL[:].rearrange("p j l -> p (j l)"), ['angL'], ['sinL', 'cosL'])
                for j in range(8):
                    Q.ts(Tr[:, j, :], cosL[:, j, :], cor[:, j:j + 1], ALU.mult, ['cosL', 'cor'], ['Tr'])
                    Q.stt(Tr[:, j, :], sinL[:, j, :], coi[:, j:j + 1], Tr[:, j, :], ALU.mult, ALU.add, ['sinL', 'coi', 'Tr'], ['Tr'])
                    Q.ts(Ti[:, j, :], cosL[:, j, :], coi[:, j:j + 1], ALU.mult, ['cosL', 'coi'], ['Ti'])
                    Q.stt(Ti[:, j, :], sinL[:, j, :], ncor[:, j:j + 1], Ti[:, j, :], ALU.mult, ALU.add, ['sinL', 'ncor', 'Ti'], ['Ti'])
                Q.cp(magbc[:], mag[:, :, None].to_broadcast([128, 8, LC]), ['mag'], ['magbc'])
            with Sub(P) as Q:
                xin_r = Q.sb("xin_r", [128, 8]); xin_i = Q.sb("xin_i", [128, 8])
                Q.memset(xin_r[:], 0.0, ['xin_r']); Q.memset(xin_i[:], 0.0, ['xin_i'])
                NF = 3
                pbr = [Q.ps("pbr%d" % i, [128, LC]) for i in range(2)]; pbi = [Q.ps("pbi%d" % i, [128, LC]) for i in range(2)]
                py = [Q.ps("py%d" % i, [128, LC]) for i in range(2)]
                W = {}
                for nm in ['bur', 'bui', 'm1', 'm2', 'm3', 'm4']:
                    W[nm] = [Q.sb("%s%d" % (nm, i), [128, LC]) for i in range(2)]
                for nm in ['br', 'bi']:
                    W[nm] = [Q.sb("%s%d" % (nm, i), [128, LC]) for i in range(NF)]
                for nm in ['xr', 'xi', 'o1', 'o2', 'o3', 'o4', 'xro', 'xio']:
                    W[nm] = [Q.sb("%s%d" % (nm, i), [128, LC]) for i in range(2)]
                tsm = Q.sb("tsm", [128, 2])
                chunks = list(range(9)) if d == 0 else [0] + list(range(8, 0, -1))
                fx = (lambda ap: ap) if d == 0 else rev
                iters = [(ci, ct, jj) for ci in chunks for ct in range(2) for jj in range(4)]

                def front(n):
                    ci, ct, jj = iters[n]
                    j = ct * 4 + jj
                    tsl = slice(ci * LC, (ci + 1) * LC)
                    i = n % 2; f = n % NF
                    k = lambda nm: '%s%d' % (nm, i)
                    Q.mm(pbr[i][:], WB[0][:, ct, j * 128:(j + 1) * 128], uT[:, ct, tsl], True, True, ['WB', 'uT'], [k('pbr')])
                    Q.mm(pbi[i][:], WB[1][:, ct, j * 128:(j + 1) * 128], uT[:, ct, tsl], True, True, ['WB', 'uT'], [k('pbi')])
                    Q.cp(W['bur'][i][:], pbr[i][:], [k('pbr')], [k('bur')], eng='act')
                    Q.cp(W['bui'][i][:], pbi[i][:], [k('pbi')], [k('bui')], eng='act')
                    trj, tij = fx(Tr[:, j, :]), fx(Ti[:, j, :])
                    Q.tt(W['m1'][i][:], W['bur'][i][:], trj, ALU.mult, [k('bur'), 'Tr'], [k('m1')], eng='pool')
                    Q.tt(W['m2'][i][:], W['bui'][i][:], tij, ALU.mult, [k('bui'), 'Ti'], [k('m2')], eng='pool')
                    Q.tt(W['br'][f][:], W['m1'][i][:], W['m2'][i][:], ALU.subtract, [k('m1'), k('m2')], ['br%d' % f], eng='pool')
                    Q.tt(W['m3'][i][:], W['bui'][i][:], trj, ALU.mult, [k('bui'), 'Tr'], [k('m3')], eng='pool')
                    Q.tt(W['m4'][i][:], W['bur'][i][:], tij, ALU.mult, [k('bur'), 'Ti'], [k('m4')], eng='pool')
                    Q.tt(W['bi'][f][:], W['m3'][i][:], W['m4'][i][:], ALU.add, [k('m3'), k('m4')], ['bi%d' % f], eng='pool')

                def back(n):
                    ci, ct, jj = iters[n]
                    j = ct * 4 + jj
                    tsl = slice(ci * LC, (ci + 1) * LC)
                    i = n % 2; f = n % NF
                    pyi = (n // 4) % 2
                    k = lambda nm: '%s%d' % (nm, i)
                    Q.scan(fx(W['xr'][i][:]), fx(magbc[:, j, :]), fx(W['br'][f][:]), xin_r[:, j:j + 1], ['magbc', 'br%d' % f, 'xin_r'], [k('xr')])
                    Q.scan(fx(W['xi'][i][:]), fx(magbc[:, j, :]), fx(W['bi'][f][:]), xin_i[:, j:j + 1], ['magbc', 'bi%d' % f, 'xin_i'], [k('xi')])
                    last = (LC - 1) if d == 0 else 0
                    xrl, xil = W['xr'][i][:, last:last + 1], W['xi'][i][:, last:last + 1]
                    Q.ts(tsm[:, 0:1], xrl, cL[:, j:j + 1], ALU.mult, [k('xr'), 'cL'], ['tsm'])
                    Q.stt(xin_r[:, j:j + 1], xil, nsL[:, j:j + 1], tsm[:, 0:1], ALU.mult, ALU.add, [k('xi'), 'nsL', 'tsm'], ['xin_r'])
                    Q.ts(tsm[:, 1:2], xrl, sL[:, j:j + 1], ALU.mult, [k('xr'), 'sL'], ['tsm'])
                    Q.stt(xin_i[:, j:j + 1], xil, cL[:, j:j + 1], tsm[:, 1:2], ALU.mult, ALU.add, [k('xi'), 'cL', 'tsm'], ['xin_i'])
                    cj, sj = fx(cosL[:, j, :]), fx(sinL[:, j, :])
                    Q.tt(W['o1'][i][:], W['xr'][i][:], cj, ALU.mult, [k('xr'), 'cosL'], [k('o1')])
                    Q.tt(W['o2'][i][:], W['xi'][i][:], sj, ALU.mult, [k('xi'), 'sinL'], [k('o2')])
                    Q.tt(W['xro'][i][:], W['o1'][i][:], W['o2'][i][:], ALU.subtract, [k('o1'), k('o2')], [k('xro')])
                    Q.tt(W['o3'][i][:], W['xr'][i][:], sj, ALU.mult, [k('xr'), 'sinL'], [k('o3')])
                    Q.tt(W['o4'][i][:], W['xi'][i][:], cj, ALU.mult, [k('xi'), 'cosL'], [k('o4')])
                    Q.stt(W['xio'][i][:], W['o3'][i][:], -1.0, W['o4'][i][:], ALU.mult, ALU.subtract, [k('o3'), k('o4')], [k('xio')])
                    Q.mm(py[pyi][:], WC[0][:, j, ct * 128:(ct + 1) * 128], W['xro'][i][:], jj == 0, False, ['WC', k('xro')], ['py%d' % pyi])
                    Q.mm(py[pyi][:], WC[1][:, j, ct * 128:(ct + 1) * 128], W['xio'][i][:], False, jj == 3, ['WC', k('xio')], ['py%d' % pyi])
                    if jj == 3:
                        if d == 0:
                            Q.cp(yT[:, ct, tsl], py[pyi][:], ['py%d' % pyi], ['yT'], eng='act')
                        else:
                            Q.tt(yT[:, ct, tsl], yT[:, ct, tsl], py[pyi][:], ALU.add, ['py%d' % pyi, 'yT'], ['yT'])

                front(0)
                for n in range(len(iters)):
                    if n + 1 < len(iters):
                        front(n + 1)
                    back(n)
        with Sub(P) as Q:
            dsk = Q.sb("dsk", [128, 2]); gb = Q.sb("gb", [128, 2]); gw = Q.sb("gw", [128, 2, 256])
            Q.dma(dsk[:], dr['s5_d'][li].rearrange("(k p) -> p k", p=128), [], ['dsk'], q='sp', allow_slow_non_contiguous=True)
            Q.dma(gb[:], dr['s5_glu_b'][li].rearrange("(k p) -> p k", p=128), [], ['gb'], q='sp', allow_slow_non_contiguous=True)
            Q.dma(gw[:], dr['s5_glu_w'][li].rearrange("(k p) c -> p k c", p=128), [], ['gw'], q='sp')
            tq = Q.sb("tq", [128, 2, T])
            for ct in range(2):
                Q.stt(yT[:, ct, :], uT[:, ct, :], dsk[:, ct:ct + 1], yT[:, ct, :], ALU.mult, ALU.add, ['uT', 'dsk', 'yT'], ['yT'])
            Q.tt(tq[:], yT[:], yT[:], ALU.mult, ['yT'], ['tq'])
            Q.ts(tq[:], tq[:], 0.044715, ALU.mult, ['tq'], ['tq'], s2=1.0, op1=ALU.add)
            Q.tt(tq[:], tq[:], yT[:], ALU.mult, ['tq', 'yT'], ['tq'])
            Q.act(tq[:], tq[:], AF.Tanh, ['tq'], ['tq'], scale=0.7978845608028654)
            Q.ts(tq[:], tq[:], 1.0, ALU.add, ['tq'], ['tq'], s2=0.5, op1=ALU.mult)
            Q.tt(yT[:], yT[:], tq[:], ALU.mult, ['yT', 'tq'], ['yT'])
            pz = [Q.ps("pz%d" % i, [128, 512]) for i in range(2)]
            n = 0
            for m_ in range(2):
                for t0 in range(0, T, 512):
                    tn = min(512, T - t0)
                    i = n % 2; n += 1
                    for kc in range(2):
                        Q.mm(pz[i][:, 0:tn], gw[:, kc, m_ * 128:(m_ + 1) * 128], yT[:, kc, t0:t0 + tn], kc == 0, kc == 1, ['gw', 'yT'], ['pz%d' % i])
                    Q.act(tq[:, m_, t0:t0 + tn], pz[i][:, 0:tn], AF.Sigmoid, ['pz%d' % i, 'gb'], ['tq'], bias=gb[:, m_:m_ + 1])
            Q.tt(tq[:], tq[:], yT[:], ALU.mult, ['tq', 'yT'], ['tq'])
            for m_ in range(2):
                Q.dma(dr['S5T'][s][m_ * 128:(m_ + 1) * 128, :], tq[:, m_, :], ['tq'], ['S5T%d' % s])


NE = 16
CAPL = 256
CAPC = 32


def phase_moe_route(kb, C, li, s):
    dr = kb.dr
    last = (li == DEPTH - 1)
    with kb.phase() as P:
        P._junk = P.sb("junk", [128, D]); P._ss = P.sb("ss", [128, 1]); P._eps = P.sb("eps", [128, 1])
        P.memset(P._eps[:], EPS, ['eps'])
        modbc = P.sb("modbc", [128, 2, 2, D])
        for si, st in enumerate([2, s]):
            P.dma(modbc[:, si, 0, :], dr['MODV'][st, 3 * D:4 * D].partition_broadcast(128), ['MODV'], ['modbc'], q='sp')
            P.dma(modbc[:, si, 1, :], dr['MODV'][st, 4 * D:5 * D].partition_broadcast(128), ['MODV'], ['modbc'], q='sp')
        rw = P.sb("rw", [128, 8, NE])
        P.dma(rw[:], dr['router_w'][li].rearrange("(k p) e -> p k e", p=128), [], ['rw'], q='sp')
        affTM = P.sb("affTM", [128, NT, NE]); affT = P.sb("affT", [NE, T]); posT = P.sb("posT", [NE, T]); posTM = P.sb("posTM", [128, NT, NE])
        rhs2 = P.sb("rhs2", [128, NT, NE, 2]); tokc = P.sb("tokc", [128, NT]); iotaJ = P.sb("iotaJ", [128, 256])
        P.dma(tokc[:], dr['c_tok'], [], ['tokc'], q='sp')
        P.dma(iotaJ[:], dr['c_iota256'], [], ['iotaJ'], q='sp')
        if s:
            P.ts(tokc[:], tokc[:], float(s * T), ALU.add, ['tokc'], ['tokc'])
        tiles = list(range(2 if last else 0, NT))
        with Sub(P) as Q:
            hts = [Q.sb("ht%d" % i, [128, D]) for i in range(2)]
            xms = [Q.sb("xm%d" % i, [128, D]) for i in range(2)]
            xmT = [Q.sb("xmT%d" % i, [128, 8, 128]) for i in range(2)]
            tps = [Q.ps("tp%d" % i, [128, 1024]) for i in range(2)]
            pl = [Q.ps("pl%d" % i, [128, NE]) for i in range(2)]
            pT = [Q.ps("pT%d" % i, [NE, 128]) for i in range(2)]
            mx = Q.sb("mx", [128, 1]); sm = Q.sb("sm", [128, 1]); e16 = Q.sb("e16", [128, NE])
            for tt in tiles:
                i = tt % 2
                si = 0 if tt < 2 else 1
                tsl = slice(tt * 128, (tt + 1) * 128)
                Q.dma(hts[i][:], dr['H'][s, tsl, :], ['H%d' % s], ['%dh' % i])
                norm_mod_tile(Q, C, hts[i][:], xms[i][:], modbc[:, si, 1, :], modbc[:, si, 0, :], '%d' % i)
                Q.dma(dr['XM2'][s * T + tt * 128:s * T + (tt + 1) * 128, :], xms[i][:], ['%dxm' % i], ['XM2'])
                for kc in range(8):
                    Q.tr(tps[i][:, kc * 128:(kc + 1) * 128], xms[i][:, kc * 128:(kc + 1) * 128], C['idn'][:], ['%dxm' % i], ['tp%d' % i])
                Q.cp(xmT[i][:], tps[i][:].rearrange("p (k t) -> p k t", k=8), ['tp%d' % i], ['xmT%d' % i], eng='act')
                for kc in range(8):
                    Q.mm(pl[i][:], xmT[i][:, kc, :], rw[:, kc, :], kc == 0, kc == 7, ['xmT%d' % i, 'rw'], ['pl%d' % i])
                Q.S.op('dve', lambda hh, i=i: hh.reduce_max(out=mx[:], in_=pl[i][:], axis=AX.X), reads=['pl%d' % i], writes=['mx'])
                Q.ts(mx[:], mx[:], -1.0, ALU.mult, ['mx'], ['mx'])
                Q.memset(sm[:], 0.0, ['sm'])
                Q.act(e16[:], pl[i][:], AF.Exp, ['pl%d' % i, 'mx', 'sm'], ['e16', 'sm'], bias=mx[:], accum=sm[:])
                Q.recip(sm[:], sm[:], ['sm'], ['sm'])
                Q.ts(affTM[:, tt, :], e16[:], sm[:], ALU.mult, ['e16', 'sm'], ['affTM'])
                Q.tr(pT[i][:], affTM[:, tt, :], C['idn'][:], ['affTM'], ['pT%d' % i])
                Q.cp(affT[:, tsl], pT[i][:], ['pT%d' % i], ['affT'], eng='act')
        sets = [(NCTX, T, CAPL)] + ([] if last else [(0, NCTX, CAPC)])
        with Sub(P) as Q:
            work = Q.sb("work", [NE, NLAT]); m8 = Q.sb("m8", [NE, 8]); thr = Q.sb("thr", [NE, 1]); onesr = Q.sb("onesr", [NE, NLAT]); msk = Q.sb("mskr", [NE, NLAT])
            Q.memset(onesr[:], 1.0, ['onesr'])
            for (t0, t1, cap) in sets:
                n = t1 - t0
                Q.cp(work[:, 0:n], affT[:, t0:t1], ['affT'], ['work'])
                for r_ in range(cap // 8):
                    Q.S.op('dve', lambda hh, n=n: hh.max(out=m8[:], in_=work[:, 0:n]), reads=['work'], writes=['m8'])
                    if r_ < cap // 8 - 1:
                        Q.S.op('dve', lambda hh, n=n: hh.match_replace(out=work[:, 0:n], in_to_replace=m8[:], in_values=work[:, 0:n], imm_value=-1.0), reads=['work', 'm8'], writes=['work'])
                Q.cp(thr[:], m8[:, 7:8], ['m8'], ['thr'])
                Q.ts(msk[:, 0:n], affT[:, t0:t1], thr[:], ALU.is_ge, ['affT', 'thr'], ['mskr'])
                Q.scan(posT[:, t0:t1], onesr[:, 0:n], msk[:, 0:n], 0.0, ['onesr', 'mskr'], ['posT'])
                Q.tt(posT[:, t0:t1], posT[:, t0:t1], msk[:, 0:n], ALU.mult, ['posT', 'mskr'], ['posT'])
                Q.ts(posT[:, t0:t1], posT[:, t0:t1], -1.0, ALU.add, ['posT'], ['posT'])
        with Sub(P) as Q:
            pq = [Q.ps("pq%d" % i, [128, NE]) for i in range(2)]
            for tt in tiles:
                i = tt % 2
                Q.tr(pq[i][:], posT[:, tt * 128:(tt + 1) * 128], C['idn'][0:NE, 0:NE], ['posT'], ['pq%d' % i])
                Q.cp(posTM[:, tt, :], pq[i][:], ['pq%d' % i], ['posTM'], eng=('act' if i else 'dve'))
            if 'DBG_POS' in kb.debug and s == 0:
                Q.dma(dr['DBG_POS'], posT[:], ['posT'], ['DBG_POS'], q='sp')
                Q.dma(dr['DBG_AFF'], affT[:], ['affT'], ['DBG_AFF'], q='sp')
                Q.dma(dr['DBG_PTM'], posTM[:], ['posTM'], ['DBG_PTM'], q='sp')
            Q.cp(rhs2[:, :, :, 0], tokc[:, :, None].to_broadcast([128, NT, NE]), ['tokc'], ['rhs2'])
            Q.cp(rhs2[:, :, :, 1], affTM[:], ['affTM'], ['rhs2'], eng='pool')
            Pall = [Q.sb("Pall%d" % i, [128, NE, 256]) for i in range(2)]
            pacc = Q.ps("pacc", [128, 2 * NE, 16]); paccc = Q.ps("paccc", [CAPC, NE, 16])
            idxf = Q.sb("idxf", [128, 2 * NE, 2]); idxi = Q.sb("idxi", [128, 2 * NE], I32); gat = Q.sb("gat", [128, 2 * NE])
            for tt in range(2, NT):
                i = tt % 2
                Q.tt(Pall[i][:], iotaJ[:, None, :].to_broadcast([128, NE, 256]), posTM[:, tt, :, None].to_broadcast([128, NE, 256]), ALU.is_equal, ['iotaJ', 'posTM'], ['Pall%d' % i])
                for e in range(NE):
                    for jt in range(2):
                        Q.mm(pacc[:, e * 2 + jt, 0:2], Pall[i][:, e, jt * 128:(jt + 1) * 128], rhs2[:, tt, e, :], (tt == 2 and e == 0 and jt == 0), tt == NT - 1, ['Pall%d' % i, 'rhs2'], ['pacc'], skip=True)
            Q.cp(idxf[:], pacc[:, :, 0:2], ['pacc'], ['idxf'])
            Q.cp(idxi[:], idxf[:, :, 0], ['idxf'], ['idxi'])
            Q.cp(gat[:], idxf[:, :, 1], ['idxf'], ['gat'], eng='pool')
            Q.dma(dr['IDXL'][s], idxi[:], ['idxi'], ['IDXL'], q='sp')
            Q.dma(dr['GATEL'][s], gat[:], ['gat'], ['GATEL'], q='sp')
            if not last:
                Pc = [Q.sb("Pc%d" % i, [128, NE, CAPC]) for i in range(2)]
                idxfc = Q.sb("idxfc", [CAPC, NE, 2]); idxic = Q.sb("idxic", [CAPC, NE], I32); gatc = Q.sb("gatc", [CAPC, NE])
                for tt in range(2):
                    Q.tt(Pc[tt][:], iotaJ[:, None, 0:CAPC].to_broadcast([128, NE, CAPC]), posTM[:, tt, :, None].to_broadcast([128, NE, CAPC]), ALU.is_equal, ['iotaJ', 'posTM'], ['Pc%d' % tt])
                    for e in range(NE):
                        Q.mm(paccc[:, e, 0:2], Pc[tt][:, e, :], rhs2[:, tt, e, :], (tt == 0 and e == 0), tt == 1, ['Pc%d' % tt, 'rhs2'], ['paccc'], skip=True)
                Q.cp(idxfc[:], paccc[:, :, 0:2], ['paccc'], ['idxfc'])
                Q.cp(idxic[:], idxfc[:, :, 0], ['idxfc'], ['idxic'])
                Q.cp(gatc[:], idxfc[:, :, 1], ['idxfc'], ['gatc'], eng='pool')
                Q.dma(dr['IDXC'][s * CAPC:(s + 1) * CAPC, :], idxic[:], ['idxic'], ['IDXC'], q='sp')
                Q.dma(dr['GATEC'][s * CAPC:(s + 1) * CAPC, :], gatc[:], ['gatc'], ['GATEC'], q='sp')


def phase_moe_experts(kb, C, li):
    dr = kb.dr
    last = (li == DEPTH - 1)
    NJ = 512 if last else 576
    with kb.phase() as P:
        with Sub(P) as Q:
            zt = Q.sb("zt", [128, 4, D])
            Q.memset(zt[:], 0.0, ['zt'])
            for r0 in range(0, 2 * T, 512):
                Q.dma(dr['MOE'][r0:r0 + 512, :].rearrange("(a p) d -> p a d", p=128), zt[:], ['zt'], ['MOE'])
        idxL = P.sb("idxL", [128, 2, 2 * NE], I32); gatL = P.sb("gatL", [128, 2, 2 * NE])
        idxC = P.sb("idxC", [2 * CAPC, NE], I32); gatC = P.sb("gatC", [2 * CAPC, NE])
        for s in range(2):
            P.dma(idxL[:, s, :], dr['IDXL'][s], ['IDXL'], ['idxL'], q='sp')
            P.dma(gatL[:, s, :], dr['GATEL'][s], ['GATEL'], ['gatL'], q='sp')
        if not last:
            P.dma(idxC[:], dr['IDXC'], ['IDXC'], ['idxC'], q='sp')
            P.dma(gatC[:], dr['GATEC'], ['GATEC'], ['gatC'], q='sp')
        xs = P.sb("xs", [128, 4, D]); xsc = P.sb("xsc", [2 * CAPC, D])
        xsT = P.sb("xsT", [128, 8, 576]); gT = P.sb("gT", [128, 16, 576])
        w13r = [P.sb("w%dr" % a, [128, 8, 256]) for a in range(2)]
        w13 = [[P.sb("w%d_%d" % (a, i), [128, 8, 256]) for i in range(2)] for a in range(2)]
        w2r = P.sb("w2r", [128, 16, 256]); w2q = P.sb("w2q", [128, 16, 256])
        ysb = P.sb("ysb", [128, 5, D]); sg = P.sb("sg", [128, 576])
        ptr = [P.ps("ptr%d" % i, [128, 512]) for i in range(2)]
        ph = [P.ps("ph%d" % i, [128, 1024]) for i in range(2)]
        pye = P.ps("pye", [128, 1024])
        nw = 0
        ntr = 0
        for e in range(NE):
            for s in range(2):
                for jt in range(2):
                    col = e * 2 + jt
                    P.S.dma('pool', None, None, reads=['XM2', 'idxL'], writes=['xs'],
                            indirect=(lambda hh, s=s, jt=jt, col=col: hh.indirect_dma_start(
                                out=xs[:, s * 2 + jt, :], out_offset=None, in_=dr['XM2'][:, :],
                                in_offset=bass.IndirectOffsetOnAxis(ap=idxL[:, s, col:col + 1], axis=0))))
            if not last:
                P.S.dma('pool', None, None, reads=['XM2', 'idxC'], writes=['xsc'],
                        indirect=(lambda hh, e=e: hh.indirect_dma_start(
                            out=xsc[:, :], out_offset=None, in_=dr['XM2'][:, :],
                            in_offset=bass.IndirectOffsetOnAxis(ap=idxC[:, e:e + 1], axis=0))))
            for a in range(4):
                for kh in range(2):
                    i = ntr % 2; ntr += 1
                    for k4 in range(4):
                        kc = kh * 4 + k4
                        P.tr(ptr[i][:, k4 * 128:(k4 + 1) * 128], xs[:, a, kc * 128:(kc + 1) * 128], C['idn'][:], ['xs'], ['ptr%d' % i])
                    P.cp(xsT[:, kh * 4:kh * 4 + 4, a * 128:(a + 1) * 128].bitcast(F32R), ptr[i][:].rearrange("p (k t) -> p k t", k=4), ['ptr%d' % i], ['xsT'], eng=('act' if i else 'dve'))
            if not last:
                i = ntr % 2; ntr += 1
                for kc in range(8):
                    P.tr(ptr[i][:, kc * 64:(kc + 1) * 64], xsc[:, kc * 128:(kc + 1) * 128], C['idn'][0:64, 0:64], ['xsc'], ['ptr%d' % i])
                P.cp(xsT[:, :, 512:576].bitcast(F32R), ptr[i][:].rearrange("p (k t) -> p k t", k=8), ['ptr%d' % i], ['xsT'])
            for fc in range(8):
                wi = nw % 2; nw += 1
                for a, nm in enumerate(['exp_w1', 'exp_w3']):
                    P.dma(w13r[a][:], dr[nm][li, e][:, fc * 256:(fc + 1) * 256].rearrange("(k p) f -> p k f", p=128), [], ['w%dr' % a], q=('sp' if a == 0 else 'act'))
                    P.cp(w13[a][wi][:].bitcast(F32R), w13r[a][:], ['w%dr' % a], ['w%d_%d' % (a, wi)], eng=('act' if a == 0 else 'dve'))
                for hf in range(2):
                    f16 = fc * 2 + hf
                    for a in range(2):
                        for kc in range(8):
                            P.mm(ph[a][:, 0:512], w13[a][wi][:, kc, hf * 128:(hf + 1) * 128], xsT[:, kc, 0:512], kc == 0, kc == 7, ['w%d_%d' % (a, wi), 'xsT'], ['ph%d' % a], r32=True)
                        if not last:
                            for kc in range(8):
                                P.mm(ph[a][:, 512:576], w13[a][wi][:, kc, hf * 128:(hf + 1) * 128], xsT[:, kc, 512:576], kc == 0, kc == 7, ['w%d_%d' % (a, wi), 'xsT'], ['ph%d' % a], r32=True)
                    P.act(sg[:, 0:NJ], ph[0][:, 0:NJ], AF.Silu, ['ph0'], ['sg'])
                    P.tt(gT[:, f16, 0:NJ].bitcast(F32R), sg[:, 0:NJ], ph[1][:, 0:NJ], ALU.mult, ['sg', 'ph1'], ['gT'])
            njt = 4 if last else 5
            for qq in range(4):
                P.dma(w2r[:], dr['exp_w2'][li, e][:, qq * 256:(qq + 1) * 256].rearrange("(k p) c -> p k c", p=128), [], ['w2r'], q='pool')
                P.cp(w2q[:, 0:8, :].bitcast(F32R), w2r[:, 0:8, :], ['w2r'], ['w2q'], eng='act')
                P.cp(w2q[:, 8:16, :].bitcast(F32R), w2r[:, 8:16, :], ['w2r'], ['w2q'], eng='dve')
                for jt in range(njt):
                    jn = 128 if jt < 4 else 2 * CAPC
                    b0 = (jt % 2) * 512
                    for f16 in range(16):
                        P.mm(pye[0:jn, b0:b0 + 256], gT[:, f16, jt * 128:jt * 128 + jn], w2q[:, f16, :], f16 == 0, f16 == 15, ['gT', 'w2q'], ['pye%d' % (jt % 2)], r32=True)
                    if jt < 4:
                        gcol = gatL[:, jt // 2, e * 2 + jt % 2:e * 2 + jt % 2 + 1]
                    else:
                        gcol = gatC[:, e:e + 1]
                    P.ts(ysb[0:jn, jt, qq * 256:(qq + 1) * 256], pye[0:jn, b0:b0 + 256], gcol, ALU.mult, ['pye%d' % (jt % 2), 'gatL', 'gatC'], ['ysb'], eng=('pool' if False else 'dve'))
            for jt in range(njt):
                if jt < 4:
                    iap = idxL[:, jt // 2, e * 2 + jt % 2:e * 2 + jt % 2 + 1]
                    src = ysb[:, jt, :]
                else:
                    iap = idxC[:, e:e + 1]
                    src = ysb[0:2 * CAPC, jt, :]
                P.S.dma('pool', None, None, reads=['ysb', 'idxL', 'idxC'], writes=['MOE'],
                        indirect=(lambda hh, iap=iap, src=src: hh.indirect_dma_start(
                            out=dr['MOE'][:, :], out_offset=bass.IndirectOffsetOnAxis(ap=iap, axis=0), in_=src, in_offset=None, compute_op=ALU.add)))


def phase_moe_residual(kb, C, li, s):
    dr = kb.dr
    last = (li == DEPTH - 1)
    with kb.phase() as P:
        gbc = P.sb("gbc", [128, 2, D])
        P.dma(gbc[:, 0, :], dr['MODV'][2, 5 * D:6 * D].partition_broadcast(128), ['MODV'], ['gbc'], q='sp')
        P.dma(gbc[:, 1, :], dr['MODV'][s, 5 * D:6 * D].partition_broadcast(128), ['MODV'], ['gbc'], q='sp')
        ht = [P.sb("rh%d" % i, [128, D]) for i in range(2)]
        mt = [P.sb("rm%d" % i, [128, D]) for i in range(2)]
        for tt in range(2 if last else 0, NT):
            i = tt % 2
            si = 0 if tt < 2 else 1
            tsl = slice(tt * 128, (tt + 1) * 128)
            P.dma(ht[i][:], dr['H'][s, tsl, :], ['H%d' % s], ['rh%d' % i])
            P.dma(mt[i][:], dr['MOE'][s * T + tt * 128:s * T + (tt + 1) * 128, :], ['MOE'], ['rm%d' % i])
            P.tt(mt[i][:], mt[i][:], gbc[:, si, :], ALU.mult, ['rm%d' % i, 'gbc'], ['rm%d' % i], eng=('pool' if i else 'dve'))
            P.tt(ht[i][:], ht[i][:], mt[i][:], ALU.add, ['rh%d' % i, 'rm%d' % i], ['rh%d' % i], eng=('pool' if i else 'dve'))
            P.dma(dr['H'][s, tsl, :], ht[i][:], ['rh%d' % i], ['H%d' % s])


def phase_final(kb, C):
    dr = kb.dr
    with kb.phase() as P:
        P._junk = P.sb("junk", [128, D]); P._ss = P.sb("ss", [128, 1]); P._eps = P.sb("eps", [128, 1])
        P.memset(P._eps[:], EPS, ['eps'])
        fw = P.sb("fw", [128, D])
        P.dma(fw[:], dr['final_norm_w'].partition_broadcast(128), [], ['modbc'], q='sp')
        hts = [P.sb("ht%d" % i, [128, D]) for i in range(2)]
        xms = [P.sb("xm%d" % i, [128, D]) for i in range(2)]
        n = 0
        for s in range(2):
            for tt in range(2, NT):
                i = n % 2; n += 1
                P.dma(hts[i][:], dr['H'][s, tt * 128:(tt + 1) * 128, :], ['H%d' % s], ['%dh' % i])
                norm_mod_tile(P, C, hts[i][:], xms[i][:], fw[:], None, '%d' % i)
                P.dma(dr['out'][s, (tt - 2) * 128:(tt - 1) * 128, :], xms[i][:], ['%dxm' % i], ['out'])


def phase_zero_scratch(kb, C):
    dr = kb.dr
    with kb.phase() as P:
        z = P.sb("zz", [128, T])
        P.memset(z[:], 0.0, ['zz'])
        for s in range(2):
            for a in range(NT):
                P.dma(dr['MIX'][s][a * 128:(a + 1) * 128, :], z[:, 0:768], ['zz'], ['MIX%d' % s])
            for a in range(2):
                P.dma(dr['S5T'][s][a * 128:(a + 1) * 128, :], z[:], ['zz'], ['S5T%d' % s])


def phase_outproj(kb, C, li, s):
    dr = kb.dr
    last = (li == DEPTH - 1)
    with kb.phase() as P:
        wo = P.sb("wo", [128, 8, D])
        with Sub(P) as Q:
            woraw = Q.sb("woraw", [128, 8, D])
            Q.dma(woraw[:], dr['w_out'][li].rearrange("(k p) c -> p k c", p=128), [], ['woraw'])
            Q.cp(wo[:, 0:4, :].bitcast(F32R), woraw[:, 0:4, :], ['woraw'], ['wo'], eng='act')
            Q.cp(wo[:, 4:8, :].bitcast(F32R), woraw[:, 4:8, :], ['woraw'], ['wo'], eng='dve')
        gbc = P.sb("gbc", [128, 2, D])
        P.dma(gbc[:, 0, :], dr['MODV'][2, 2 * D:3 * D].partition_broadcast(128), ['MODV'], ['gbc'], q='sp')
        P.dma(gbc[:, 1, :], dr['MODV'][s, 2 * D:3 * D].partition_broadcast(128), ['MODV'], ['gbc'], q='sp')
        mx = [P.sb("mx%d" % i, [128, 768]) for i in range(2)]
        mT = [P.sb("mT%d" % i, [128, 8, 128]) for i in range(2)]
        s5r = [P.sb("s5r%d" % i, [128, 2, 128]) for i in range(2)]
        ht = [P.sb("oh%d" % i, [128, D]) for i in range(2)]
        tp = [P.ps("otp%d" % i, [128, 768]) for i in range(2)]
        po = [P.ps("opo%d" % i, [128, D]) for i in range(2)]
        for tt in range(2 if last else 0, NT):
            i = tt % 2
            si = 0 if tt < 2 else 1
            tsl = slice(tt * 128, (tt + 1) * 128)
            P.dma(mx[i][:], dr['MIX'][s][tsl, :], ['MIX%d' % s], ['mx%d' % i])
            P.dma(s5r[i][:], dr['S5T'][s][:, tsl].rearrange("(k p) t -> p k t", p=128), ['S5T%d' % s], ['s5r%d' % i])
            P.dma(ht[i][:], dr['H'][s, tsl, :], ['H%d' % s], ['oh%d' % i])
            for j in range(6):
                P.tr(tp[i][:, j * 128:(j + 1) * 128], mx[i][:, j * 128:(j + 1) * 128], C['idn'][:], ['mx%d' % i], ['otp%d' % i])
            P.cp(mT[i][:, 0:2, :].bitcast(F32R), tp[i][:, 0:256].rearrange("p (k t) -> p k t", k=2), ['otp%d' % i], ['mT%d' % i], eng='act')
            P.cp(mT[i][:, 2:4, :].bitcast(F32R), s5r[i][:], ['s5r%d' % i], ['mT%d' % i], eng='pool' if False else 'act')
            P.cp(mT[i][:, 4:8, :].bitcast(F32R), tp[i][:, 256:768].rearrange("p (k t) -> p k t", k=4), ['otp%d' % i], ['mT%d' % i], eng='dve')
            for hf in range(2):
                for kc in range(8):
                    P.mm(po[i][:, hf * 512:(hf + 1) * 512], mT[i][:, kc, :], wo[:, kc, hf * 512:(hf + 1) * 512], kc == 0, kc == 7, ['mT%d' % i, 'wo'], ['opo%d' % i], r32=True)
            P.tt(po_sb(P, i)[:], po[i][:], gbc[:, si, :], ALU.mult, ['opo%d' % i, 'gbc'], ['osb%d' % i])
            P.tt(ht[i][:], ht[i][:], po_sb(P, i)[:], ALU.add, ['oh%d' % i, 'osb%d' % i], ['oh%d' % i], eng='pool')
            P.dma(dr['H'][s, tsl, :], ht[i][:], ['oh%d' % i], ['H%d' % s])


def po_sb(P, i):
    if not hasattr(P, '_posb'):
        P._posb = [P.sb("osb%d" % k, [128, D]) for k in range(2)]
    return P._posb[i]


def declare_io(kb, nlayers=DEPTH):
    L = nlayers
    kb.dram_in('x', [2, NLAT, D]); kb.dram_in('ctx', [2, NCTX, D]); kb.dram_in('c', [2, D]); kb.dram_in('c_ctx', [D])
    kb.dram_in('mod_w', [L, D, 6 * D]); kb.dram_in('mod_b', [L, 6 * D])
    kb.dram_in('norm1_w', [L, D]); kb.dram_in('norm2_w', [L, D])
    kb.dram_in('WT', [L, D, NZT]); kb.dram_in('BT', [L, NZT]); kb.dram_in('WF', [L, D, NZF]); kb.dram_in('BF', [L, NZF])
    kb.dram_in('c_idn', [128, 128]); kb.dram_in('c_maskf', [128, 128]); kb.dram_in('c_maskb', [128, 128]); kb.dram_in('c_onesbd', [128, 128])
    kb.dram_in('c_chm', [128, 4]); kb.dram_in('c_rm', [2, 64, T])
    kb.dram_in('hg_lb_logits', [DEPTH, 2, 256]); kb.dram_in('hg_norm_w', [L, 256]); kb.dram_in('ml_norm_w', [L, 256])
    kb.dram('LB', [DEPTH, 2, 256]); kb.dram('MIX', [2, T, 768]); kb.dram('S5T', [2, 256, T]); kb.dram_in('w_out', [L, D, D])
    kb.dram_in('c_ropec', [128, NLAT]); kb.dram_in('c_ropes', [128, NLAT])
    for nm in ['da_lq1', 'da_lk1', 'da_lq2', 'da_lk2']:
        kb.dram_in(nm, [L, 32])
    kb.dram_in('da_norm_w', [L, 256])
    for nm in ['s5_lam_re', 's5_lam_im', 'S5_LOGDT']:
        kb.dram_in(nm, [L, 2, 16, 64])
    kb.dram_in('S5_WBre', [L, 256, 1024]); kb.dram_in('S5_WBim', [L, 256, 1024]); kb.dram_in('S5_WCre', [L, 1024, 256]); kb.dram_in('S5_WCim', [L, 1024, 256])
    kb.dram_in('s5_d', [L, 256]); kb.dram_in('s5_glu_w', [L, 256, 256]); kb.dram_in('s5_glu_b', [L, 256]); kb.dram_in('c_iota256', [128, 256])
    kb.dram_in('router_w', [L, D, NE]); kb.dram_in('exp_w1', [L, NE, D, 2 * D]); kb.dram_in('exp_w3', [L, NE, D, 2 * D]); kb.dram_in('exp_w2', [L, NE, 2 * D, D])
    kb.dram_in('c_tok', [128, NT])
    kb.dram('DBG_POS', [NE, T]); kb.dram('DBG_AFF', [NE, T]); kb.dram('DBG_PTM', [128, NT, NE])
    kb.dram('XM2', [2 * T, D]); kb.dram('MOE', [2 * T, D])
    kb.dram('IDXL', [2, 128, 2 * NE], I32); kb.dram('GATEL', [2, 128, 2 * NE]); kb.dram('IDXC', [2 * CAPC, NE], I32); kb.dram('GATEC', [2 * CAPC, NE])
    kb.dram_in('final_norm_w', [D]); kb.dram('out', [2, NLAT, D], out=True)
    kb.dram('H', [2, T, D]); kb.dram('ZT', [2, T, NZT]); kb.dram('ZF', [2, NZF, T]); kb.dram('MODV', [3, 6 * D])


def host_inputs(inp, core, L=DEPTH):
    zt, zf = colmaps()
    b0 = 2 * core
    m = {}
    m['x'] = np.ascontiguousarray(inp['x'][b0:b0 + 2]); m['ctx'] = np.ascontiguousarray(inp['ctx'][b0:b0 + 2])
    m['c'] = np.ascontiguousarray(inp['c'][b0:b0 + 2]); m['c_ctx'] = inp['c_ctx']
    return m


def host_shared(inp, L=DEPTH):
    zt, zf = colmaps()
    m = {}
    for k in ['mod_w', 'mod_b', 'norm1_w', 'norm2_w']:
        m[k] = np.ascontiguousarray(inp[k][:L])
    m['WT'] = np.ascontiguousarray(inp['w_in'][:L][:, :, zt]); m['BT'] = np.ascontiguousarray(inp['b_in'][:L][:, zt])
    m['WF'] = np.ascontiguousarray(inp['w_in'][:L][:, :, zf]); m['BF'] = np.ascontiguousarray(inp['b_in'][:L][:, zf])
    m['c_idn'] = np.eye(128, dtype=np.float32)
    m['final_norm_w'] = inp['final_norm_w']
    p = np.arange(128)
    same = (p[:, None] // 32) == (p[None, :] // 32)
    m['c_maskf'] = (same & (p[:, None] <= p[None, :])).astype(np.float32)
    m['c_maskb'] = (same & (p[:, None] >= p[None, :])).astype(np.float32)
    m['c_onesbd'] = same.astype(np.float32)
    m['c_chm'] = (p[:, None] // 32 == np.arange(4)[None, :]).astype(np.float32)
    t = np.arange(T)
    rmm = np.stack([(t % 32 != 0), (t % 32 != 31)]).astype(np.float32)
    m['c_rm'] = np.ascontiguousarray(np.broadcast_to(rmm[:, None, :], (2, 64, T)))
    m['hg_lb_logits'] = np.ascontiguousarray(inp['hg_lb_logits'])
    for k in ['router_w', 'exp_w1', 'exp_w3', 'exp_w2']:
        m[k] = inp[k] if L == inp[k].shape[0] else np.ascontiguousarray(inp[k][:L])
    m['c_tok'] = (np.arange(128, dtype=np.float32)[:, None] + 128.0 * np.arange(NT, dtype=np.float32)[None, :]).astype(np.float32)
    for k in ['s5_lam_re', 's5_lam_im', 's5_d', 's5_glu_w', 's5_glu_b']:
        m[k] = np.ascontiguousarray(inp[k][:L])
    m['S5_LOGDT'] = np.ascontiguousarray(np.broadcast_to(inp['s5_log_dt'][:L][..., None], (L, 2, 16, 64)))
    m['c_iota256'] = np.ascontiguousarray(np.broadcast_to(np.arange(256, dtype=np.float32)[None, :], (128, 256)))
    WBr = np.zeros((L, 256, 1024), np.float32); WBi = np.zeros((L, 256, 1024), np.float32)
    WCr = np.zeros((L, 1024, 256), np.float32); WCi = np.zeros((L, 1024, 256), np.float32)
    for g in range(16):
        WBr[:, g * 16:(g + 1) * 16, g * 64:(g + 1) * 64] = np.transpose(inp['s5_b_re'][:L, g], (0, 2, 1))
        WBi[:, g * 16:(g + 1) * 16, g * 64:(g + 1) * 64] = np.transpose(inp['s5_b_im'][:L, g], (0, 2, 1))
        WCr[:, g * 64:(g + 1) * 64, g * 16:(g + 1) * 16] = np.transpose(inp['s5_c_re'][:L, g], (0, 2, 1))
        WCi[:, g * 64:(g + 1) * 64, g * 16:(g + 1) * 16] = np.transpose(inp['s5_c_im'][:L, g], (0, 2, 1))
    m['S5_WBre'], m['S5_WBim'], m['S5_WCre'], m['S5_WCim'] = WBr, WBi, WCr, WCi
    tl = np.arange(NLAT)
    inv = 10000.0 ** (-np.arange(0, 16, 2, dtype=np.float32) / np.float32(16))
    rowp = (tl // 64).astype(np.float32); colp = (tl % 64).astype(np.float32)
    ang = np.concatenate([rowp[:, None] * inv[None, :], colp[:, None] * inv[None, :]], axis=-1).astype(np.float32)
    dd = np.arange(128) % 32
    cosT = np.cos(ang)[:, dd // 2].T.astype(np.float32)
    sinT = np.sin(ang)[:, dd // 2].T.astype(np.float32)
    sgn = np.where(dd % 2 == 0, -1.0, 1.0).astype(np.float32)
    m['c_ropec'] = np.ascontiguousarray(cosT)
    m['c_ropes'] = np.ascontiguousarray(sinT * sgn[:, None])
    for k in ['hg_norm_w', 'ml_norm_w', 'w_out', 'da_lq1', 'da_lk1', 'da_lq2', 'da_lk2', 'da_norm_w']:
        m[k] = np.ascontiguousarray(inp[k][:L])
    return m


def copy_inputs_to_H(kb, C):
    dr = kb.dr
    with kb.phase() as P:
        bufs = [P.sb("cpb%d" % i, [128, 4, D]) for i in range(2)]
        n = 0
        for s in range(2):
            srcs = [(dr['ctx'][s], 0, NCTX), (dr['x'][s], NCTX, NLAT)]
            for (src, base, cnt) in srcs:
                for r0 in range(0, cnt, 512):
                    rn = min(512, cnt - r0)
                    i = n % 2
                    n += 1
                    P.dma(bufs[i][:, 0:rn // 128, :], src[r0:r0 + rn, :].rearrange("(a p) d -> p a d", p=128), [], ['cpb%d' % i])
                    P.dma(dr['H'][s, base + r0:base + r0 + rn, :].rearrange("(a p) d -> p a d", p=128), bufs[i][:, 0:rn // 128, :], ['cpb%d' % i], ['H%d' % s])


def build_program(debug=(), upto='all', nlayers=DEPTH, mixers='ABCD'):
    nc = bass.Bass("TRN2", target_bir_lowering=False)
    with ExitStack() as es:
        kb = KB(nc, es, debug)
        declare_io(kb, nlayers)
        with kb.phase() as PC:
            C = load_consts(kb, PC)
            kb.S.barrier()
            copy_inputs_to_H(kb, C)
            phase_lb(kb, C)
            phase_zero_scratch(kb, C)
            for li in range(nlayers):
                phase_mod(kb, C, li)
                for s in range(2):
                    phase_inproj(kb, C, li, s)
                    if upto == 'inproj':
                        break
                    if 'A' in mixers:
                        phase_gla(kb, C, li, s, 'A')
                    if 'D' in mixers:
                        phase_gla(kb, C, li, s, 'D')
                    if 'C' in mixers:
                        phase_attn(kb, C, li, s)
                    if 'B' in mixers:
                        phase_s5(kb, C, li, s)
                    if upto == 'mix1':
                        break
                    phase_outproj(kb, C, li, s)
                    if upto == 'outproj1':
                        continue
                    phase_moe_route(kb, C, li, s)
                if upto not in ('inproj', 'mix1', 'outproj1'):
                    phase_moe_experts(kb, C, li)
                    for s in range(2):
                        phase_moe_residual(kb, C, li, s)
                if upto in ('inproj', 'mix1', 'outproj1', 'layer1'):
                    break
            phase_final(kb, C)
        kb.S.finish()
    return nc, kb


def kernel(**inputs):
    inp = {k: np.asarray(v) for k, v in inputs.items()}
    nc, kb = build_program()
    shared = host_shared(inp)
    in_maps = []
    for core in range(8):
        m = host_inputs(inp, core)
        m.update(shared)
        in_maps.append({k: v for k, v in m.items() if k in kb.dr})
    res = run_bass_kernel_spmd(nc, in_maps, core_ids=list(range(8)))
    out = np.concatenate([r['out'] for r in res.results], axis=0)
    return out.astype(np.float32)
```

```python
import math
from contextlib import ExitStack
import numpy as np
import concourse.bass as bass
import concourse.mybir as mybir
from concourse.ap import AP
from concourse.bass_utils import run_bass_kernel_spmd

F32 = mybir.dt.float32
I32 = mybir.dt.int32
F32R = mybir.dt.float32r
ALU = mybir.AluOpType
AF = mybir.ActivationFunctionType
AX = mybir.AxisListType

D = 1024
T = 2304
NCTX = 256
NLAT = 2048
NT = T // 128
DEPTH = 2
EPS = 1e-6
IN_COLS = 3344
NZT = 3072
NZF = 3584
NDSEM = 6

ZT_A_I, ZT_A_FF, ZT_A_FB, ZT_A_G, ZT_C_V, ZT_D_K, ZT_D_V, ZT_D_O, ZT_D_IF, ZT_D_IB, ZT_D_FF, ZT_D_FB = [256 * i for i in range(12)]
ZF_A_Q, ZF_A_FF, ZF_A_FB, ZF_B_U, ZF_C_Q, ZF_C_K, ZF_C_QS, ZF_C_KS, ZF_D_Q, ZF_D_K, ZF_D_IF, ZF_D_IB, ZF_D_FF, ZF_D_FB = [256 * i for i in range(14)]


def colmaps():
    r = np.arange(256)
    HG, S5, DA, ML, MG = 0, 1280, 1536, 2304, 3328
    def gate(j):
        return MG + j * 4 + r // 64
    zt = np.concatenate([HG + 256 + r, HG + 512 + r, HG + 768 + r, HG + 1024 + r, DA + 512 + r,
                         ML + 256 + r, ML + 512 + r, ML + 768 + r, gate(0), gate(1), gate(2), gate(3)])
    zf = np.concatenate([HG + r, HG + 512 + r, HG + 768 + r, S5 + r, DA + r, DA + 256 + r, DA + (r ^ 1), DA + 256 + (r ^ 1),
                         ML + r, ML + 256 + r, gate(0), gate(1), gate(2), gate(3)])
    return zt, zf


class Sched:
    def __init__(self, nc, es):
        self.nc = nc
        self.eng = {}
        for name in ['pe', 'act', 'dve', 'pool', 'sp']:
            sem = es.enter_context(nc.semaphore("s_" + name))
            self.eng[name] = dict(sem=sem, cnt=0, seen={}, ops=[])
        self.dsem = {}
        for q in ['sp', 'act', 'pool']:
            self.dsem[q] = [[es.enter_context(nc.semaphore("d_%s%d" % (q, i))), 0] for i in range(NDSEM)]
        self.drr = {'sp': 0, 'act': 0, 'pool': 0}
        self.bufs = {}
        self.nops = 0

    def _deps(self, reads, writes):
        deps = []
        for k in reads:
            b = self.bufs.get(k)
            if b and b['w'] is not None:
                deps.append(b['w'])
        for k in writes:
            b = self.bufs.get(k)
            if b:
                if b['w'] is not None:
                    deps.append(b['w'])
                deps.extend(b['r'])
        return deps

    def _commit(self, tok, reads, writes):
        for k in reads:
            b = self.bufs.setdefault(k, dict(w=None, r=[]))
            b['r'].append(tok)
            if len(b['r']) > 48:
                best = {}
                for (s, v, e) in b['r']:
                    if id(s) not in best or best[id(s)][1] < v:
                        best[id(s)] = (s, v, e)
                b['r'] = list(best.values())
        for k in writes:
            self.bufs[k] = dict(w=tok, r=[])

    def _waits(self, ename, deps, skip_same=False):
        e = self.eng[ename]
        waits = []
        for (sem, val, src) in deps:
            if skip_same and src == ename:
                continue
            if e['seen'].get(id(sem), 0) < val:
                e['seen'][id(sem)] = val
                waits.append((sem, val))
        return waits

    def op(self, ename, fn, reads=(), writes=()):
        e = self.eng[ename]
        deps = self._deps(reads, writes)
        waits = self._waits(ename, deps, skip_same=(ename == 'pe'))
        e['cnt'] += 1
        tok = (e['sem'], e['cnt'], ename)
        e['ops'].append((waits, fn, (e['sem'], 1)))
        self._commit(tok, reads, writes)
        self.nops += 1
        return tok

    def dma(self, q, out, in_, reads=(), writes=(), indirect=None, **kw):
        e = self.eng[q]
        slot = self.dsem[q][self.drr[q] % NDSEM]
        self.drr[q] += 1
        deps = self._deps(reads, writes)
        if slot[1] > 0:
            deps.append((slot[0], 16 * slot[1], 'dma'))
        waits = self._waits(q, deps)
        slot[1] += 1
        tok = (slot[0], 16 * slot[1], 'dma')
        if indirect is None:
            fn = (lambda h: h.dma_start(out=out, in_=in_, **kw))
        else:
            fn = indirect
        e['ops'].append((waits, fn, (slot[0], 16)))
        self._commit(tok, reads, writes)
        self.nops += 1
        return tok

    def barrier(self):
        toks = []
        for name, e in self.eng.items():
            if e['cnt'] > 0:
                toks.append((e['sem'], e['cnt'], name))
        for q in self.dsem:
            for slot in self.dsem[q]:
                if slot[1] > 0:
                    toks.append((slot[0], 16 * slot[1], 'dma'))
        for name in self.eng:
            w = self._waits(name, [t for t in toks if t[2] != name or t[2] == 'dma'])
            if w:
                self.eng[name]['ops'].append((w, None, None))
        self.bufs = {}

    def finish(self):
        self.barrier()
        nc = self.nc
        handles = {'pe': 'tensor', 'act': 'scalar', 'dve': 'vector', 'pool': 'gpsimd', 'sp': 'sync'}
        with nc.Block() as block:
            for name in ['pe', 'act', 'dve', 'pool', 'sp']:
                ops = self.eng[name]['ops']

                def body(h, ops=ops):
                    for (waits, fn, inc) in ops:
                        for (sem, val) in waits:
                            h.wait_ge(sem, val)
                        if fn is not None:
                            ins = fn(h)
                            ins.then_inc(inc[0], inc[1])
                getattr(block, handles[name])(body)


def rev(ap):
    a = [list(x) for x in ap.ap]
    st, n = a[-1]
    off = ap.offset + st * (n - 1)
    a[-1] = [-st, n]
    return AP(ap.tensor, off, a)


class KB:
    def __init__(self, nc, es, debug=()):
        self.nc = nc
        self.es = es
        self.S = Sched(nc, es)
        self.debug = set(debug)
        self.dr = {}
        self.uid = 0
        self.qrr = 0

    def dram_in(self, name, shape, dt=F32):
        self.dr[name] = self.nc.dram_tensor(name, list(shape), dt, kind="ExternalInput").ap()
        return self.dr[name]

    def dram(self, name, shape, dt=F32, out=False):
        kind = "ExternalOutput" if (out or name in self.debug) else "Internal"
        self.dr[name] = self.nc.dram_tensor(name, list(shape), dt, kind=kind).ap()
        return self.dr[name]

    def phase(self):
        return Phase(self)

    def q(self):
        self.qrr += 1
        return ['sp', 'pool'][self.qrr % 2]


class Phase:
    def __init__(self, kb):
        self.kb = kb
        self.S = kb.S
        self.nc = kb.nc
        self.st = ExitStack()

    def __enter__(self):
        self.st.__enter__()
        return self

    def __exit__(self, *a):
        self.S.barrier()
        return self.st.__exit__(*a)

    def sb(self, name, shape, dt=F32):
        self.kb.uid += 1
        return self.st.enter_context(self.nc.sbuf_tensor("%s_%d" % (name, self.kb.uid), list(shape), dt))

    def ps(self, name, shape, dt=F32):
        self.kb.uid += 1
        return self.st.enter_context(self.nc.psum_tensor("%s_%d" % (name, self.kb.uid), list(shape), dt))

    def mm(self, out, lhsT, rhs, start, stop, r, w, skip=False, r32=False):
        if r32:
            if lhsT.dtype != F32R:
                lhsT = lhsT.bitcast(F32R)
            if rhs.dtype != F32R:
                rhs = rhs.bitcast(F32R)
        if skip:
            self.S.op('pe', lambda h: h.matmul(out, lhsT, rhs, start=start, stop=stop, skip_group_check=True), reads=r, writes=w)
        else:
            self.S.op('pe', lambda h: h.matmul(out, lhsT, rhs, start=start, stop=stop), reads=r, writes=w)

    def tr(self, out, in_, idn, r, w):
        self.S.op('pe', lambda h: h.transpose(out, in_, idn), reads=list(r) + ['const'], writes=w)

    def act(self, out, in_, func, r, w, bias=None, scale=None, accum=None, eng='act'):
        kw = {}
        if bias is not None:
            kw['bias'] = bias
        if scale is not None:
            kw['scale'] = scale
        if accum is not None:
            kw['accum_out'] = accum
        self.S.op('act', lambda h: h.activation(out=out, in_=in_, func=func, **kw), reads=r, writes=w)

    def tt(self, out, in0, in1, op, r, w, eng='dve'):
        self.S.op(eng, lambda h: h.tensor_tensor(out=out, in0=in0, in1=in1, op=op), reads=r, writes=w)

    def ts(self, out, in0, s1, op0, r, w, s2=None, op1=None, eng='dve', accum=None):
        kw = {}
        if op1 is not None:
            kw['op1'] = op1
        if accum is not None:
            kw['accum_out'] = accum
        self.S.op(eng, lambda h: h.tensor_scalar(out=out, in0=in0, scalar1=s1, scalar2=s2, op0=op0, **kw), reads=r, writes=w)

    def stt(self, out, in0, scalar, in1, op0, op1, r, w, eng='dve'):
        self.S.op(eng, lambda h: h.scalar_tensor_tensor(out=out, in0=in0, scalar=scalar, in1=in1, op0=op0, op1=op1), reads=r, writes=w)

    def cp(self, out, in_, r, w, eng='dve'):
        if eng == 'act':
            self.S.op('act', lambda h: h.copy(out=out, in_=in_), reads=r, writes=w)
        else:
            self.S.op(eng, lambda h: h.tensor_copy(out=out, in_=in_), reads=r, writes=w)

    def round32r(self, ap, key, eng='pool'):
        self.cp(ap.bitcast(F32R), ap, [key], [key], eng=eng)

    def memset(self, ap, val, w, eng='dve'):
        self.S.op(eng, lambda h: h.memset(ap, val), writes=w)

    def scan(self, out, d0, d1, init, r, w, op0=ALU.mult, op1=ALU.add):
        self.S.op('dve', lambda h: h.tensor_tensor_scan(out=out, data0=d0, data1=d1, initial=init, op0=op0, op1=op1), reads=r, writes=w)

    def recip(self, out, in_, r, w):
        self.S.op('dve', lambda h: h.reciprocal(out=out, in_=in_), reads=r, writes=w)

    def dma(self, out, in_, r, w, q=None, **kw):
        self.S.dma(q or self.kb.q(), out, in_, reads=r, writes=w, **kw)


def load_consts(kb, P):
    c = {}
    idn = P.sb("idn", [128, 128])
    P.dma(idn[:], kb.dr['c_idn'], [], ['const'])
    ones = P.sb("ones", [128, 128])
    P.memset(ones[:], 1.0, ['const'])
    onecol = P.sb("onecol", [128, 1])
    P.memset(onecol[:], 1.0, ['const'])
    c['onecol'] = onecol
    c['idn'] = idn
    c['ones'] = ones
    return c


def phase_mod(kb, C, li):
    dr = kb.dr
    with kb.phase() as P:
        cT = P.sb("cT", [128, 3, 8])
        for s_ in range(2):
            P.dma(cT[:, s_, :], dr['c'][s_].rearrange("(k p) -> p k", p=128), [], ['cT'], q='sp', allow_slow_non_contiguous=True)
        P.dma(cT[:, 2, :], dr['c_ctx'].rearrange("(k p) -> p k", p=128), [], ['cT'], q='sp', allow_slow_non_contiguous=True)
        scT = P.sb("scT", [128, 3, 8])
        P.act(scT[:], cT[:], AF.Silu, ['cT'], ['scT'])
        modsb = P.sb("modsb", [3, 6 * D])
        nw = P.sb("nw", [3, 2, D])
        P.dma(nw[:, 0, :], dr['norm1_w'][li].partition_broadcast(3), [], ['nw'], q='sp')
        P.dma(nw[:, 1, :], dr['norm2_w'][li].partition_broadcast(3), [], ['nw'], q='sp')
        wb = [P.sb("mw%d" % i, [128, 8, 512]) for i in range(2)]
        bb = [P.sb("mb%d" % i, [1, 512]) for i in range(2)]
        pp = [P.ps("mps%d" % i, [3, 512]) for i in range(2)]
        for cb in range(12):
            i = cb % 2
            P.dma(wb[i][:], dr['mod_w'][li][:, cb * 512:(cb + 1) * 512].rearrange("(k p) c -> p k c", p=128), [], ['mw%d' % i])
            P.dma(bb[i][:], dr['mod_b'][li:li + 1, cb * 512:(cb + 1) * 512], [], ['mb%d' % i], q='sp')
            for kc in range(8):
                P.mm(pp[i][:], scT[:, :, kc], wb[i][:, kc, :], kc == 0, False, ['scT', 'mw%d' % i], ['mps%d' % i])
            P.mm(pp[i][:], C['ones'][0:1, 0:3], bb[i][:], False, True, ['const', 'mb%d' % i], ['mps%d' % i])
            P.cp(modsb[:, cb * 512:(cb + 1) * 512], pp[i][:], ['mps%d' % i], ['modsb'], eng='act')
        for j, ch in enumerate([1, 4]):
            P.stt(modsb[:, ch * D:(ch + 1) * D], modsb[:, ch * D:(ch + 1) * D], 1.0, nw[:, j, :], ALU.add, ALU.mult, ['modsb', 'nw'], ['modsb'])
        P.dma(dr['MODV'], modsb[:], ['modsb'], ['MODV'], q='sp')


def norm_mod_tile(P, C, ht, xm, weff, shift, tag):
    junk = P._junk
    ss = P._ss
    P.memset(ss[:], 0.0, ['ss'])
    P.act(junk[:], ht, AF.Square, [tag + 'h', 'ss'], ['junk', 'ss'], accum=ss[:])
    P.act(ss[:], ss[:], AF.Sqrt, ['ss'], ['ss'], scale=1.0 / D, bias=P._eps[:])
    P.recip(ss[:], ss[:], ['ss'], ['ss'])
    P.stt(xm, ht, ss[:], weff, ALU.mult, ALU.mult, [tag + 'h', 'ss', 'modbc'], [tag + 'xm'])
    if shift is not None:
        P.tt(xm, xm, shift, ALU.add, [tag + 'xm', 'modbc'], [tag + 'xm'])


def phase_inproj(kb, C, li, s):
    dr = kb.dr
    with kb.phase() as P:
        xmT = P.sb("xmT", [128, 8, T])
        with Sub(P) as Q:
            Q._junk = Q.sb("junk", [128, D])
            Q._ss = Q.sb("ss", [128, 1])
            Q._eps = Q.sb("eps", [128, 1])
            Q.memset(Q._eps[:], EPS, ['eps'])
            modbc = Q.sb("modbc", [128, 2, 2, D])
            for si, st in enumerate([2, s]):
                Q.dma(modbc[:, si, 0, :], dr['MODV'][st, 0:D].partition_broadcast(128), ['MODV'], ['modbc'], q='sp')
                Q.dma(modbc[:, si, 1, :], dr['MODV'][st, D:2 * D].partition_broadcast(128), ['MODV'], ['modbc'], q='sp')
            hts = [Q.sb("ht%d" % i, [128, D]) for i in range(2)]
            xms = [Q.sb("xm%d" % i, [128, D]) for i in range(2)]
            tps = [Q.ps("tp%d" % i, [128, 1024]) for i in range(2)]
            for tt in range(NT):
                i = tt % 2
                si = 0 if tt < 2 else 1
                Q.dma(hts[i][:], dr['H'][s, tt * 128:(tt + 1) * 128, :], ['H%d' % s], ['%dh' % i])
                norm_mod_tile(Q, C, hts[i][:], xms[i][:], modbc[:, si, 1, :], modbc[:, si, 0, :], '%d' % i)
                for kc in range(8):
                    Q.tr(tps[i][:, kc * 128:(kc + 1) * 128], xms[i][:, kc * 128:(kc + 1) * 128], C['idn'][:], ['%dxm' % i], ['tp%d' % i])
                Q.cp(xmT[:, :, tt * 128:(tt + 1) * 128].bitcast(F32R), tps[i][:].rearrange("p (k t) -> p k t", k=8), ['tp%d' % i], ['xmT'], eng=('act' if tt % 2 else 'dve'))
        wraw = P.sb("wraw", [128, 8, 512])
        wts = [P.sb("wt%d" % i, [128, 8, 512]) for i in range(2)]
        bts = [P.sb("bt%d" % i, [1, 512]) for i in range(2)]
        ops_ = [P.ps("ops%d" % i, [128, 512]) for i in range(4)]
        stg = [P.sb("stg%d" % i, [128, 512]) for i in range(2)]
        n = 0
        for cb in range(NZT // 512):
            i = cb % 2
            P.dma(wraw[:], dr['WT'][li][:, cb * 512:(cb + 1) * 512].rearrange("(k p) c -> p k c", p=128), [], ['wraw'])
            P.dma(bts[i][:], dr['BT'][li:li + 1, cb * 512:(cb + 1) * 512], [], ['bt%d' % i], q='sp')
            P.cp(wts[i][:, 0:4, :].bitcast(F32R), wraw[:, 0:4, :], ['wraw'], ['wt%d' % i], eng='act')
            P.cp(wts[i][:, 4:8, :].bitcast(F32R), wraw[:, 4:8, :], ['wraw'], ['wt%d' % i], eng='dve')
            for tt in range(NT):
                j = n % 4
                n += 1
                for kc in range(8):
                    P.mm(ops_[j][:], xmT[:, kc, tt * 128:(tt + 1) * 128], wts[i][:, kc, :], kc == 0, False, ['xmT', 'wt%d' % i], ['ops%d' % j], r32=True)
                P.mm(ops_[j][:], C['ones'][0:1, 0:128], bts[i][:], False, True, ['const', 'bt%d' % i], ['ops%d' % j])
                P.cp(stg[j % 2][:], ops_[j][:], ['ops%d' % j], ['stg%d' % (j % 2)], eng=('act' if j % 2 else 'dve'))
                P.dma(dr['ZT'][s, tt * 128:(tt + 1) * 128, cb * 512:(cb + 1) * 512], stg[j % 2][:], ['stg%d' % (j % 2)], ['ZT%d' % s])
        wfraw = P.sb("wfraw", [128, 8, 128])
        wfs = [P.sb("wf%d" % i, [128, 8, 128]) for i in range(2)]
        bF = P.sb("bF", [128, NZF // 128])
        P.dma(bF[:], dr['BF'][li].rearrange("(m p) -> p m", p=128), [], ['bF'], q='sp', allow_slow_non_contiguous=True)
        stf = [P.sb("stf%d" % i, [128, T]) for i in range(2)]
        for m in range(NZF // 128):
            i = m % 2
            P.dma(wfraw[:], dr['WF'][li][:, m * 128:(m + 1) * 128].rearrange("(k p) c -> p k c", p=128), [], ['wfraw'])
            P.cp(wfs[i][:].bitcast(F32R), wfraw[:], ['wfraw'], ['wf%d' % i], eng='pool' if False else 'dve')
            for tg in range(5):
                t0 = tg * 512
                tn = min(512, T - t0)
                j = n % 4
                n += 1
                for kc in range(8):
                    P.mm(ops_[j][:, 0:tn], wfs[i][:, kc, :], xmT[:, kc, t0:t0 + tn], kc == 0, kc == 7, ['xmT', 'wf%d' % i], ['ops%d' % j], r32=True)
                P.act(stf[i][:, t0:t0 + tn], ops_[j][:, 0:tn], AF.Identity, ['ops%d' % j, 'bF'], ['stf%d' % i], bias=bF[:, m:m + 1])
            P.dma(dr['ZF'][s, m * 128:(m + 1) * 128, :], stf[i][:], ['stf%d' % i], ['ZF%d' % s])


class Sub:
    def __init__(self, P):
        self.P = P
    def __enter__(self):
        self.saved = self.P.st
        self.P.st = ExitStack()
        self.P.st.__enter__()
        return self.P
    def __exit__(self, *a):
        self.P.S.barrier()
        r = self.P.st.__exit__(*a)
        self.P.st = self.saved
        return r


def pp_of_chunk(c, d):
    if d == 0:
        return c
    return (7 - c) if c < 8 else 8 + (71 - c)


def phase_lb(kb, C):
    dr = kb.dr
    with kb.phase() as P:
        a = P.sb("lba", [1, 2, 512])
        P.dma(a[:, 0, :], dr['hg_lb_logits'][0:1].rearrange("o d c -> o (d c)"), [], ['lba'], q='sp')
        P.dma(a[:, 1, :], dr['hg_lb_logits'][1:2].rearrange("o d c -> o (d c)"), [], ['lba'], q='sp')
        o = P.sb("lbo", [1, 2, 512])
        P.memset(o[:], 0.0, ['lbo'])
        P.tt(a[:, 0, :], a[:, 1, :], a[:, 0, :], ALU.subtract, ['lba'], ['lba'])
        P.act(o[:, 1, :], a[:, 0, :], AF.Sigmoid, ['lba', 'lbo'], ['lbo'])
        P.dma(dr['LB'].rearrange("(o l) d c -> o l (d c)", o=1), o[:], ['lbo'], ['LB'], q='sp')


def phase_gla(kb, C, li, s, mixer):
    dr = kb.dr
    ML = (mixer == 'D')
    dv = 65 if ML else 64
    ZT, ZF = dr['ZT'][s], dr['ZF'][s]
    def ztile(colbase):
        return ZT[:, colbase:colbase + 256].rearrange("(a p) c -> p a c", p=128)
    with kb.phase() as P:
        vaug = P.sb("vaug", [128, NT, 4, dv])
        oacc = P.sb("oacc", [128, NT, 4, 64])
        khat = P.sb("khat", [128, NT, 256])
        msk = P.sb("msk", [128, 2, 128])
        onesbd = P.sb("onesbd", [128, 128])
        chm = P.sb("chm", [128, 4])
        rm = P.sb("rm", [64, T])
        P.dma(msk[:, 0, :], dr['c_maskf'], [], ['msk'], q='sp')
        P.dma(msk[:, 1, :], dr['c_maskb'], [], ['msk'], q='sp')
        P.dma(onesbd[:], dr['c_onesbd'], [], ['onesbd'], q='sp')
        P.dma(chm[:], dr['c_chm'], [], ['chm'], q='sp')
        if ML:
            P.memset(vaug[:], 1.0, ['vaug'])
        vsrc = ztile(ZT_D_V if ML else ZT_A_I)
        for tt_ in range(NT):
            P.dma(vaug[:, tt_, :, 0:64], vsrc[:, tt_, :].rearrange("p (h v) -> p h v", h=4), ['ZT%d' % s], ['vaug'])
        for d in range(2):
            P.dma(rm[:], dr['c_rm'][d], [], ['rm'], q='sp')
            with Sub(P) as Q:
                zf = Q.sb("zf", [128, NT, 256])
                kt = Q.sb("kt", [128, NT, 256])
                bt = Q.sb("bt", [128, NT, 256])
                be = Q.sb("be", [128, NT, 256])
                pb = [Q.ps("pb%d" % i, [128, 512]) for i in range(2)]
                if ML:
                    Q.dma(zf[:], ztile(ZT_D_FF + 256 * d), ['ZT%d' % s], ['zf'])
                    Q.dma(kt[:], ztile(ZT_D_K), ['ZT%d' % s], ['kt'])
                    Q.dma(bt[:], ztile(ZT_D_IF + 256 * d), ['ZT%d' % s], ['bt'])
                    Q.act(bt[:], bt[:], AF.Exp, ['bt'], ['bt'])
                    Q.stt(kt[:], kt[:], 0.125, bt[:], ALU.mult, ALU.mult, ['kt', 'bt'], ['kt'])
                    Q.act(zf[:], zf[:], AF.Exp, ['zf'], ['zf'], scale=-1.0)
                    Q.act(zf[:], zf[:], AF.Ln, ['zf'], ['zf'], bias=C['onecol'][:])
                    Q.ts(zf[:], zf[:], -1.0, ALU.mult, ['zf'], ['zf'])
                else:
                    lbb = Q.sb("lbb", [128, 2, 256])
                    Q.dma(zf[:], ztile(ZT_A_FF + 256 * d), ['ZT%d' % s], ['zf'])
                    Q.dma(lbb[:, 0, :], dr['LB'][li, d].partition_broadcast(128), ['LB'], ['lbb'], q='sp')
                    Q.ts(lbb[:, 1, :], lbb[:, 0, :], -1.0, ALU.mult, ['lbb'], ['lbb'], s2=1.0, op1=ALU.add)
                    Q.act(zf[:], zf[:], AF.Sigmoid, ['zf'], ['zf'])
                    Q.tt(zf[:], zf[:], lbb[:, None, 1, :].to_broadcast([128, NT, 256]), ALU.mult, ['zf', 'lbb'], ['zf'])
                    Q.tt(zf[:], zf[:], lbb[:, None, 0, :].to_broadcast([128, NT, 256]), ALU.add, ['zf', 'lbb'], ['zf'])
                    Q.ts(kt[:], zf[:], -1.0, ALU.mult, ['zf'], ['kt'], s2=1.0, op1=ALU.add)
                    Q.act(zf[:], zf[:], AF.Ln, ['zf'], ['zf'])
                zff = zf[:].rearrange("p a c -> p (a c)")
                btf = bt[:].rearrange("p a c -> p (a c)")
                bef = be[:].rearrange("p a c -> p (a c)")
                for j in range(NT * 256 // 512):
                    i = j % 2
                    Q.mm(pb[i][:], msk[:, d, :], zff[:, j * 512:(j + 1) * 512], True, True, ['msk', 'zf'], ['pb%d' % i])
                    Q.cp(btf[:, j * 512:(j + 1) * 512], pb[i][:], ['pb%d' % i], ['bt'], eng='act')
                    Q.mm(pb[i][:], onesbd[:], zff[:, j * 512:(j + 1) * 512], True, True, ['onesbd', 'zf'], ['pb%d' % i])
                    Q.tt(bef[:, j * 512:(j + 1) * 512], pb[i][:], btf[:, j * 512:(j + 1) * 512], ALU.subtract, ['pb%d' % i, 'bt'], ['be'])
                Q.act(be[:], be[:], AF.Exp, ['be'], ['be'])
                Q.tt(khat[:], kt[:], be[:], ALU.mult, ['kt', 'be'], ['khat'])
            for h in range(4):
                with Sub(P) as Q:
                    qT = Q.sb("qT", [64, T]); kT = Q.sb("kT", [64, T]); fT = Q.sb("fT", [64, T]); eb = Q.sb("eb", [64, T])
                    r0 = h * 64
                    if ML:
                        Q.dma(qT[:], ZF[ZF_D_Q + r0:ZF_D_Q + r0 + 64, :], ['ZF%d' % s], ['qT'])
                        Q.dma(kT[:], ZF[ZF_D_K + r0:ZF_D_K + r0 + 64, :], ['ZF%d' % s], ['kT'])
                        Q.dma(fT[:], ZF[ZF_D_FF + 256 * d + r0:ZF_D_FF + 256 * d + r0 + 64, :], ['ZF%d' % s], ['fT'])
                        Q.dma(eb[:], ZF[ZF_D_IF + 256 * d + r0:ZF_D_IF + 256 * d + r0 + 64, :], ['ZF%d' % s], ['eb'])
                        Q.act(eb[:], eb[:], AF.Exp, ['eb'], ['eb'])
                        Q.stt(kT[:], kT[:], 0.125, eb[:], ALU.mult, ALU.mult, ['kT', 'eb'], ['kT'])
                        Q.act(fT[:], fT[:], AF.Exp, ['fT'], ['fT'], scale=-1.0)
                        Q.act(fT[:], fT[:], AF.Ln, ['fT'], ['fT'], bias=C['onecol'][0:64, :])
                        Q.ts(fT[:], fT[:], -1.0, ALU.mult, ['fT'], ['fT'])
                    else:
                        lbc = Q.sb("lbc", [64, 2])
                        Q.dma(qT[:], ZF[ZF_A_Q + r0:ZF_A_Q + r0 + 64, :], ['ZF%d' % s], ['qT'])
                        Q.dma(fT[:], ZF[ZF_A_FF + 256 * d + r0:ZF_A_FF + 256 * d + r0 + 64, :], ['ZF%d' % s], ['fT'])
                        Q.dma(lbc[:, 0:1], dr['LB'][li, d, r0:r0 + 64].rearrange("(p o) -> p o", o=1), ['LB'], ['lbc'], q='sp', allow_slow_non_contiguous=True)
                        Q.ts(lbc[:, 1:2], lbc[:, 0:1], -1.0, ALU.mult, ['lbc'], ['lbc'], s2=1.0, op1=ALU.add)
                        Q.act(fT[:], fT[:], AF.Sigmoid, ['fT'], ['fT'])
                        Q.ts(fT[:], fT[:], lbc[:, 1:2], ALU.mult, ['fT', 'lbc'], ['fT'], s2=lbc[:, 0:1], op1=ALU.add)
                        Q.ts(kT[:], fT[:], -1.0, ALU.mult, ['fT'], ['kT'], s2=1.0, op1=ALU.add)
                        Q.act(fT[:], fT[:], AF.Ln, ['fT'], ['fT'])
                    if d == 0:
                        Q.scan(eb[:], rm[:], fT[:], 0.0, ['rm', 'fT'], ['eb'])
                    else:
                        Q.scan(rev(eb[:]), rev(rm[:]), rev(fT[:]), 0.0, ['rm', 'fT'], ['eb'])
                    Q.act(fT[:], eb[:], AF.Exp, ['eb'], ['fT'], scale=-1.0)
                    Q.tt(fT[:], fT[:], kT[:], ALU.mult, ['fT', 'kT'], ['fT'])
                    Q.act(eb[:], eb[:], AF.Exp, ['eb'], ['eb'])
                    Q.stt(qT[:], qT[:], (1.0 if ML else 0.125), eb[:], ALU.mult, ALU.mult, ['qT', 'eb'], ['qT'])
                    ktil = fT
                    dS = Q.sb("dS", [64, dv, 72]); dec = Q.sb("dec", [64, dv, 72]); So = Q.sb("So", [64, dv, 72])
                    ebt = eb[:]
                    pst = ebt.ap[0][0]
                    if d == 0:
                        src = AP(ebt.tensor, ebt.offset + 31, [[pst, 64], [0, dv], [32, 72]])
                        Q.cp(dec[:], src, ['eb'], ['dec'])
                    else:
                        src = AP(ebt.tensor, ebt.offset + 32 * 7, [[pst, 64], [0, dv], [-32, 8]])
                        Q.cp(dec[:, :, 0:8], src, ['eb'], ['dec'])
                        src = AP(ebt.tensor, ebt.offset + 32 * 71, [[pst, 64], [0, dv], [-32, 64]])
                        Q.cp(dec[:, :, 8:72], src, ['eb'], ['dec'])
                    Q.memset(dec[:, :, 0:1], 0.0, ['dec'])
                    vm = [Q.sb("vm%d" % i, [128, 4, dv]) for i in range(2)]
                    pd = [Q.ps("pd%d" % i, [64, 4 * dv]) for i in range(2)]
                    dSt = dS[:]
                    dpst = dSt.ap[0][0]
                    for tt in range(NT):
                        i = tt % 2
                        Q.tt(vm[i][:], vaug[:, tt, h, None, :].to_broadcast([128, 4, dv]), chm[:, :, None].to_broadcast([128, 4, dv]), ALU.mult, ['vaug', 'chm'], ['vm%d' % i], eng=('pool' if i else 'dve'))
                        Q.mm(pd[i][:], khat[:, tt, h * 64:(h + 1) * 64], vm[i][:].rearrange("p c v -> p (c v)"), True, True, ['khat', 'vm%d' % i], ['pd%d' % i])
                        pp0 = pp_of_chunk(tt * 4, d)
                        pdt = pd[i][:]
                        src = AP(pdt.tensor, pdt.offset, [[pdt.ap[0][0], 64], [1, dv], [dv, 4]])
                        dst_ = AP(dSt.tensor, dSt.offset + pp0, [[dpst, 64], [72, dv], [1 if d == 0 else -1, 4]])
                        Q.cp(dst_, src, ['pd%d' % i], ['dS'], eng='act')
                    Q.scan(So[:].rearrange("p v c -> p (v c)"), dec[:].rearrange("p v c -> p (v c)"), dS[:].rearrange("p v c -> p (v c)"), 0.0, ['dec', 'dS'], ['So'])
                    pa = [Q.ps("pa%d" % i, [128, 128]) for i in range(2)]
                    po = [Q.ps("po%d" % i, [128, dv]) for i in range(2)]
                    pi = [Q.ps("pi%d" % i, [128, 4 * dv]) for i in range(2)]
                    asb = [Q.sb("asb%d" % i, [128, 128]) for i in range(2)]
                    acc = [Q.sb("acc%d" % i, [128, dv]) for i in range(2)]
                    dtmp = Q.sb("dtmp", [128, 1])
                    mcnt = [0]

                    def stage_a(tt):
                        i = tt % 2
                        tsl = slice(tt * 128, (tt + 1) * 128)
                        Q.mm(pa[i][:], ktil[:, tsl], qT[:, tsl], True, True, ['fT', 'qT'], ['pa%d' % i])
                        Q.tt(asb[i][:], pa[i][:], msk[:, d, :], ALU.mult, ['pa%d' % i, 'msk'], ['asb%d' % i])

                    def stage_b(tt):
                        i = tt % 2
                        tsl = slice(tt * 128, (tt + 1) * 128)
                        Q.mm(po[i][:], asb[i][:], vaug[:, tt, h, :], True, True, ['asb%d' % i, 'vaug'], ['po%d' % i])
                        Q.cp(acc[i][:], po[i][:], ['po%d' % i], ['acc%d' % i], eng='act')
                        pps = [pp_of_chunk(tt * 4 + cc, d) for cc in range(4)]
                        if 0 in pps:
                            for cc in range(4):
                                ppx = pps[cc]
                                if ppx == 0:
                                    continue
                                j = mcnt[0] % 2; mcnt[0] += 1
                                Q.mm(pi[j][:, 0:dv], qT[:, tsl], So[:, :, ppx - 1], True, True, ['qT', 'So'], ['pi%d' % j])
                                Q.stt(acc[i][:], pi[j][:, 0:dv], chm[:, cc:cc + 1], acc[i][:], ALU.mult, ALU.add, ['pi%d' % j, 'chm', 'acc%d' % i], ['acc%d' % i])
                        else:
                            j = mcnt[0] % 2; mcnt[0] += 1
                            Sot = So[:]
                            rhs4 = AP(Sot.tensor, Sot.offset + pps[0] - 1, [[Sot.ap[0][0], 64], [1 if d == 0 else -1, 4], [72, dv]])
                            Q.mm(pi[j][:], qT[:, tsl], rhs4, True, True, ['qT', 'So'], ['pi%d' % j])
                            for cc in range(4):
                                Q.stt(acc[i][:], pi[j][:, cc * dv:(cc + 1) * dv], chm[:, cc:cc + 1], acc[i][:], ALU.mult, ALU.add, ['pi%d' % j, 'chm', 'acc%d' % i], ['acc%d' % i])
                        dst = oacc[:, tt, h, :]
                        if ML:
                            den = acc[i][:, 64:65]
                            Q.ts(dtmp[:], den, -1.0, ALU.mult, ['acc%d' % i], ['dtmp'])
                            Q.tt(den, den, dtmp[:], ALU.max, ['acc%d' % i, 'dtmp'], ['acc%d' % i])
                            Q.ts(den, den, 1.0, ALU.max, ['acc%d' % i], ['acc%d' % i])
                            Q.recip(den, den, ['acc%d' % i], ['acc%d' % i])
                            if d == 0:
                                Q.ts(dst, acc[i][:, 0:64], den, ALU.mult, ['acc%d' % i], ['oacc'])
                            else:
                                Q.stt(dst, acc[i][:, 0:64], den, dst, ALU.mult, ALU.add, ['acc%d' % i, 'oacc'], ['oacc'])
                        else:
                            if d == 0:
                                Q.cp(dst, acc[i][:, 0:64], ['acc%d' % i], ['oacc'], eng='pool')
                            else:
                                Q.tt(dst, dst, acc[i][:, 0:64], ALU.add, ['acc%d' % i, 'oacc'], ['oacc'], eng='pool')

                    stage_a(0)
                    for tt in range(NT):
                        if tt + 1 < NT:
                            stage_a(tt + 1)
                        stage_b(tt)
        with Sub(P) as Q:
            g = Q.sb("g", [128, NT, 256]); sq = Q.sb("sq", [128, NT * 4, 64]); ssum = Q.sb("ssum", [128, NT * 4]); nwb = Q.sb("nwb", [128, 256])
            epsc = Q.sb("epsc", [128, 1])
            Q.memset(epsc[:], EPS, ['epsc'])
            Q.dma(g[:], ztile(ZT_D_O if ML else ZT_A_G), ['ZT%d' % s], ['g'])
            Q.dma(nwb[:], dr['ml_norm_w' if ML else 'hg_norm_w'][li].partition_broadcast(128), [], ['nwb'], q='sp')
            Q.act(g[:], g[:], AF.Sigmoid if ML else AF.Silu, ['g'], ['g'])
            of = oacc[:].rearrange("p a h v -> p (a h) v")
            Q.tt(sq[:], of, of, ALU.mult, ['oacc'], ['sq'])
            Q.S.op('dve', lambda hh: hh.tensor_reduce(out=ssum[:], in_=sq[:], axis=AX.X, op=ALU.add), reads=['sq'], writes=['ssum'])
            Q.act(ssum[:], ssum[:], AF.Sqrt, ['ssum', 'epsc'], ['ssum'], scale=1.0 / 64, bias=epsc[:])
            Q.recip(ssum[:], ssum[:], ['ssum'], ['ssum'])
            Q.tt(of, of, ssum[:, :, None].to_broadcast([128, NT * 4, 64]), ALU.mult, ['oacc', 'ssum'], ['oacc'])
            o3 = oacc[:].rearrange("p a h v -> p a (h v)")
            Q.tt(o3, o3, nwb[:, None, :].to_broadcast([128, NT, 256]), ALU.mult, ['oacc', 'nwb'], ['oacc'])
            Q.tt(o3, o3, g[:], ALU.mult, ['oacc', 'g'], ['oacc'])
            base = 512 if ML else 0
            Q.dma(dr['MIX'][s][:, base:base + 256].rearrange("(a p) c -> p a c", p=128), o3, ['oacc'], ['MIX%d' % s], q='sp')


def phase_attn(kb, C, li, s):
    dr = kb.dr
    ZT, ZF = dr['ZT'][s], dr['ZF'][s]
    lam_init = 0.8 - 0.6 * math.exp(-0.3 * li)
    scl = 32 ** -0.5
    with kb.phase() as P:
        KR = P.sb("KR", [128, 2, T]); QR = P.sb("QR", [128, 2, T]); V = P.sb("V", [128, NT, 4, 65])
        chm = P.sb("chm", [128, 4]); epsc = P.sb("epsc", [128, 1]); nwb = P.sb("nwb", [128, 256]); lamcol = P.sb("lamcol", [128, 1])
        negcb = P.sb("negcb", [128, 8]); oall = P.sb("oall", [128, NT, 256])
        P.memset(epsc[:], EPS, ['epsc'])
        P.dma(chm[:], dr['c_chm'], [], ['chm'], q='sp')
        P.dma(nwb[:], dr['da_norm_w'][li].partition_broadcast(128), [], ['nwb'], q='sp')
        P.ts(nwb[:], nwb[:], 1.0 - lam_init, ALU.mult, ['nwb'], ['nwb'])
        P.memset(V[:], 1.0, ['V'])
        vsrc = ZT[:, ZT_C_V:ZT_C_V + 256].rearrange("(a p) c -> p a c", p=128)
        for tt_ in range(NT):
            P.dma(V[:, tt_, :, 0:64], vsrc[:, tt_, :].rearrange("p (h v) -> p h v", h=4), ['ZT%d' % s], ['V'])
        with Sub(P) as Q:
            rc = Q.sb("rc", [128, NLAT]); rs = Q.sb("rs", [128, NLAT]); tmp = Q.sb("tmp", [128, NLAT])
            Q.dma(rc[:], dr['c_ropec'], [], ['rc'])
            Q.dma(rs[:], dr['c_ropes'], [], ['rs'])
            kraw = Q.sb("kraw", [128, T])
            for j in range(2):
                Q.dma(kraw[:], ZF[ZF_C_K + 128 * j:ZF_C_K + 128 * j + 128, :], ['ZF%d' % s], ['kraw'])
                Q.dma(tmp[:], ZF[ZF_C_KS + 128 * j:ZF_C_KS + 128 * j + 128, NCTX:T], ['ZF%d' % s], ['tmp'])
                Q.tt(tmp[:], tmp[:], rs[:], ALU.mult, ['tmp', 'rs'], ['tmp'], eng='pool')
                Q.cp(KR[:, j, 0:NCTX].bitcast(F32R), kraw[:, 0:NCTX], ['kraw'], ['KR'], eng='act')
                Q.tt(kraw[:, NCTX:T], kraw[:, NCTX:T], rc[:], ALU.mult, ['kraw', 'rc'], ['kraw'])
                Q.tt(KR[:, j, NCTX:T].bitcast(F32R), kraw[:, NCTX:T], tmp[:], ALU.add, ['kraw', 'tmp'], ['KR'])
                Q.dma(QR[:, j, :], ZF[ZF_C_Q + 128 * j:ZF_C_Q + 128 * j + 128, :], ['ZF%d' % s], ['QR'])
                Q.dma(tmp[:], ZF[ZF_C_QS + 128 * j:ZF_C_QS + 128 * j + 128, NCTX:T], ['ZF%d' % s], ['tmp'])
                Q.tt(tmp[:], tmp[:], rs[:], ALU.mult, ['tmp', 'rs'], ['tmp'], eng='pool')
                Q.tt(QR[:, j, NCTX:T], QR[:, j, NCTX:T], rc[:], ALU.mult, ['QR', 'rc'], ['QR'])
                Q.tt(QR[:, j, NCTX:T], QR[:, j, NCTX:T], tmp[:], ALU.add, ['QR', 'tmp'], ['QR'])
            l4 = Q.sb("l4", [1, 4, 32]); pr = Q.sb("pr", [1, 2, 32]); sm = Q.sb("sm", [1, 2]); lam1 = Q.sb("lam1", [1, 1])
            pl = Q.ps("pl", [128, 8])
            for i_, nm in enumerate(['da_lq1', 'da_lk1', 'da_lq2', 'da_lk2']):
                Q.dma(l4[:, i_, :], dr[nm][li:li + 1, :], [], ['l4'], q='sp')
            Q.tt(pr[:, 0, :], l4[:, 0, :], l4[:, 1, :], ALU.mult, ['l4'], ['pr'])
            Q.tt(pr[:, 1, :], l4[:, 2, :], l4[:, 3, :], ALU.mult, ['l4', 'pr'], ['pr'])
            Q.S.op('dve', lambda hh: hh.tensor_reduce(out=sm[:], in_=pr[:], axis=AX.X, op=ALU.add), reads=['pr'], writes=['sm'])
            Q.act(sm[:], sm[:], AF.Exp, ['sm'], ['sm'])
            Q.ts(lam1[:], sm[:, 0:1], sm[:, 1:2], ALU.subtract, ['sm'], ['lam1'], s2=lam_init, op1=ALU.add)
            Q.mm(pl[:, 0:1], C['ones'][0:1, 0:128], lam1[:], True, True, ['const', 'lam1'], ['pl'])
            Q.cp(lamcol[:], pl[:, 0:1], ['pl'], ['lamcol'])
            pn = [Q.ps("pn%d" % i, [4, 512]) for i in range(2)]
            nrm = Q.sb("nrm", [4, 2, 2, 5]); nmax = Q.sb("nmax", [4, 2, 2]); dg = Q.sb("dg", [4, 2, 4])
            n_ = 0
            for a_, (src, key) in enumerate([(QR, 'QR'), (KR, 'KR')]):
                for j in range(2):
                    Q.tt(tmp[:, 0:NLAT], src[:, j, 0:NLAT], src[:, j, 0:NLAT], ALU.mult, [key], ['tmp'], eng=('pool' if j else 'dve'))
                    Q.tt(rc[:, 0:NCTX], src[:, j, NLAT:T], src[:, j, NLAT:T], ALU.mult, [key], ['rc'], eng=('pool' if j else 'dve'))
                    for ch in range(5):
                        i = n_ % 2; n_ += 1
                        rhs_ = tmp[:, ch * 512:(ch + 1) * 512] if ch < 4 else rc[:, 0:NCTX]
                        wn = 512 if ch < 4 else NCTX
                        Q.mm(pn[i][:, 0:wn], chm[:, 0:4], rhs_, True, True, ['chm', 'tmp', 'rc'], ['pn%d' % i])
                        Q.S.op('dve', lambda hh, i=i, wn=wn, a_=a_, j=j, ch=ch: hh.reduce_max(out=nrm[:, a_, j, ch:ch + 1], in_=pn[i][:, 0:wn], axis=AX.X), reads=['pn%d' % i], writes=['nrm'])
            Q.S.op('dve', lambda hh: hh.reduce_max(out=nmax[:], in_=nrm[:], axis=AX.X), reads=['nrm'], writes=['nmax'])
            Q.tt(nmax[:, 0, :], nmax[:, 0, :], nmax[:, 1, :], ALU.mult, ['nmax'], ['nmax'])
            Q.act(nmax[:, 0, :], nmax[:, 0, :], AF.Sqrt, ['nmax'], ['nmax'])
            Q.ts(nmax[:, 0, :], nmax[:, 0, :], -scl, ALU.mult, ['nmax'], ['nmax'])
            for j in range(2):
                Q.ts(dg[:, j, :], C['idn'][0:4, 0:4], nmax[:, 0, j:j + 1], ALU.mult, ['const', 'nmax'], ['dg'])
            Q.mm(pl[:, 0:8], C['ones'][0:4, 0:128], dg[:].rearrange("p j c -> p (j c)"), True, True, ['const', 'dg'], ['pl'])
            Q.cp(negcb[:], pl[:, 0:8], ['pl'], ['negcb'])
        with Sub(P) as Q:
            ps = [Q.ps("aps%d" % i, [128, 512]) for i in range(4)]
            pavT = [Q.ps("pavT%d" % i, [65, 512]) for i in range(2)]
            ptn = Q.ps("ptn", [128, 4, 128])
            Vr = Q.sb("Vr", [128, NT, 4, 65])
            Q.cp(Vr[:, 0:9].bitcast(F32R), V[:, 0:9], ['V'], ['Vr'], eng='act')
            Q.cp(Vr[:, 9:NT].bitcast(F32R), V[:, 9:NT], ['V'], ['Vr'], eng='dve')
            PT = [Q.sb("PT%d" % i, [128, NT, 512]) for i in range(2)]
            qp = [Q.sb("qp%d" % i, [128, 512]) for i in range(2)]
            numT = [Q.sb("numT%d" % i, [65, 512]) for i in range(2)]
            num = [Q.sb("num%d" % i, [128, 4, 65]) for i in range(2)]
            rec = Q.sb("rec", [128, 4, 2]); t64 = Q.sb("t64", [128, 64])
            qgroups = [(NCTX + 512 * g, 512, NT) for g in range(4)] + ([(0, NCTX, 2)] if li < DEPTH - 1 else [])
            units = [(h, grp, m) for h in range(4) for grp in qgroups for m in range(2)]
            n1 = [0]

            def s_prep(u):
                h, (q0, N, nkt), m = units[u]
                j = h // 2
                cc = 2 * (h % 2) + m
                Q.ts(qp[u % 2][:, 0:N].bitcast(F32R), QR[:, j, q0:q0 + N], chm[:, cc:cc + 1], ALU.mult, ['QR', 'chm'], ['qp%d' % (u % 2)], eng='dve')

            def s_step(u, kt):
                h, (q0, N, nkt), m = units[u]
                j = h // 2
                col = j * 4 + 2 * (h % 2) + m
                i = n1[0] % 4; n1[0] += 1
                Q.mm(ps[i][:, 0:N], KR[:, j, kt * 128:(kt + 1) * 128], qp[u % 2][:, 0:N], True, True, ['KR', 'qp%d' % (u % 2)], ['aps%d' % i], r32=True)
                Q.act(PT[u % 2][:, kt, 0:N].bitcast(F32R), ps[i][:, 0:N], AF.Exp, ['aps%d' % i, 'negcb'], ['PT%d' % (u % 2)], bias=negcb[:, col:col + 1], scale=scl)

            def av_step(u, kt):
                h, (q0, N, nkt), m = units[u]
                Q.mm(pavT[u % 2][:, 0:N], Vr[:, kt, h, :], PT[u % 2][:, kt, 0:N], kt == 0, kt == nkt - 1, ['PT%d' % (u % 2), 'Vr'], ['pavT%d' % (u % 2)], r32=True)

            def av_finish(u):
                h, (q0, N, nkt), m = units[u]
                nqt = N // 128
                Q.cp(numT[u % 2][:, 0:N], pavT[u % 2][:, 0:N], ['pavT%d' % (u % 2)], ['numT%d' % (u % 2)], eng='act')
                for qt in range(nqt):
                    Q.tr(ptn[:, qt, 0:65], numT[u % 2][:, qt * 128:(qt + 1) * 128], C['idn'][0:65, 0:65], ['numT%d' % (u % 2)], ['ptn'])
                Q.cp(num[m][:, 0:nqt, :], ptn[:, 0:nqt, 0:65], ['ptn'], ['num%d' % m])
                if m == 1:
                    Q.cp(rec[:, 0:nqt, 0], num[0][:, 0:nqt, 64], ['num0'], ['rec'])
                    Q.cp(rec[:, 0:nqt, 1], num[1][:, 0:nqt, 64], ['num1', 'rec'], ['rec'])
                    Q.recip(rec[:, 0:nqt, :], rec[:, 0:nqt, :], ['rec'], ['rec'])
                    Q.ts(rec[:, 0:nqt, 1], rec[:, 0:nqt, 1], lamcol[:], ALU.mult, ['rec', 'lamcol'], ['rec'])
                    for qt in range(nqt):
                        tg = q0 // 128 + qt
                        Q.ts(t64[:], num[1][:, qt, 0:64], rec[:, qt, 1:2], ALU.mult, ['num1', 'rec'], ['t64'], eng='pool')
                        Q.stt(oall[:, tg, h * 64:(h + 1) * 64], num[0][:, qt, 0:64], rec[:, qt, 0:1], t64[:], ALU.mult, ALU.subtract, ['num0', 'rec', 't64'], ['oall'])

            s_prep(0)
            for kt in range(units[0][1][2]):
                s_step(0, kt)
            for u in range(len(units)):
                nkt_u = units[u][1][2]
                nxt = u + 1 if u + 1 < len(units) else None
                nkt_n = units[nxt][1][2] if nxt is not None else 0
                if nxt is not None:
                    s_prep(nxt)
                for kt in range(max(nkt_u, nkt_n)):
                    if kt < nkt_n:
                        s_step(nxt, kt)
                    if kt < nkt_u:
                        av_step(u, kt)
                av_finish(u)
        with Sub(P) as Q:
            t0_ = 0 if li < DEPTH - 1 else 2
            na = NT - t0_
            sq = Q.sb("sq", [128, NT * 4, 64]); ssum = Q.sb("ssum", [128, NT * 4])
            ov = oall[:, t0_:NT, :]
            of = ov.rearrange("p a (h v) -> p (a h) v", h=4)
            Q.tt(sq[:, 0:na * 4, :], of, of, ALU.mult, ['oall'], ['sq'])
            Q.S.op('dve', lambda hh: hh.tensor_reduce(out=ssum[:, 0:na * 4], in_=sq[:, 0:na * 4, :], axis=AX.X, op=ALU.add), reads=['sq'], writes=['ssum'])
            Q.act(ssum[:, 0:na * 4], ssum[:, 0:na * 4], AF.Sqrt, ['ssum', 'epsc'], ['ssum'], scale=1.0 / 64, bias=epsc[:])
            Q.recip(ssum[:, 0:na * 4], ssum[:, 0:na * 4], ['ssum'], ['ssum'])
            Q.tt(of, of, ssum[:, 0:na * 4, None].to_broadcast([128, na * 4, 64]), ALU.mult, ['oall', 'ssum'], ['oall'])
            Q.tt(ov, ov, nwb[:, None, :].to_broadcast([128, na, 256]), ALU.mult, ['oall', 'nwb'], ['oall'])
            Q.dma(dr['MIX'][s][t0_ * 128:T, 256:512].rearrange("(a p) c -> p a c", p=128), ov, ['oall'], ['MIX%d' % s], q='sp')


TWO_PI = 2.0 * math.pi
CW1 = 6.28125
CW2 = TWO_PI - CW1
PI_LO = 3.1415925


def sincos(Q, ang, F, sin_out, cos_out, rkeys, wkeys):
    ni = Q.sb("sc_ni", [128, F], I32); nf = Q.sb("sc_nf", [128, F]); r = Q.sb("sc_r", [128, F]); a2 = Q.sb("sc_a2", [128, F])
    for (shift, dst, wk) in [(0.0, sin_out, wkeys[0]), (math.pi / 2, cos_out, wkeys[1])]:
        Q.ts(a2[:], ang, shift, ALU.add, list(rkeys), ['sc_a2'])
        Q.ts(ni[:], a2[:], 1.0 / TWO_PI, ALU.mult, ['sc_a2'], ['sc_ni'])
        Q.cp(nf[:], ni[:], ['sc_ni'], ['sc_nf'])
        Q.stt(r[:], nf[:], -CW1, a2[:], ALU.mult, ALU.add, ['sc_nf', 'sc_a2'], ['sc_r'])
        Q.stt(r[:], nf[:], -CW2, r[:], ALU.mult, ALU.add, ['sc_nf', 'sc_r'], ['sc_r'])
        Q.ts(r[:], r[:], PI_LO, ALU.min, ['sc_r'], ['sc_r'], s2=-PI_LO, op1=ALU.max)
        Q.act(dst, r[:], AF.Sin, ['sc_r'], [wk])


def phase_s5(kb, C, li, s):
    dr = kb.dr
    ZF = dr['ZF'][s]
    LC = 256
    with kb.phase() as P:
        uT = P.sb("uT", [128, 2, T]); yT = P.sb("yT", [128, 2, T])
        WB = [P.sb("WB%d" % i, [128, 2, 1024]) for i in range(2)]
        WC = [P.sb("WC%d" % i, [128, 8, 256]) for i in range(2)]
        iotaL = P.sb("iotaL", [128, LC])
        cosL = P.sb("cosL", [128, 8, LC]); sinL = P.sb("sinL", [128, 8, LC]); Tr = P.sb("Tr", [128, 8, LC]); Ti = P.sb("Ti", [128, 8, LC])
        magbc = P.sb("magbc", [128, 8, LC])
        cL = P.sb("cL", [128, 8]); sL = P.sb("sL", [128, 8]); nsL = P.sb("nsL", [128, 8])
        for k in range(2):
            P.dma(uT[:, k, :], ZF[ZF_B_U + 128 * k:ZF_B_U + 128 * k + 128, :], ['ZF%d' % s], ['uT'])
        for i, nm in enumerate(['S5_WBre', 'S5_WBim']):
            P.dma(WB[i][:], dr[nm][li].rearrange("(k p) n -> p k n", p=128), [], ['WB'])
        for i, nm in enumerate(['S5_WCre', 'S5_WCim']):
            P.dma(WC[i][:], dr[nm][li].rearrange("(j p) c -> p j c", p=128), [], ['WC'])
        P.dma(iotaL[:], dr['c_iota256'], [], ['iotaL'], q='sp')
        for d in range(2):
            with Sub(P) as Q:
                lr = Q.sb("lr", [128, 8]); lim = Q.sb("lim", [128, 8]); dt = Q.sb("dt", [128, 8])
                for (tile_, nm, key) in [(lr, 's5_lam_re', 'lr'), (lim, 's5_lam_im', 'lim'), (dt, 'S5_LOGDT', 'dt')]:
                    Q.dma(tile_[:], dr[nm][li, d].rearrange("(j g) p -> (g p) j", g=2), [], [key], q='sp', allow_slow_non_contiguous=True)
                mag = Q.sb("mag", [128, 8]); th = Q.sb("th", [128, 8]); sn = Q.sb("sn", [128, 8]); cs = Q.sb("cs", [128, 8])
                Q.act(dt[:], dt[:], AF.Exp, ['dt'], ['dt'])
                Q.tt(mag[:], lr[:], dt[:], ALU.mult, ['lr', 'dt'], ['mag'])
                Q.act(mag[:], mag[:], AF.Exp, ['mag'], ['mag'])
                Q.tt(th[:], lim[:], dt[:], ALU.mult, ['lim', 'dt'], ['th'])
                with Sub(Q) as R:
                    sincos(R, th[:], 8, sn[:], cs[:], ['th'], ['sn', 'cs'])
                abr1 = Q.sb("abr1", [128, 8]); abi = Q.sb("abi", [128, 8]); den = Q.sb("den", [128, 8]); t1 = Q.sb("t1", [128, 8]); t2 = Q.sb("t2", [128, 8])
                cor = Q.sb("cor", [128, 8]); coi = Q.sb("coi", [128, 8]); ncor = Q.sb("ncor", [128, 8]); thL = Q.sb("thL", [128, 8])
                Q.tt(abr1[:], mag[:], cs[:], ALU.mult, ['mag', 'cs'], ['abr1'])
                Q.ts(abr1[:], abr1[:], -1.0, ALU.add, ['abr1'], ['abr1'])
                Q.tt(abi[:], mag[:], sn[:], ALU.mult, ['mag', 'sn'], ['abi'])
                Q.tt(den[:], lr[:], lr[:], ALU.mult, ['lr'], ['den'])
                Q.tt(t1[:], lim[:], lim[:], ALU.mult, ['lim'], ['t1'])
                Q.tt(den[:], den[:], t1[:], ALU.add, ['den', 't1'], ['den'])
                Q.recip(den[:], den[:], ['den'], ['den'])
                Q.tt(t1[:], abr1[:], lr[:], ALU.mult, ['abr1', 'lr'], ['t1'])
                Q.tt(t2[:], abi[:], lim[:], ALU.mult, ['abi', 'lim'], ['t2'])
                Q.tt(t1[:], t1[:], t2[:], ALU.add, ['t1', 't2'], ['t1'])
                Q.tt(cor[:], t1[:], den[:], ALU.mult, ['t1', 'den'], ['cor'])
                Q.tt(t1[:], abi[:], lr[:], ALU.mult, ['abi', 'lr'], ['t1'])
                Q.tt(t2[:], abr1[:], lim[:], ALU.mult, ['abr1', 'lim'], ['t2'])
                Q.tt(t1[:], t1[:], t2[:], ALU.subtract, ['t1', 't2'], ['t1'])
                Q.tt(coi[:], t1[:], den[:], ALU.mult, ['t1', 'den'], ['coi'])
                Q.ts(ncor[:], cor[:], -1.0, ALU.mult, ['cor'], ['ncor'])
                Q.ts(thL[:], th[:], float(LC), ALU.mult, ['th'], ['thL'])
                with Sub(Q) as R:
                    sincos(R, thL[:], 8, sL[:], cL[:], ['thL'], ['sL', 'cL'])
                Q.ts(nsL[:], sL[:], -1.0, ALU.mult, ['sL'], ['nsL'])
                with Sub(Q) as R:
                    angL = R.sb("angL", [128, 8, LC])
                    for j in range(8):
                        R.ts(angL[:, j, :], iotaL[:], th[:, j:j + 1], ALU.mult, ['iotaL', 'th'], ['angL'])
                    sincos(R, angL[:].rearrange("p j l -> p (j l)"), 8 * LC, sinL[:].rearrange("p j l -> p (j l)"), cosL[:].rearrange("p j l -> p (j l)"), ['angL'], ['sinL', 'cosL'])
                for j in range(8):
                    Q.ts(Tr[:, j, :], cosL[:, j, :], cor[:, j:j + 1], ALU.mult, ['cosL', 'cor'], ['Tr'])
                    Q.stt(Tr[:, j, :], sinL[:, j, :], coi[:, j:j + 1], Tr[:, j, :], ALU.mult, ALU.add, ['sinL', 'coi', 'Tr'], ['Tr'])
                    Q.ts(Ti[:, j, :], cosL[:, j, :], coi[:, j:j + 1], ALU.mult, ['cosL', 'coi'], ['Ti'])
                    Q.stt(Ti[:, j, :], sinL[:, j, :], ncor[:, j:j + 1], Ti[:, j, :], ALU.mult, ALU.add, ['sinL', 'ncor', 'Ti'], ['Ti'])
                Q.cp(magbc[:], mag[:, :, None].to_broadcast([128, 8, LC]), ['mag'], ['magbc'])
            with Sub(P) as Q:
                xin_r = Q.sb("xin_r", [128, 8]); xin_i = Q.sb("xin_i", [128, 8])
                Q.memset(xin_r[:], 0.0, ['xin_r']); Q.memset(xin_i[:], 0.0, ['xin_i'])
                NF = 3
                pbr = [Q.ps("pbr%d" % i, [128, LC]) for i in range(2)]; pbi = [Q.ps("pbi%d" % i, [128, LC]) for i in range(2)]
                py = [Q.ps("py%d" % i, [128, LC]) for i in range(2)]
                W = {}
                for nm in ['bur', 'bui', 'm1', 'm2', 'm3', 'm4']:
                    W[nm] = [Q.sb("%s%d" % (nm, i), [128, LC]) for i in range(2)]
                for nm in ['br', 'bi']:
                    W[nm] = [Q.sb("%s%d" % (nm, i), [128, LC]) for i in range(NF)]
                for nm in ['xr', 'xi', 'o1', 'o2', 'o3', 'o4', 'xro', 'xio']:
                    W[nm] = [Q.sb("%s%d" % (nm, i), [128, LC]) for i in range(2)]
                tsm = Q.sb("tsm", [128, 2])
                chunks = list(range(9)) if d == 0 else [0] + list(range(8, 0, -1))
                fx = (lambda ap: ap) if d == 0 else rev
                iters = [(ci, ct, jj) for ci in chunks for ct in range(2) for jj in range(4)]

                def front(n):
                    ci, ct, jj = iters[n]
                    j = ct * 4 + jj
                    tsl = slice(ci * LC, (ci + 1) * LC)
                    i = n % 2; f = n % NF
                    k = lambda nm: '%s%d' % (nm, i)
                    Q.mm(pbr[i][:], WB[0][:, ct, j * 128:(j + 1) * 128], uT[:, ct, tsl], True, True, ['WB', 'uT'], [k('pbr')])
                    Q.mm(pbi[i][:], WB[1][:, ct, j * 128:(j + 1) * 128], uT[:, ct, tsl], True, True, ['WB', 'uT'], [k('pbi')])
                    Q.cp(W['bur'][i][:], pbr[i][:], [k('pbr')], [k('bur')], eng='act')
                    Q.cp(W['bui'][i][:], pbi[i][:], [k('pbi')], [k('bui')], eng='act')
                    trj, tij = fx(Tr[:, j, :]), fx(Ti[:, j, :])
                    Q.tt(W['m1'][i][:], W['bur'][i][:], trj, ALU.mult, [k('bur'), 'Tr'], [k('m1')], eng='pool')
                    Q.tt(W['m2'][i][:], W['bui'][i][:], tij, ALU.mult, [k('bui'), 'Ti'], [k('m2')], eng='pool')
                    Q.tt(W['br'][f][:], W['m1'][i][:], W['m2'][i][:], ALU.subtract, [k('m1'), k('m2')], ['br%d' % f], eng='pool')
                    Q.tt(W['m3'][i][:], W['bui'][i][:], trj, ALU.mult, [k('bui'), 'Tr'], [k('m3')], eng='pool')
                    Q.tt(W['m4'][i][:], W['bur'][i][:], tij, ALU.mult, [k('bur'), 'Ti'], [k('m4')], eng='pool')
                    Q.tt(W['bi'][f][:], W['m3'][i][:], W['m4'][i][:], ALU.add, [k('m3'), k('m4')], ['bi%d' % f], eng='pool')

                def back(n):
                    ci, ct, jj = iters[n]
                    j = ct * 4 + jj
                    tsl = slice(ci * LC, (ci + 1) * LC)
                    i = n % 2; f = n % NF
                    pyi = (n // 4) % 2
                    k = lambda nm: '%s%d' % (nm, i)
                    Q.scan(fx(W['xr'][i][:]), fx(magbc[:, j, :]), fx(W['br'][f][:]), xin_r[:, j:j + 1], ['magbc', 'br%d' % f, 'xin_r'], [k('xr')])
                    Q.scan(fx(W['xi'][i][:]), fx(magbc[:, j, :]), fx(W['bi'][f][:]), xin_i[:, j:j + 1], ['magbc', 'bi%d' % f, 'xin_i'], [k('xi')])
                    last = (LC - 1) if d == 0 else 0
                    xrl, xil = W['xr'][i][:, last:last + 1], W['xi'][i][:, last:last + 1]
                    Q.ts(tsm[:, 0:1], xrl, cL[:, j:j + 1], ALU.mult, [k('xr'), 'cL'], ['tsm'])
                    Q.stt(xin_r[:, j:j + 1], xil, nsL[:, j:j + 1], tsm[:, 0:1], ALU.mult, ALU.add, [k('xi'), 'nsL', 'tsm'], ['xin_r'])
                    Q.ts(tsm[:, 1:2], xrl, sL[:, j:j + 1], ALU.mult, [k('xr'), 'sL'], ['tsm'])
                    Q.stt(xin_i[:, j:j + 1], xil, cL[:, j:j + 1], tsm[:, 1:2], ALU.mult, ALU.add, [k('xi'), 'cL', 'tsm'], ['xin_i'])
                    cj, sj = fx(cosL[:, j, :]), fx(sinL[:, j, :])
                    Q.tt(W['o1'][i][:], W['xr'][i][:], cj, ALU.mult, [k('xr'), 'cosL'], [k('o1')])
                    Q.tt(W['o2'][i][:], W['xi'][i][:], sj, ALU.mult, [k('xi'), 'sinL'], [k('o2')])
                    Q.tt(W['xro'][i][:], W['o1'][i][:], W['o2'][i][:], ALU.subtract, [k('o1'), k('o2')], [k('xro')])
                    Q.tt(W['o3'][i][:], W['xr'][i][:], sj, ALU.mult, [k('xr'), 'sinL'], [k('o3')])
                    Q.tt(W['o4'][i][:], W['xi'][i][:], cj, ALU.mult, [k('xi'), 'cosL'], [k('o4')])
                    Q.stt(W['xio'][i][:], W['o3'][i][:], -1.0, W['o4'][i][:], ALU.mult, ALU.subtract, [k('o3'), k('o4')], [k('xio')])
                    Q.mm(py[pyi][:], WC[0][:, j, ct * 128:(ct + 1) * 128], W['xro'][i][:], jj == 0, False, ['WC', k('xro')], ['py%d' % pyi])
                    Q.mm(py[pyi][:], WC[1][:, j, ct * 128:(ct + 1) * 128], W['xio'][i][:], False, jj == 3, ['WC', k('xio')], ['py%d' % pyi])
                    if jj == 3:
                        if d == 0:
                            Q.cp(yT[:, ct, tsl], py[pyi][:], ['py%d' % pyi], ['yT'], eng='act')
                        else:
                            Q.tt(yT[:, ct, tsl], yT[:, ct, tsl], py[pyi][:], ALU.add, ['py%d' % pyi, 'yT'], ['yT'])

                front(0)
                for n in range(len(iters)):
                    if n + 1 < len(iters):
                        front(n + 1)
                    back(n)
        with Sub(P) as Q:
            dsk = Q.sb("dsk", [128, 2]); gb = Q.sb("gb", [128, 2]); gw = Q.sb("gw", [128, 2, 256])
            Q.dma(dsk[:], dr['s5_d'][li].rearrange("(k p) -> p k", p=128), [], ['dsk'], q='sp', allow_slow_non_contiguous=True)
            Q.dma(gb[:], dr['s5_glu_b'][li].rearrange("(k p) -> p k", p=128), [], ['gb'], q='sp', allow_slow_non_contiguous=True)
            Q.dma(gw[:], dr['s5_glu_w'][li].rearrange("(k p) c -> p k c", p=128), [], ['gw'], q='sp')
            tq = Q.sb("tq", [128, 2, T])
            for ct in range(2):
                Q.stt(yT[:, ct, :], uT[:, ct, :], dsk[:, ct:ct + 1], yT[:, ct, :], ALU.mult, ALU.add, ['uT', 'dsk', 'yT'], ['yT'])
            Q.tt(tq[:], yT[:], yT[:], ALU.mult, ['yT'], ['tq'])
            Q.ts(tq[:], tq[:], 0.044715, ALU.mult, ['tq'], ['tq'], s2=1.0, op1=ALU.add)
            Q.tt(tq[:], tq[:], yT[:], ALU.mult, ['tq', 'yT'], ['tq'])
            Q.act(tq[:], tq[:], AF.Tanh, ['tq'], ['tq'], scale=0.7978845608028654)
            Q.ts(tq[:], tq[:], 1.0, ALU.add, ['tq'], ['tq'], s2=0.5, op1=ALU.mult)
            Q.tt(yT[:], yT[:], tq[:], ALU.mult, ['yT', 'tq'], ['yT'])
            pz = [Q.ps("pz%d" % i, [128, 512]) for i in range(2)]
            n = 0
            for m_ in range(2):
                for t0 in range(0, T, 512):
                    tn = min(512, T - t0)
                    i = n % 2; n += 1
                    for kc in range(2):
                        Q.mm(pz[i][:, 0:tn], gw[:, kc, m_ * 128:(m_ + 1) * 128], yT[:, kc, t0:t0 + tn], kc == 0, kc == 1, ['gw', 'yT'], ['pz%d' % i])
                    Q.act(tq[:, m_, t0:t0 + tn], pz[i][:, 0:tn], AF.Sigmoid, ['pz%d' % i, 'gb'], ['tq'], bias=gb[:, m_:m_ + 1])
            Q.tt(tq[:], tq[:], yT[:], ALU.mult, ['tq', 'yT'], ['tq'])
            for m_ in range(2):
                Q.dma(dr['S5T'][s][m_ * 128:(m_ + 1) * 128, :], tq[:, m_, :], ['tq'], ['S5T%d' % s])


NE = 16
CAPL = 256
CAPC = 32


def phase_moe_route(kb, C, li, s):
    dr = kb.dr
    last = (li == DEPTH - 1)
    with kb.phase() as P:
        P._junk = P.sb("junk", [128, D]); P._ss = P.sb("ss", [128, 1]); P._eps = P.sb("eps", [128, 1])
        P.memset(P._eps[:], EPS, ['eps'])
        modbc = P.sb("modbc", [128, 2, 2, D])
        for si, st in enumerate([2, s]):
            P.dma(modbc[:, si, 0, :], dr['MODV'][st, 3 * D:4 * D].partition_broadcast(128), ['MODV'], ['modbc'], q='sp')
            P.dma(modbc[:, si, 1, :], dr['MODV'][st, 4 * D:5 * D].partition_broadcast(128), ['MODV'], ['modbc'], q='sp')
        rw = P.sb("rw", [128, 8, NE])
        P.dma(rw[:], dr['router_w'][li].rearrange("(k p) e -> p k e", p=128), [], ['rw'], q='sp')
        affTM = P.sb("affTM", [128, NT, NE]); affT = P.sb("affT", [NE, T]); posT = P.sb("posT", [NE, T]); posTM = P.sb("posTM", [128, NT, NE])
        rhs2 = P.sb("rhs2", [128, NT, NE, 2]); tokc = P.sb("tokc", [128, NT]); iotaJ = P.sb("iotaJ", [128, 256])
        P.dma(tokc[:], dr['c_tok'], [], ['tokc'], q='sp')
        P.dma(iotaJ[:], dr['c_iota256'], [], ['iotaJ'], q='sp')
        if s:
            P.ts(tokc[:], tokc[:], float(s * T), ALU.add, ['tokc'], ['tokc'])
        tiles = list(range(2 if last else 0, NT))
        with Sub(P) as Q:
            hts = [Q.sb("ht%d" % i, [128, D]) for i in range(2)]
            xms = [Q.sb("xm%d" % i, [128, D]) for i in range(2)]
            xmT = [Q.sb("xmT%d" % i, [128, 8, 128]) for i in range(2)]
            tps = [Q.ps("tp%d" % i, [128, 1024]) for i in range(2)]
            pl = [Q.ps("pl%d" % i, [128, NE]) for i in range(2)]
            pT = [Q.ps("pT%d" % i, [NE, 128]) for i in range(2)]
            mx = Q.sb("mx", [128, 1]); sm = Q.sb("sm", [128, 1]); e16 = Q.sb("e16", [128, NE])
            for tt in tiles:
                i = tt % 2
                si = 0 if tt < 2 else 1
                tsl = slice(tt * 128, (tt + 1) * 128)
                Q.dma(hts[i][:], dr['H'][s, tsl, :], ['H%d' % s], ['%dh' % i])
                norm_mod_tile(Q, C, hts[i][:], xms[i][:], modbc[:, si, 1, :], modbc[:, si, 0, :], '%d' % i)
                Q.dma(dr['XM2'][s * T + tt * 128:s * T + (tt + 1) * 128, :], xms[i][:], ['%dxm' % i], ['XM2'])
                for kc in range(8):
                    Q.tr(tps[i][:, kc * 128:(kc + 1) * 128], xms[i][:, kc * 128:(kc + 1) * 128], C['idn'][:], ['%dxm' % i], ['tp%d' % i])
                Q.cp(xmT[i][:], tps[i][:].rearrange("p (k t) -> p k t", k=8), ['tp%d' % i], ['xmT%d' % i], eng='act')
                for kc in range(8):
                    Q.mm(pl[i][:], xmT[i][:, kc, :], rw[:, kc, :], kc == 0, kc == 7, ['xmT%d' % i, 'rw'], ['pl%d' % i])
                Q.S.op('dve', lambda hh, i=i: hh.reduce_max(out=mx[:], in_=pl[i][:], axis=AX.X), reads=['pl%d' % i], writes=['mx'])
                Q.ts(mx[:], mx[:], -1.0, ALU.mult, ['mx'], ['mx'])
                Q.memset(sm[:], 0.0, ['sm'])
                Q.act(e16[:], pl[i][:], AF.Exp, ['pl%d' % i, 'mx', 'sm'], ['e16', 'sm'], bias=mx[:], accum=sm[:])
                Q.recip(sm[:], sm[:], ['sm'], ['sm'])
                Q.ts(affTM[:, tt, :], e16[:], sm[:], ALU.mult, ['e16', 'sm'], ['affTM'])
                Q.tr(pT[i][:], affTM[:, tt, :], C['idn'][:], ['affTM'], ['pT%d' % i])
                Q.cp(affT[:, tsl], pT[i][:], ['pT%d' % i], ['affT'], eng='act')
        sets = [(NCTX, T, CAPL)] + ([] if last else [(0, NCTX, CAPC)])
        with Sub(P) as Q:
            work = Q.sb("work", [NE, NLAT]); m8 = Q.sb("m8", [NE, 8]); thr = Q.sb("thr", [NE, 1]); onesr = Q.sb("onesr", [NE, NLAT]); msk = Q.sb("mskr", [NE, NLAT])
            Q.memset(onesr[:], 1.0, ['onesr'])
            for (t0, t1, cap) in sets:
                n = t1 - t0
                Q.cp(work[:, 0:n], affT[:, t0:t1], ['affT'], ['work'])
                for r_ in range(cap // 8):
                    Q.S.op('dve', lambda hh, n=n: hh.max(out=m8[:], in_=work[:, 0:n]), reads=['work'], writes=['m8'])
                    if r_ < cap // 8 - 1:
                        Q.S.op('dve', lambda hh, n=n: hh.match_replace(out=work[:, 0:n], in_to_replace=m8[:], in_values=work[:, 0:n], imm_value=-1.0), reads=['work', 'm8'], writes=['work'])
                Q.cp(thr[:], m8[:, 7:8], ['m8'], ['thr'])
                Q.ts(msk[:, 0:n], affT[:, t0:t1], thr[:], ALU.is_ge, ['affT', 'thr'], ['mskr'])
                Q.scan(posT[:, t0:t1], onesr[:, 0:n], msk[:, 0:n], 0.0, ['onesr', 'mskr'], ['posT'])
                Q.tt(posT[:, t0:t1], posT[:, t0:t1], msk[:, 0:n], ALU.mult, ['posT', 'mskr'], ['posT'])
                Q.ts(posT[:, t0:t1], posT[:, t0:t1], -1.0, ALU.add, ['posT'], ['posT'])
        with Sub(P) as Q:
            pq = [Q.ps("pq%d" % i, [128, NE]) for i in range(2)]
            for tt in tiles:
                i = tt % 2
                Q.tr(pq[i][:], posT[:, tt * 128:(tt + 1) * 128], C['idn'][0:NE, 0:NE], ['posT'], ['pq%d' % i])
                Q.cp(posTM[:, tt, :], pq[i][:], ['pq%d' % i], ['posTM'], eng=('act' if i else 'dve'))
            if 'DBG_POS' in kb.debug and s == 0:
                Q.dma(dr['DBG_POS'], posT[:], ['posT'], ['DBG_POS'], q='sp')
                Q.dma(dr['DBG_AFF'], affT[:], ['affT'], ['DBG_AFF'], q='sp')
                Q.dma(dr['DBG_PTM'], posTM[:], ['posTM'], ['DBG_PTM'], q='sp')
            Q.cp(rhs2[:, :, :, 0], tokc[:, :, None].to_broadcast([128, NT, NE]), ['tokc'], ['rhs2'])
            Q.cp(rhs2[:, :, :, 1], affTM[:], ['affTM'], ['rhs2'], eng='pool')
            Pall = [Q.sb("Pall%d" % i, [128, NE, 256]) for i in range(2)]
            pacc = Q.ps("pacc", [128, 2 * NE, 16]); paccc = Q.ps("paccc", [CAPC, NE, 16])
            idxf = Q.sb("idxf", [128, 2 * NE, 2]); idxi = Q.sb("idxi", [128, 2 * NE], I32); gat = Q.sb("gat", [128, 2 * NE])
            for tt in range(2, NT):
                i = tt % 2
                Q.tt(Pall[i][:], iotaJ[:, None, :].to_broadcast([128, NE, 256]), posTM[:, tt, :, None].to_broadcast([128, NE, 256]), ALU.is_equal, ['iotaJ', 'posTM'], ['Pall%d' % i])
                for e in range(NE):
                    for jt in range(2):
                        Q.mm(pacc[:, e * 2 + jt, 0:2], Pall[i][:, e, jt * 128:(jt + 1) * 128], rhs2[:, tt, e, :], (tt == 2 and e == 0 and jt == 0), tt == NT - 1, ['Pall%d' % i, 'rhs2'], ['pacc'], skip=True)
            Q.cp(idxf[:], pacc[:, :, 0:2], ['pacc'], ['idxf'])
            Q.cp(idxi[:], idxf[:, :, 0], ['idxf'], ['idxi'])
            Q.cp(gat[:], idxf[:, :, 1], ['idxf'], ['gat'], eng='pool')
            Q.dma(dr['IDXL'][s], idxi[:], ['idxi'], ['IDXL'], q='sp')
            Q.dma(dr['GATEL'][s], gat[:], ['gat'], ['GATEL'], q='sp')
            if not last:
                Pc = [Q.sb("Pc%d" % i, [128, NE, CAPC]) for i in range(2)]
                idxfc = Q.sb("idxfc", [CAPC, NE, 2]); idxic = Q.sb("idxic", [CAPC, NE], I32); gatc = Q.sb("gatc", [CAPC, NE])
                for tt in range(2):
                    Q.tt(Pc[tt][:], iotaJ[:, None, 0:CAPC].to_broadcast([128, NE, CAPC]), posTM[:, tt, :, None].to_broadcast([128, NE, CAPC]), ALU.is_equal, ['iotaJ', 'posTM'], ['Pc%d' % tt])
                    for e in range(NE):
                        Q.mm(paccc[:, e, 0:2], Pc[tt][:, e, :], rhs2[:, tt, e, :], (tt == 0 and e == 0), tt == 1, ['Pc%d' % tt, 'rhs2'], ['paccc'], skip=True)
                Q.cp(idxfc[:], paccc[:, :, 0:2], ['paccc'], ['idxfc'])
                Q.cp(idxic[:], idxfc[:, :, 0], ['idxfc'], ['idxic'])
                Q.cp(gatc[:], idxfc[:, :, 1], ['idxfc'], ['gatc'], eng='pool')
                Q.dma(dr['IDXC'][s * CAPC:(s + 1) * CAPC, :], idxic[:], ['idxic'], ['IDXC'], q='sp')
                Q.dma(dr['GATEC'][s * CAPC:(s + 1) * CAPC, :], gatc[:], ['gatc'], ['GATEC'], q='sp')


def phase_moe_experts(kb, C, li):
    dr = kb.dr
    last = (li == DEPTH - 1)
    NJ = 512 if last else 576
    with kb.phase() as P:
        with Sub(P) as Q:
            zt = Q.sb("zt", [128, 4, D])
            Q.memset(zt[:], 0.0, ['zt'])
            for r0 in range(0, 2 * T, 512):
                Q.dma(dr['MOE'][r0:r0 + 512, :].rearrange("(a p) d -> p a d", p=128), zt[:], ['zt'], ['MOE'])
        idxL = P.sb("idxL", [128, 2, 2 * NE], I32); gatL = P.sb("gatL", [128, 2, 2 * NE])
        idxC = P.sb("idxC", [2 * CAPC, NE], I32); gatC = P.sb("gatC", [2 * CAPC, NE])
        for s in range(2):
            P.dma(idxL[:, s, :], dr['IDXL'][s], ['IDXL'], ['idxL'], q='sp')
            P.dma(gatL[:, s, :], dr['GATEL'][s], ['GATEL'], ['gatL'], q='sp')
        if not last:
            P.dma(idxC[:], dr['IDXC'], ['IDXC'], ['idxC'], q='sp')
            P.dma(gatC[:], dr['GATEC'], ['GATEC'], ['gatC'], q='sp')
        xs = P.sb("xs", [128, 4, D]); xsc = P.sb("xsc", [2 * CAPC, D])
        xsT = P.sb("xsT", [128, 8, 576]); gT = P.sb("gT", [128, 16, 576])
        w13r = [P.sb("w%dr" % a, [128, 8, 256]) for a in range(2)]
        w13 = [[P.sb("w%d_%d" % (a, i), [128, 8, 256]) for i in range(2)] for a in range(2)]
        w2r = P.sb("w2r", [128, 16, 256]); w2q = P.sb("w2q", [128, 16, 256])
        ysb = P.sb("ysb", [128, 5, D]); sg = P.sb("sg", [128, 576])
        ptr = [P.ps("ptr%d" % i, [128, 512]) for i in range(2)]
        ph = [P.ps("ph%d" % i, [128, 1024]) for i in range(2)]
        pye = P.ps("pye", [128, 1024])
        def gather(e):
            for s in range(2):
                for jt in range(2):
                    col = e * 2 + jt
                    P.S.dma('pool', None, None, reads=['XM2', 'idxL'], writes=['xs'],
                            indirect=(lambda hh, s=s, jt=jt, col=col: hh.indirect_dma_start(
                                out=xs[:, s * 2 + jt, :], out_offset=None, in_=dr['XM2'][:, :],
                                in_offset=bass.IndirectOffsetOnAxis(ap=idxL[:, s, col:col + 1], axis=0))))
            if not last:
                P.S.dma('pool', None, None, reads=['XM2', 'idxC'], writes=['xsc'],
                        indirect=(lambda hh, e=e: hh.indirect_dma_start(
                            out=xsc[:, :], out_offset=None, in_=dr['XM2'][:, :],
                            in_offset=bass.IndirectOffsetOnAxis(ap=idxC[:, e:e + 1], axis=0))))

        gather(0)
        nw = 0
        ntr = 0
        for e in range(NE):
            for a in range(4):
                for kh in range(2):
                    i = ntr % 2; ntr += 1
                    for k4 in range(4):
                        kc = kh * 4 + k4
                        P.tr(ptr[i][:, k4 * 128:(k4 + 1) * 128], xs[:, a, kc * 128:(kc + 1) * 128], C['idn'][:], ['xs'], ['ptr%d' % i])
                    P.cp(xsT[:, kh * 4:kh * 4 + 4, a * 128:(a + 1) * 128].bitcast(F32R), ptr[i][:].rearrange("p (k t) -> p k t", k=4), ['ptr%d' % i], ['xsT'], eng=('act' if i else 'dve'))
            if not last:
                i = ntr % 2; ntr += 1
                for kc in range(8):
                    P.tr(ptr[i][:, kc * 64:(kc + 1) * 64], xsc[:, kc * 128:(kc + 1) * 128], C['idn'][0:64, 0:64], ['xsc'], ['ptr%d' % i])
                P.cp(xsT[:, :, 512:576].bitcast(F32R), ptr[i][:].rearrange("p (k t) -> p k t", k=8), ['ptr%d' % i], ['xsT'])
            for fc in range(8):
                wi = nw % 2; nw += 1
                for a, nm in enumerate(['exp_w1', 'exp_w3']):
                    P.dma(w13r[a][:], dr[nm][li, e][:, fc * 256:(fc + 1) * 256].rearrange("(k p) f -> p k f", p=128), [], ['w%dr' % a], q=('sp' if a == 0 else 'act'))
                    P.cp(w13[a][wi][:].bitcast(F32R), w13r[a][:], ['w%dr' % a], ['w%d_%d' % (a, wi)], eng=('act' if a == 0 else 'dve'))
                for hf in range(2):
                    f16 = fc * 2 + hf
                    for a in range(2):
                        for kc in range(8):
                            P.mm(ph[a][:, 0:512], w13[a][wi][:, kc, hf * 128:(hf + 1) * 128], xsT[:, kc, 0:512], kc == 0, kc == 7, ['w%d_%d' % (a, wi), 'xsT'], ['ph%d' % a], r32=True)
                        if not last:
                            for kc in range(8):
                                P.mm(ph[a][:, 512:576], w13[a][wi][:, kc, hf * 128:(hf + 1) * 128], xsT[:, kc, 512:576], kc == 0, kc == 7, ['w%d_%d' % (a, wi), 'xsT'], ['ph%d' % a], r32=True)
                    P.act(sg[:, 0:NJ], ph[0][:, 0:NJ], AF.Silu, ['ph0'], ['sg'])
                    P.tt(gT[:, f16, 0:NJ].bitcast(F32R), sg[:, 0:NJ], ph[1][:, 0:NJ], ALU.mult, ['sg', 'ph1'], ['gT'])
            if e + 1 < NE:
                gather(e + 1)
            njt = 4 if last else 5
            for qq in range(4):
                P.dma(w2r[:], dr['exp_w2'][li, e][:, qq * 256:(qq + 1) * 256].rearrange("(k p) c -> p k c", p=128), [], ['w2r'], q='pool')
                P.cp(w2q[:, 0:8, :].bitcast(F32R), w2r[:, 0:8, :], ['w2r'], ['w2q'], eng='act')
                P.cp(w2q[:, 8:16, :].bitcast(F32R), w2r[:, 8:16, :], ['w2r'], ['w2q'], eng='dve')
                for jt in range(njt):
                    jn = 128 if jt < 4 else 2 * CAPC
                    b0 = (jt % 2) * 512
                    for f16 in range(16):
                        P.mm(pye[0:jn, b0:b0 + 256], gT[:, f16, jt * 128:jt * 128 + jn], w2q[:, f16, :], f16 == 0, f16 == 15, ['gT', 'w2q'], ['pye%d' % (jt % 2)], r32=True)
                    if jt < 4:
                        gcol = gatL[:, jt // 2, e * 2 + jt % 2:e * 2 + jt % 2 + 1]
                    else:
                        gcol = gatC[:, e:e + 1]
                    P.ts(ysb[0:jn, jt, qq * 256:(qq + 1) * 256], pye[0:jn, b0:b0 + 256], gcol, ALU.mult, ['pye%d' % (jt % 2), 'gatL', 'gatC'], ['ysb'], eng=('pool' if False else 'dve'))
            for jt in range(njt):
                if jt < 4:
                    iap = idxL[:, jt // 2, e * 2 + jt % 2:e * 2 + jt % 2 + 1]
                    src = ysb[:, jt, :]
                else:
                    iap = idxC[:, e:e + 1]
                    src = ysb[0:2 * CAPC, jt, :]
                P.S.dma('pool', None, None, reads=['ysb', 'idxL', 'idxC'], writes=['MOE'],
                        indirect=(lambda hh, iap=iap, src=src: hh.indirect_dma_start(
                            out=dr['MOE'][:, :], out_offset=bass.IndirectOffsetOnAxis(ap=iap, axis=0), in_=src, in_offset=None, compute_op=ALU.add)))


def phase_moe_residual(kb, C, li, s):
    dr = kb.dr
    last = (li == DEPTH - 1)
    with kb.phase() as P:
        gbc = P.sb("gbc", [128, 2, D])
        P.dma(gbc[:, 0, :], dr['MODV'][2, 5 * D:6 * D].partition_broadcast(128), ['MODV'], ['gbc'], q='sp')
        P.dma(gbc[:, 1, :], dr['MODV'][s, 5 * D:6 * D].partition_broadcast(128), ['MODV'], ['gbc'], q='sp')
        ht = [P.sb("rh%d" % i, [128, D]) for i in range(2)]
        mt = [P.sb("rm%d" % i, [128, D]) for i in range(2)]
        for tt in range(2 if last else 0, NT):
            i = tt % 2
            si = 0 if tt < 2 else 1
            tsl = slice(tt * 128, (tt + 1) * 128)
            P.dma(ht[i][:], dr['H'][s, tsl, :], ['H%d' % s], ['rh%d' % i])
            P.dma(mt[i][:], dr['MOE'][s * T + tt * 128:s * T + (tt + 1) * 128, :], ['MOE'], ['rm%d' % i])
            P.tt(mt[i][:], mt[i][:], gbc[:, si, :], ALU.mult, ['rm%d' % i, 'gbc'], ['rm%d' % i], eng=('pool' if i else 'dve'))
            P.tt(ht[i][:], ht[i][:], mt[i][:], ALU.add, ['rh%d' % i, 'rm%d' % i], ['rh%d' % i], eng=('pool' if i else 'dve'))
            P.dma(dr['H'][s, tsl, :], ht[i][:], ['rh%d' % i], ['H%d' % s])


def phase_final(kb, C):
    dr = kb.dr
    with kb.phase() as P:
        P._junk = P.sb("junk", [128, D]); P._ss = P.sb("ss", [128, 1]); P._eps = P.sb("eps", [128, 1])
        P.memset(P._eps[:], EPS, ['eps'])
        fw = P.sb("fw", [128, D])
        P.dma(fw[:], dr['final_norm_w'].partition_broadcast(128), [], ['modbc'], q='sp')
        hts = [P.sb("ht%d" % i, [128, D]) for i in range(2)]
        xms = [P.sb("xm%d" % i, [128, D]) for i in range(2)]
        n = 0
        for s in range(2):
            for tt in range(2, NT):
                i = n % 2; n += 1
                P.dma(hts[i][:], dr['H'][s, tt * 128:(tt + 1) * 128, :], ['H%d' % s], ['%dh' % i])
                norm_mod_tile(P, C, hts[i][:], xms[i][:], fw[:], None, '%d' % i)
                P.dma(dr['out'][s, (tt - 2) * 128:(tt - 1) * 128, :], xms[i][:], ['%dxm' % i], ['out'])


def phase_zero_scratch(kb, C):
    dr = kb.dr
    with kb.phase() as P:
        z = P.sb("zz", [128, T])
        P.memset(z[:], 0.0, ['zz'])
        for s in range(2):
            for a in range(NT):
                P.dma(dr['MIX'][s][a * 128:(a + 1) * 128, :], z[:, 0:768], ['zz'], ['MIX%d' % s])
            for a in range(2):
                P.dma(dr['S5T'][s][a * 128:(a + 1) * 128, :], z[:], ['zz'], ['S5T%d' % s])


def phase_outproj(kb, C, li, s):
    dr = kb.dr
    last = (li == DEPTH - 1)
    with kb.phase() as P:
        wo = P.sb("wo", [128, 8, D])
        with Sub(P) as Q:
            woraw = Q.sb("woraw", [128, 8, D])
            Q.dma(woraw[:], dr['w_out'][li].rearrange("(k p) c -> p k c", p=128), [], ['woraw'])
            Q.cp(wo[:, 0:4, :].bitcast(F32R), woraw[:, 0:4, :], ['woraw'], ['wo'], eng='act')
            Q.cp(wo[:, 4:8, :].bitcast(F32R), woraw[:, 4:8, :], ['woraw'], ['wo'], eng='dve')
        gbc = P.sb("gbc", [128, 2, D])
        P.dma(gbc[:, 0, :], dr['MODV'][2, 2 * D:3 * D].partition_broadcast(128), ['MODV'], ['gbc'], q='sp')
        P.dma(gbc[:, 1, :], dr['MODV'][s, 2 * D:3 * D].partition_broadcast(128), ['MODV'], ['gbc'], q='sp')
        mx = [P.sb("mx%d" % i, [128, 768]) for i in range(2)]
        mT = [P.sb("mT%d" % i, [128, 8, 128]) for i in range(2)]
        s5r = [P.sb("s5r%d" % i, [128, 2, 128]) for i in range(2)]
        ht = [P.sb("oh%d" % i, [128, D]) for i in range(2)]
        tp = [P.ps("otp%d" % i, [128, 768]) for i in range(2)]
        po = [P.ps("opo%d" % i, [128, D]) for i in range(2)]
        for tt in range(2 if last else 0, NT):
            i = tt % 2
            si = 0 if tt < 2 else 1
            tsl = slice(tt * 128, (tt + 1) * 128)
            P.dma(mx[i][:], dr['MIX'][s][tsl, :], ['MIX%d' % s], ['mx%d' % i])
            P.dma(s5r[i][:], dr['S5T'][s][:, tsl].rearrange("(k p) t -> p k t", p=128), ['S5T%d' % s], ['s5r%d' % i])
            P.dma(ht[i][:], dr['H'][s, tsl, :], ['H%d' % s], ['oh%d' % i])
            for j in range(6):
                P.tr(tp[i][:, j * 128:(j + 1) * 128], mx[i][:, j * 128:(j + 1) * 128], C['idn'][:], ['mx%d' % i], ['otp%d' % i])
            P.cp(mT[i][:, 0:2, :].bitcast(F32R), tp[i][:, 0:256].rearrange("p (k t) -> p k t", k=2), ['otp%d' % i], ['mT%d' % i], eng='act')
            P.cp(mT[i][:, 2:4, :].bitcast(F32R), s5r[i][:], ['s5r%d' % i], ['mT%d' % i], eng='pool' if False else 'act')
            P.cp(mT[i][:, 4:8, :].bitcast(F32R), tp[i][:, 256:768].rearrange("p (k t) -> p k t", k=4), ['otp%d' % i], ['mT%d' % i], eng='dve')
            for hf in range(2):
                for kc in range(8):
                    P.mm(po[i][:, hf * 512:(hf + 1) * 512], mT[i][:, kc, :], wo[:, kc, hf * 512:(hf + 1) * 512], kc == 0, kc == 7, ['mT%d' % i, 'wo'], ['opo%d' % i], r32=True)
            P.tt(po_sb(P, i)[:], po[i][:], gbc[:, si, :], ALU.mult, ['opo%d' % i, 'gbc'], ['osb%d' % i])
            P.tt(ht[i][:], ht[i][:], po_sb(P, i)[:], ALU.add, ['oh%d' % i, 'osb%d' % i], ['oh%d' % i], eng='pool')
            P.dma(dr['H'][s, tsl, :], ht[i][:], ['oh%d' % i], ['H%d' % s])


def po_sb(P, i):
    if not hasattr(P, '_posb'):
        P._posb = [P.sb("osb%d" % k, [128, D]) for k in range(2)]
    return P._posb[i]


def declare_io(kb, nlayers=DEPTH):
    L = nlayers
    kb.dram_in('x', [2, NLAT, D]); kb.dram_in('ctx', [2, NCTX, D]); kb.dram_in('c', [2, D]); kb.dram_in('c_ctx', [D])
    kb.dram_in('mod_w', [L, D, 6 * D]); kb.dram_in('mod_b', [L, 6 * D])
    kb.dram_in('norm1_w', [L, D]); kb.dram_in('norm2_w', [L, D])
    kb.dram_in('WT', [L, D, NZT]); kb.dram_in('BT', [L, NZT]); kb.dram_in('WF', [L, D, NZF]); kb.dram_in('BF', [L, NZF])
    kb.dram_in('c_idn', [128, 128]); kb.dram_in('c_maskf', [128, 128]); kb.dram_in('c_maskb', [128, 128]); kb.dram_in('c_onesbd', [128, 128])
    kb.dram_in('c_chm', [128, 4]); kb.dram_in('c_rm', [2, 64, T])
    kb.dram_in('hg_lb_logits', [DEPTH, 2, 256]); kb.dram_in('hg_norm_w', [L, 256]); kb.dram_in('ml_norm_w', [L, 256])
    kb.dram('LB', [DEPTH, 2, 256]); kb.dram('MIX', [2, T, 768]); kb.dram('S5T', [2, 256, T]); kb.dram_in('w_out', [L, D, D])
    kb.dram_in('c_ropec', [128, NLAT]); kb.dram_in('c_ropes', [128, NLAT])
    for nm in ['da_lq1', 'da_lk1', 'da_lq2', 'da_lk2']:
        kb.dram_in(nm, [L, 32])
    kb.dram_in('da_norm_w', [L, 256])
    for nm in ['s5_lam_re', 's5_lam_im', 'S5_LOGDT']:
        kb.dram_in(nm, [L, 2, 16, 64])
    kb.dram_in('S5_WBre', [L, 256, 1024]); kb.dram_in('S5_WBim', [L, 256, 1024]); kb.dram_in('S5_WCre', [L, 1024, 256]); kb.dram_in('S5_WCim', [L, 1024, 256])
    kb.dram_in('s5_d', [L, 256]); kb.dram_in('s5_glu_w', [L, 256, 256]); kb.dram_in('s5_glu_b', [L, 256]); kb.dram_in('c_iota256', [128, 256])
    kb.dram_in('router_w', [L, D, NE]); kb.dram_in('exp_w1', [L, NE, D, 2 * D]); kb.dram_in('exp_w3', [L, NE, D, 2 * D]); kb.dram_in('exp_w2', [L, NE, 2 * D, D])
    kb.dram_in('c_tok', [128, NT])
    kb.dram('DBG_POS', [NE, T]); kb.dram('DBG_AFF', [NE, T]); kb.dram('DBG_PTM', [128, NT, NE])
    kb.dram('XM2', [2 * T, D]); kb.dram('MOE', [2 * T, D])
    kb.dram('IDXL', [2, 128, 2 * NE], I32); kb.dram('GATEL', [2, 128, 2 * NE]); kb.dram('IDXC', [2 * CAPC, NE], I32); kb.dram('GATEC', [2 * CAPC, NE])
    kb.dram_in('final_norm_w', [D]); kb.dram('out', [2, NLAT, D], out=True)
    kb.dram('H', [2, T, D]); kb.dram('ZT', [2, T, NZT]); kb.dram('ZF', [2, NZF, T]); kb.dram('MODV', [3, 6 * D])


def host_inputs(inp, core, L=DEPTH):
    zt, zf = colmaps()
    b0 = 2 * core
    m = {}
    m['x'] = np.ascontiguousarray(inp['x'][b0:b0 + 2]); m['ctx'] = np.ascontiguousarray(inp['ctx'][b0:b0 + 2])
    m['c'] = np.ascontiguousarray(inp['c'][b0:b0 + 2]); m['c_ctx'] = inp['c_ctx']
    return m


def host_shared(inp, L=DEPTH):
    zt, zf = colmaps()
    m = {}
    for k in ['mod_w', 'mod_b', 'norm1_w', 'norm2_w']:
        m[k] = np.ascontiguousarray(inp[k][:L])
    m['WT'] = np.ascontiguousarray(inp['w_in'][:L][:, :, zt]); m['BT'] = np.ascontiguousarray(inp['b_in'][:L][:, zt])
    m['WF'] = np.ascontiguousarray(inp['w_in'][:L][:, :, zf]); m['BF'] = np.ascontiguousarray(inp['b_in'][:L][:, zf])
    m['c_idn'] = np.eye(128, dtype=np.float32)
    m['final_norm_w'] = inp['final_norm_w']
    p = np.arange(128)
    same = (p[:, None] // 32) == (p[None, :] // 32)
    m['c_maskf'] = (same & (p[:, None] <= p[None, :])).astype(np.float32)
    m['c_maskb'] = (same & (p[:, None] >= p[None, :])).astype(np.float32)
    m['c_onesbd'] = same.astype(np.float32)
    m['c_chm'] = (p[:, None] // 32 == np.arange(4)[None, :]).astype(np.float32)
    t = np.arange(T)
    rmm = np.stack([(t % 32 != 0), (t % 32 != 31)]).astype(np.float32)
    m['c_rm'] = np.ascontiguousarray(np.broadcast_to(rmm[:, None, :], (2, 64, T)))
    m['hg_lb_logits'] = np.ascontiguousarray(inp['hg_lb_logits'])
    for k in ['router_w', 'exp_w1', 'exp_w3', 'exp_w2']:
        m[k] = inp[k] if L == inp[k].shape[0] else np.ascontiguousarray(inp[k][:L])
    m['c_tok'] = (np.arange(128, dtype=np.float32)[:, None] + 128.0 * np.arange(NT, dtype=np.float32)[None, :]).astype(np.float32)
    for k in ['s5_lam_re', 's5_lam_im', 's5_d', 's5_glu_w', 's5_glu_b']:
        m[k] = np.ascontiguousarray(inp[k][:L])
    m['S5_LOGDT'] = np.ascontiguousarray(np.broadcast_to(inp['s5_log_dt'][:L][..., None], (L, 2, 16, 64)))
    m['c_iota256'] = np.ascontiguousarray(np.broadcast_to(np.arange(256, dtype=np.float32)[None, :], (128, 256)))
    WBr = np.zeros((L, 256, 1024), np.float32); WBi = np.zeros((L, 256, 1024), np.float32)
    WCr = np.zeros((L, 1024, 256), np.float32); WCi = np.zeros((L, 1024, 256), np.float32)
    for g in range(16):
        WBr[:, g * 16:(g + 1) * 16, g * 64:(g + 1) * 64] = np.transpose(inp['s5_b_re'][:L, g], (0, 2, 1))
        WBi[:, g * 16:(g + 1) * 16, g * 64:(g + 1) * 64] = np.transpose(inp['s5_b_im'][:L, g], (0, 2, 1))
        WCr[:, g * 64:(g + 1) * 64, g * 16:(g + 1) * 16] = np.transpose(inp['s5_c_re'][:L, g], (0, 2, 1))
        WCi[:, g * 64:(g + 1) * 64, g * 16:(g + 1) * 16] = np.transpose(inp['s5_c_im'][:L, g], (0, 2, 1))
    m['S5_WBre'], m['S5_WBim'], m['S5_WCre'], m['S5_WCim'] = WBr, WBi, WCr, WCi
    tl = np.arange(NLAT)
    inv = 10000.0 ** (-np.arange(0, 16, 2, dtype=np.float32) / np.float32(16))
    rowp = (tl // 64).astype(np.float32); colp = (tl % 64).astype(np.float32)
    ang = np.concatenate([rowp[:, None] * inv[None, :], colp[:, None] * inv[None, :]], axis=-1).astype(np.float32)
    dd = np.arange(128) % 32
    cosT = np.cos(ang)[:, dd // 2].T.astype(np.float32)
    sinT = np.sin(ang)[:, dd // 2].T.astype(np.float32)
    sgn = np.where(dd % 2 == 0, -1.0, 1.0).astype(np.float32)
    m['c_ropec'] = np.ascontiguousarray(cosT)
    m['c_ropes'] = np.ascontiguousarray(sinT * sgn[:, None])
    for k in ['hg_norm_w', 'ml_norm_w', 'w_out', 'da_lq1', 'da_lk1', 'da_lq2', 'da_lk2', 'da_norm_w']:
        m[k] = np.ascontiguousarray(inp[k][:L])
    return m


def copy_inputs_to_H(kb, C):
    dr = kb.dr
    with kb.phase() as P:
        bufs = [P.sb("cpb%d" % i, [128, 4, D]) for i in range(2)]
        n = 0
        for s in range(2):
            srcs = [(dr['ctx'][s], 0, NCTX), (dr['x'][s], NCTX, NLAT)]
            for (src, base, cnt) in srcs:
                for r0 in range(0, cnt, 512):
                    rn = min(512, cnt - r0)
                    i = n % 2
                    n += 1
                    P.dma(bufs[i][:, 0:rn // 128, :], src[r0:r0 + rn, :].rearrange("(a p) d -> p a d", p=128), [], ['cpb%d' % i])
                    P.dma(dr['H'][s, base + r0:base + r0 + rn, :].rearrange("(a p) d -> p a d", p=128), bufs[i][:, 0:rn // 128, :], ['cpb%d' % i], ['H%d' % s])


def build_program(debug=(), upto='all', nlayers=DEPTH, mixers='ABCD'):
    nc = bass.Bass("TRN2", target_bir_lowering=False)
    with ExitStack() as es:
        kb = KB(nc, es, debug)
        declare_io(kb, nlayers)
        with kb.phase() as PC:
            C = load_consts(kb, PC)
            kb.S.barrier()
            copy_inputs_to_H(kb, C)
            phase_lb(kb, C)
            for li in range(nlayers):
                phase_mod(kb, C, li)
                for s in range(2):
                    phase_inproj(kb, C, li, s)
                    if upto == 'inproj':
                        break
                    if 'A' in mixers:
                        phase_gla(kb, C, li, s, 'A')
                    if 'D' in mixers:
                        phase_gla(kb, C, li, s, 'D')
                    if 'C' in mixers:
                        phase_attn(kb, C, li, s)
                    if 'B' in mixers:
                        phase_s5(kb, C, li, s)
                    if upto == 'mix1':
                        break
                    phase_outproj(kb, C, li, s)
                    if upto == 'outproj1':
                        continue
                    phase_moe_route(kb, C, li, s)
                if upto not in ('inproj', 'mix1', 'outproj1'):
                    phase_moe_experts(kb, C, li)
                    for s in range(2):
                        phase_moe_residual(kb, C, li, s)
                if upto in ('inproj', 'mix1', 'outproj1', 'layer1'):
                    break
            phase_final(kb, C)
        kb.S.finish()
    return nc, kb


def kernel(**inputs):
    inp = {k: np.asarray(v) for k, v in inputs.items()}
    nc, kb = build_program()
    shared = host_shared(inp)
    in_maps = []
    for core in range(8):
        m = host_inputs(inp, core)
        m.update(shared)
        in_maps.append({k: v for k, v in m.items() if k in kb.dr})
    res = run_bass_kernel_spmd(nc, in_maps, core_ids=list(range(8)))
    out = np.concatenate([r['out'] for r in res.results], axis=0)
    return out.astype(np.float32)
```
